# Optimizing a Trainium2 kernel written in Bass

```python
import jax, jax.numpy as jnp
from jax import lax
import numpy as np

D_MODEL = 1024
BATCH = 4
SEQ = 8192
DEPTH = 1

PLE_DIM = 256
ATT_GROUPS = ((128, 1), (512, 4), (2048, 16))
N_ATT_GROUPS = 3
ATT_SLOTS = 4
ATT_HEADS = N_ATT_GROUPS * ATT_SLOTS
ATT_HEAD_DIM = 128
ATT_WIDTH = ATT_HEADS * ATT_HEAD_DIM
ATT_OUT_WIDTH = ATT_SLOTS * ATT_HEAD_DIM
ROPE_THETA = 500000.0
ROPE_DIM = ATT_HEAD_DIM // 4
MLSTM_HEADS = 4
MLSTM_HEAD_DIM = 128
MLSTM_WIDTH = MLSTM_HEADS * MLSTM_HEAD_DIM
MLSTM_CHUNK = 128
CONV_WIDTH = 4
D_FF = 2816
NORM_EPS = 1e-6
IN_WIDTHS = (ATT_WIDTH, ATT_WIDTH, ATT_WIDTH,
             MLSTM_WIDTH, MLSTM_WIDTH, MLSTM_WIDTH, MLSTM_WIDTH,
             MLSTM_HEADS, MLSTM_HEADS,
             D_MODEL, D_MODEL)
D_IN = sum(IN_WIDTHS)

kernel_name = 'hybrid_dilated_attn_mlstm_macaron_block'


def rms_norm(x, g):
    xf = x.astype(jnp.float32)
    y = xf * lax.rsqrt(jnp.mean(xf * xf, axis=-1, keepdims=True) + NORM_EPS)
    return (y * g.astype(jnp.float32)).astype(x.dtype)


def swiglu(x, w_in, w_out):
    a, b = jnp.split(x @ w_in, 2, axis=-1)
    return (jax.nn.silu(a) * b) @ w_out


def split_columns(z):
    offs, acc = [], 0
    for w in IN_WIDTHS[:-1]:
        acc += w
        offs.append(acc)
    return jnp.split(z, offs, axis=-1)


def rope_partial(x, pos):
    half = ROPE_DIM // 2
    inv_freq = 1.0 / (ROPE_THETA ** (jnp.arange(half, dtype=jnp.float32) / half))
    ang = pos[:, None] * inv_freq[None, :]
    cos = jnp.cos(ang)[:, None, None, :]
    sin = jnp.sin(ang)[:, None, None, :]
    x1, x2, rest = x[..., :half], x[..., half:ROPE_DIM], x[..., ROPE_DIM:]
    return jnp.concatenate([x1 * cos - x2 * sin, x2 * cos + x1 * sin, rest], axis=-1)


def dilated_window_attention(q, k, v, window, dilation):
    B, S, H, E = q.shape
    wd = window // dilation
    n = S // dilation
    nb = -(-n // wd)
    npad = nb * wd

    def to_sub(t):
        t = t.reshape(B, n, dilation, H, E).transpose(0, 2, 3, 1, 4)
        return jnp.pad(t, ((0, 0), (0, 0), (0, 0), (0, npad - n), (0, 0)))

    def kv_blocks(t):
        t = jnp.pad(to_sub(t), ((0, 0), (0, 0), (0, 0), (wd, 0), (0, 0)))
        t = t.reshape(B, dilation, H, nb + 1, wd, E)
        return jnp.concatenate([t[:, :, :, :-1], t[:, :, :, 1:]], axis=4)

    qb = to_sub(q).reshape(B, dilation, H, nb, wd, E)
    kb, vb = kv_blocks(k), kv_blocks(v)
    s = jnp.einsum('brhnqe,brhnke->brhnqk', qb, kb) * (E ** -0.5)
    qi = jnp.arange(wd)[:, None]
    kj = jnp.arange(2 * wd)[None, :]
    blk = jnp.arange(nb)[:, None, None]
    mask = (kj >= qi) & (kj <= qi + wd) & ((blk > 0) | (kj >= wd))
    s = jnp.where(mask, s, -jnp.inf)
    m = jnp.max(s, axis=-1, keepdims=True)
    e = jnp.exp(s - m)
    den = jnp.sum(e, axis=-1)
    o = jnp.einsum('brhnqk,brhnke->brhnqe', e, vb) / den[..., None]
    lse = m[..., 0] + jnp.log(den)

    def from_sub(t):
        t = t.reshape(B, dilation, H, npad, *t.shape[5:])[:, :, :, :n]
        t = jnp.moveaxis(t, 3, 1)
        return t.reshape(B, S, H, *t.shape[4:])

    return from_sub(o), from_sub(lse)


def dilated_attention_mixer(q, k, v, q_gain, k_gain):
    B, S, _ = q.shape
    shape = (B, S, N_ATT_GROUPS, ATT_SLOTS, ATT_HEAD_DIM)
    pos = jnp.arange(S, dtype=jnp.float32)
    q = rope_partial(rms_norm(q.reshape(shape).astype(jnp.float32), q_gain), pos)
    k = rope_partial(rms_norm(k.reshape(shape).astype(jnp.float32), k_gain), pos)
    v = v.reshape(shape).astype(jnp.float32)
    outs, lses = [], []
    for g, (window, dilation) in enumerate(ATT_GROUPS):
        o, l = dilated_window_attention(q[:, :, g], k[:, :, g], v[:, :, g], window, dilation)
        outs.append(o)
        lses.append(l)
    alpha = jax.nn.softmax(jnp.stack(lses, axis=0), axis=0)
    out = jnp.einsum('gbsh,gbshe->bshe', alpha, jnp.stack(outs, axis=0))
    return out.reshape(B, S, ATT_OUT_WIDTH)


def causal_dwconv(x, w, b):
    K = w.shape[0]
    S = x.shape[1]
    xp = jnp.pad(x, ((0, 0), (K - 1, 0), (0, 0)))
    out = b
    for j in range(K):
        out = out + xp[:, j:j + S] * w[j]
    return out


def mlstm_chunkwise(q, k, v, logi, logf):
    B, S, H, E = q.shape
    L = MLSTM_CHUNK
    nc = S // L

    def chunks(t):
        return t.reshape(B, nc, L, H, E).transpose(1, 0, 3, 2, 4)

    def gchunks(t):
        return t.reshape(B, nc, L, H).transpose(1, 0, 3, 2)

    causal = jnp.tril(jnp.ones((L, L), dtype=bool))

    def step(carry, inp):
        C, n, m = carry
        qc, kc, vc, li, lf = inp
        b = jnp.cumsum(lf, axis=-1)
        Dm = b[..., :, None] - b[..., None, :] + li[..., None, :]
        Dm = jnp.where(causal, Dm, -jnp.inf)
        inter = b + m[..., None]
        m_t = jnp.maximum(inter, jnp.max(Dm, axis=-1))
        W = jnp.exp(Dm - m_t[..., None])
        a = jnp.exp(inter - m_t)
        qk = jnp.einsum('bhte,bhse->bhts', qc, kc) * W
        num = a[..., None] * jnp.einsum('bhte,bhef->bhtf', qc, C) + jnp.einsum('bhts,bhsf->bhtf', qk, vc)
        den = a * jnp.einsum('bhte,bhe->bht', qc, n) + jnp.sum(qk, axis=-1)
        h = num / jnp.maximum(jnp.abs(den), jnp.exp(-m_t))[..., None]
        bL = b[..., -1]
        g = bL[..., None] - b + li
        m_new = jnp.maximum(bL + m, jnp.max(g, axis=-1))
        wk = jnp.exp(g - m_new[..., None])
        decay = jnp.exp(bL + m - m_new)
        C_new = decay[..., None, None] * C + jnp.einsum('bhs,bhse,bhsf->bhef', wk, kc, vc)
        n_new = decay[..., None] * n + jnp.einsum('bhs,bhse->bhe', wk, kc)
        return (C_new, n_new, m_new), h

    init = (jnp.zeros((B, H, E, E), jnp.float32),
            jnp.zeros((B, H, E), jnp.float32),
            jnp.zeros((B, H), jnp.float32))
    _, hs = lax.scan(step, init, (chunks(q), chunks(k), chunks(v), gchunks(logi), gchunks(logf)))
    return hs.transpose(1, 0, 3, 2, 4).reshape(B, S, H, E)


def mlstm_mixer(q, k, v, o_pre, i_pre, f_pre, conv_w, conv_b, i_bias, f_bias):
    B, S, _ = q.shape
    qk = jax.nn.silu(causal_dwconv(jnp.concatenate([q, k], axis=-1), conv_w, conv_b))
    q, k = jnp.split(qk, 2, axis=-1)
    shape = (B, S, MLSTM_HEADS, MLSTM_HEAD_DIM)
    qh = q.reshape(shape).astype(jnp.float32)
    kh = k.reshape(shape).astype(jnp.float32) * (MLSTM_HEAD_DIM ** -0.5)
    vh = v.reshape(shape).astype(jnp.float32)
    logi = (i_pre + i_bias).astype(jnp.float32)
    logf = jax.nn.log_sigmoid((f_pre + f_bias).astype(jnp.float32))
    h = mlstm_chunkwise(qh, kh, vh, logi, logf).reshape(B, S, MLSTM_WIDTH)
    return (jax.nn.sigmoid(o_pre.astype(jnp.float32)) * h).astype(q.dtype)


def hybrid_layer(x, p_i, ffn1_norm, ffn1_w_in, ffn1_w_out, mix_norm, w_in, q_gain, k_gain,
                 conv_w, conv_b, i_bias, f_bias, w_up_att, w_up_mlstm, w_out,
                 ffn2_norm, ffn2_w_in, ffn2_w_out, ple_norm, w_ple_gate, w_ple_proj):
    h = x + 0.5 * swiglu(rms_norm(x, ffn1_norm), ffn1_w_in, ffn1_w_out)
    z = rms_norm(h, mix_norm) @ w_in
    aq, ak, av, mq, mk, mv, mo, mi, mf, ga, gb = split_columns(z)
    y_att = dilated_attention_mixer(aq, ak, av, q_gain, k_gain).astype(x.dtype) @ w_up_att
    y_ml = mlstm_mixer(mq, mk, mv, mo, mi, mf, conv_w, conv_b, i_bias, f_bias) @ w_up_mlstm
    merged = jax.nn.sigmoid(ga) * y_att + jax.nn.sigmoid(gb) * y_ml
    h = h + merged @ w_out
    h = h + 0.5 * swiglu(rms_norm(h, ffn2_norm), ffn2_w_in, ffn2_w_out)
    h = h + (p_i @ w_ple_proj) * jax.nn.sigmoid(rms_norm(h, ple_norm) @ w_ple_gate)
    return h


def setup_inputs(seed: int = 0) -> dict:
    key = jax.random.key(seed)
    ks = jax.random.split(key, 24)
    f32 = jnp.float32

    def nrm(k, shape, scale):
        return jax.random.normal(k, shape, f32) * scale

    def gain(k, shape):
        return 1.0 + 0.02 * jax.random.normal(k, shape, f32)

    return {
        'x': nrm(ks[0], (BATCH, SEQ, D_MODEL), 1.0),
        'p': nrm(ks[1], (DEPTH, BATCH, SEQ, PLE_DIM), 1.0),
        'ffn1_norm': gain(ks[2], (DEPTH, D_MODEL)),
        'ffn1_w_in': nrm(ks[3], (DEPTH, D_MODEL, 2 * D_FF), D_MODEL ** -0.5),
        'ffn1_w_out': nrm(ks[4], (DEPTH, D_FF, D_MODEL), D_FF ** -0.5),
        'mix_norm': gain(ks[5], (DEPTH, D_MODEL)),
        'w_in': nrm(ks[6], (DEPTH, D_MODEL, D_IN), D_MODEL ** -0.5),
        'q_gain': gain(ks[7], (DEPTH, ATT_HEAD_DIM)),
        'k_gain': gain(ks[8], (DEPTH, ATT_HEAD_DIM)),
        'conv_w': nrm(ks[9], (DEPTH, CONV_WIDTH, 2 * MLSTM_WIDTH), CONV_WIDTH ** -0.5),
        'conv_b': nrm(ks[10], (DEPTH, 2 * MLSTM_WIDTH), 0.02),
        'i_bias': nrm(ks[11], (DEPTH, MLSTM_HEADS), 0.1),
        'f_bias': jnp.linspace(3.0, 6.0, MLSTM_HEADS, dtype=f32)[None, :] + nrm(ks[12], (DEPTH, MLSTM_HEADS), 0.1),
        'w_up_att': nrm(ks[13], (DEPTH, ATT_OUT_WIDTH, D_MODEL), ATT_OUT_WIDTH ** -0.5),
        'w_up_mlstm': nrm(ks[14], (DEPTH, MLSTM_WIDTH, D_MODEL), MLSTM_WIDTH ** -0.5),
        'w_out': nrm(ks[15], (DEPTH, D_MODEL, D_MODEL), D_MODEL ** -0.5),
        'ffn2_norm': gain(ks[16], (DEPTH, D_MODEL)),
        'ffn2_w_in': nrm(ks[17], (DEPTH, D_MODEL, 2 * D_FF), D_MODEL ** -0.5),
        'ffn2_w_out': nrm(ks[18], (DEPTH, D_FF, D_MODEL), D_FF ** -0.5),
        'ple_norm': gain(ks[19], (DEPTH, D_MODEL)),
        'w_ple_gate': nrm(ks[20], (DEPTH, D_MODEL, D_MODEL), D_MODEL ** -0.5),
        'w_ple_proj': nrm(ks[21], (DEPTH, PLE_DIM, D_MODEL), PLE_DIM ** -0.5),
    }


def reference(x, p, ffn1_norm, ffn1_w_in, ffn1_w_out, mix_norm, w_in, q_gain, k_gain,
              conv_w, conv_b, i_bias, f_bias, w_up_att, w_up_mlstm, w_out,
              ffn2_norm, ffn2_w_in, ffn2_w_out, ple_norm, w_ple_gate, w_ple_proj):
    h = x
    for i in range(DEPTH):
        h = hybrid_layer(h, p[i], ffn1_norm[i], ffn1_w_in[i], ffn1_w_out[i], mix_norm[i], w_in[i],
                         q_gain[i], k_gain[i], conv_w[i], conv_b[i], i_bias[i], f_bias[i],
                         w_up_att[i], w_up_mlstm[i], w_out[i], ffn2_norm[i], ffn2_w_in[i],
                         ffn2_w_out[i], ple_norm[i], w_ple_gate[i], w_ple_proj[i])
    return h
```

```python
import os
import numpy as np
from contextlib import ExitStack
import concourse.bass as bass
import concourse.mybir as mybir
from concourse.bass_utils import run_bass_kernel_spmd

F32 = mybir.dt.float32
BF16 = mybir.dt.bfloat16
AF = mybir.ActivationFunctionType
ALU = mybir.AluOpType
AX = mybir.AxisListType

D = 1024
DFF = 2816
NTOK = 8192
OWN = 4096
DIN = 8712
EPS = 1e-6
C_AQ, C_AK, C_AV = 0, 1536, 3072
C_MQ, C_MK, C_MV, C_MO, C_MI, C_MF, C_GA, C_GB = 4608, 5120, 5632, 6144, 6656, 6660, 6664, 7688
DILS = (1, 4, 16)


class Sem:
    def __init__(self, h):
        self.h = h
        self.n = 0


class Prog:
    ENG = ('pe', 'act', 'dve', 'pool', 'sp')

    def __init__(self, nc, es):
        self.nc, self.es = nc, es
        self.e = {'pe': nc.tensor, 'act': nc.scalar, 'dve': nc.vector, 'pool': nc.gpsimd, 'sp': nc.sync}
        self.prog = {}
        self.seen = {k: {} for k in self.ENG}
        self.lastw = {}
        self.rd = {}
        self.nsem = 0
        self.allsems = []
        self.dsems = {}
        self.new_epoch()

    def mksem(self):
        self.nsem += 1
        s = Sem(self.es.enter_context(self.nc.semaphore("s%d" % self.nsem)))
        self.allsems.append(s)
        return s

    def new_epoch(self):
        for k in self.ENG:
            self.prog[k] = self.mksem()

    def dsem(self, name):
        if name not in self.dsems:
            self.dsems[name] = self.mksem()
        return self.dsems[name]

    def op(self, eng, fn, reads=(), writes=(), dsem=None):
        need = {}

        def add(t):
            if t is None:
                return
            sem, v = t
            if need.get(sem, 0) < v:
                need[sem] = v
        for k in reads:
            add(self.lastw.get(k))
        for k in writes:
            add(self.lastw.get(k))
            for sem, v in self.rd.get(k, {}).items():
                add((sem, v))
        E = self.e[eng]
        for sem, v in need.items():
            if eng == 'pe' and sem is self.prog['pe']:
                continue
            if self.seen[eng].get(sem, 0) >= v:
                continue
            E.wait_ge(sem.h, v)
            self.seen[eng][sem] = v
        ins = fn(E)
        if dsem is not None:
            sem = self.dsem(dsem) if isinstance(dsem, str) else dsem
            sem.n += 16
            ins.then_inc(sem.h, 16)
        else:
            sem = self.prog[eng]
            sem.n += 1
            ins.then_inc(sem.h, 1)
        tok = (sem, sem.n)
        for k in reads:
            d = self.rd.setdefault(k, {})
            if d.get(sem, 0) < sem.n:
                d[sem] = sem.n
        for k in writes:
            self.lastw[k] = tok
            self.rd[k] = {}
        return tok

    def barrier(self):
        for eng in self.ENG:
            E = self.e[eng]
            for sem in self.allsems:
                if sem.n > self.seen[eng].get(sem, 0):
                    E.wait_ge(sem.h, sem.n)
                    self.seen[eng][sem] = sem.n
        self.lastw = {}
        self.rd = {}
        self.new_epoch()


def mm_group(P, out, pairs, reads, writes):
    n = len(pairs)

    def fn(E):
        ins = None
        for i, (l, r) in enumerate(pairs):
            ins = E.matmul(out, l, r, start=(i == 0), stop=(i == n - 1))
        return ins
    return P.op('pe', fn, reads=reads, writes=writes)


def load_w_groups(P, dst, src, ngroups, nj, key, eng='pool'):
    bounds = [round(i * nj / ngroups) for i in range(ngroups + 1)]
    for g in range(ngroups):
        j0, j1 = bounds[g], bounds[g + 1]
        if j1 == j0:
            continue
        ks = [(key, j) for j in range(j0, j1)]
        P.op(eng, lambda E, j0=j0, j1=j1: E.dma_start(out=dst[:, :, j0 * 128:j1 * 128], in_=src[:, :, j0 * 128:j1 * 128]),
             writes=ks, dsem="%s_g%d" % (key, g))


def load_cast(P, stage, pieces, engs=('pool', 'act')):
    for (dst, src, key) in pieces:
        i = P.lc_i = getattr(P, 'lc_i', -1) + 1
        sl = i % len(stage)
        n = dst.shape[-1]
        st = stage[sl][:, 0:n]
        P.op('sp', lambda E: E.dma_start(out=st, in_=src), writes=[('stg', sl)], dsem="stg%d" % sl)
        eng = engs[i % len(engs)]
        if eng == 'act':
            P.op('act', lambda E: E.copy(out=dst, in_=st), reads=[('stg', sl)], writes=[key])
        else:
            P.op(eng, lambda E: E.tensor_copy(dst, st), reads=[('stg', sl)], writes=[key])


def rms_stats(P, nc, src_aps, skeys, sq, sqk, ones_bf, ps_ap, psk, tmp, tmpk, rstd, rstdk, inv_n, sq_eng='act'):
    nchunk = len(src_aps)
    for c in range(nchunk):
        s = sq[c % len(sq)]
        sk = sqk[c % len(sq)]
        P.op('act', lambda E, s=s, c=c: E.activation(out=s, in_=src_aps[c], func=AF.Square),
             reads=[skeys[c]], writes=[sk])
        P.op('pe', lambda E, s=s, c=c: E.matmul(ps_ap, ones_bf, s, start=(c == 0), stop=(c == nchunk - 1)),
             reads=[sk, 'ones'], writes=[psk])
    P.op('dve', lambda E: E.tensor_scalar(tmp, ps_ap, inv_n, EPS, op0=ALU.mult, op1=ALU.add), reads=[psk], writes=[tmpk])
    P.op('act', lambda E: E.activation(out=tmp, in_=tmp, func=AF.Sqrt), reads=[tmpk], writes=[tmpk])
    P.op('dve', lambda E: E.reciprocal(rstd, tmp), reads=[tmpk], writes=[rstdk])


def ffn_phase(P, nc, T, which):
    TT = 256
    first = (which == 1)
    ntiles = (NTOK if first else OWN) // TT
    ntiles = int(os.environ.get('KNT', ntiles))
    own0 = (NTOK - OWN) // TT if first else 0
    w_in = T['ffn1_w_in'] if first else T['ffn2_w_in']
    w_out = T['ffn1_w_out'] if first else T['ffn2_w_out']
    src = T['xT'] if first else T['h2']
    with ExitStack() as ph:
        def sb(name, shape, dt):
            return ph.enter_context(nc.sbuf_tensor("p%d_%s" % (which, name), shape, dt))

        def pst(name, shape, dt=F32):
            return ph.enter_context(nc.psum_tensor("p%d_ps_%s" % (which, name), shape, dt))
        w1 = sb("w1", [128, 8, 2 * DFF], BF16)
        w2 = sb("w2", [128, 22, D], BF16)
        g1 = sb("g1", [128, 8], F32)
        g2 = sb("g2", [128, 8], F32)
        ones_bf = sb("ones", [128, 128], BF16)
        xh = [sb("xh%d" % i, [128, 8, TT], F32) for i in range(3)]
        xn = [sb("xn%d" % i, [128, 8, TT], BF16) for i in range(2)]
        un = [sb("un%d" % i, [128, 8, TT], BF16) for i in range(2)] if first else xn
        unk = 'un' if first else 'xn'
        sq = [sb("sq%d" % i, [128, TT], BF16) for i in range(2)]
        act = sb("act", [128, 22, TT], BF16)
        sil = [sb("sil%d" % i, [128, TT], BF16) for i in range(2)]
        tmp = [sb("tmp%d" % i, [128, TT], F32) for i in range(2 if first else 1)]
        rstd = [sb("rstd%d" % i, [128, TT], F32) for i in range(2)]
        stage = [sb("stg%d" % i, [128, 512], F32) for i in range(3 if first else 2)]
        ps_ss = pst("ss", [128, 512])
        ps_a = [pst("a%d" % i, [128, 512]) for i in range(2)]
        ps_o = [pst("o%d" % i, [128, 512]) for i in range(2)]
        if not first:
            wpg = sb("wpg", [128, 8, D], BF16)
            wpe = sb("wpe", [128, 2, D], BF16)
            pt = [sb("pt%d" % i, [128, 2, TT], BF16) for i in range(2)]
            sg = [sb("sg%d" % i, [128, TT], F32) for i in range(1)]
            ps_g = [pst("g%d" % i, [128, 512]) for i in range(2)]

        P.op('pool', lambda E: E.memset(ones_bf[:], 1.0), writes=['ones'])
        P.op('sp', lambda E: E.dma_start(out=g1[:], in_=T['ffn1_norm' if first else 'ffn2_norm'][:, :]), writes=['g1'], dsem="p%d_g1" % which)
        P.op('sp', lambda E: E.dma_start(out=g2[:], in_=T['mix_norm' if first else 'ple_norm'][:, :]), writes=['g2'], dsem="p%d_g2" % which)
        w_in_r = w_in.rearrange("(c p) n -> p c n", p=128)
        w_out_r = w_out.rearrange("(j p) n -> p j n", p=128)
        pieces = []
        for jg in range(0, 22, 4):
            j1 = min(jg + 4, 22)
            for part in range(2):
                for k in range(8):
                    c0 = part * DFF + jg * 128
                    c1 = part * DFF + j1 * 128
                    pieces.append((w1[:, k, c0:c1], w_in_r[:, k, c0:c1], ('w1', part, k, jg // 4)))
        for j in range(22):
            for hh in range(2):
                pieces.append((w2[:, j, hh * 512:(hh + 1) * 512], w_out_r[:, j, hh * 512:(hh + 1) * 512], ('w2', j, hh)))
        if not first:
            wpg_r = T['w_ple_gate'].rearrange("(c p) n -> p c n", p=128)
            wpe_r = T['w_ple_proj'].rearrange("(c p) n -> p c n", p=128)
            for k in range(8):
                for hh in range(2):
                    pieces.append((wpg[:, k, hh * 512:(hh + 1) * 512], wpg_r[:, k, hh * 512:(hh + 1) * 512], ('wpg', k, hh)))
            for k in range(2):
                for hh in range(2):
                    pieces.append((wpe[:, k, hh * 512:(hh + 1) * 512], wpe_r[:, k, hh * 512:(hh + 1) * 512], ('wpe', k, hh)))
        load_cast(P, stage, pieces)

        src_r = src.rearrange("(c p) n -> p c n", p=128)

        def load_x(i):
            s = i % 3
            P.op('sp', lambda E: E.dma_start(out=xh[s][:], in_=src_r[:, :, i * TT:(i + 1) * TT]),
                 writes=[('xh', s, c) for c in range(8)], dsem="p%d_xh%d" % (which, s))
        def load_p(i):
            if not first:
                s2 = i % 2
                for kk in range(2):
                    P.op('pool', lambda E, kk=kk: E.dma_start(out=pt[s2][:, kk, :], in_=T['pT'][kk * 128:(kk + 1) * 128, i * TT:(i + 1) * TT]),
                         writes=[('pt', s2)], dsem="p2_pt%d" % s2)

        def norm(i, gain, gkey, dst, dkey):
            s = i % 3
            d = i % 2
            rms_stats(P, nc, [xh[s][:, c, :] for c in range(8)], [('xh', s, c) for c in range(8)],
                      [q[:] for q in sq], [('sq', 0), ('sq', 1)], ones_bf[:], ps_ss[:, 0:TT], 'ps_ss',
                      tmp[d % len(tmp)][:], ('tmp', d % len(tmp)), rstd[d][:], ('rstd', d), 1.0 / D)
            for c in range(8):
                P.op('dve', lambda E, c=c: E.scalar_tensor_tensor(dst[d][:, c, :], xh[s][:, c, :], gain[:, c:c + 1], rstd[d][:],
                                                                   op0=ALU.mult, op1=ALU.mult),
                     reads=[('xh', s, c), gkey, ('rstd', d)], writes=[(dkey, d, c)])

        def ffn_in(i):
            d = i % 2
            for j in range(22):
                b = j % 2
                pa = ps_a[b]
                mm_group(P, pa[:, 0:TT], [(w1[:, k, j * 128:(j + 1) * 128], xn[d][:, k, :]) for k in range(8)],
                         reads=[('w1', 0, k, j // 4) for k in range(8)] + [('xn', d, k) for k in range(8)], writes=[('bka', b)])
                mm_group(P, pa[:, TT:2 * TT], [(w1[:, k, DFF + j * 128:DFF + (j + 1) * 128], xn[d][:, k, :]) for k in range(8)],
                         reads=[('w1', 1, k, j // 4) for k in range(8)] + [('xn', d, k) for k in range(8)], writes=[('bka', b)])
                P.op('act', lambda E, b=b, pa=pa: E.activation(out=sil[b][:], in_=pa[:, 0:TT], func=AF.Silu),
                     reads=[('bka', b)], writes=[('sil', b)])
                P.op('dve', lambda E, b=b, pa=pa, j=j: E.tensor_tensor(act[:, j, :], sil[b][:], pa[:, TT:2 * TT], op=ALU.mult),
                     reads=[('sil', b), ('bka', b)], writes=[('act', j)])

        def ffn_out(i):
            s = i % 3
            for pr in range(4):
                bk = pr % 2
                for hf in range(2):
                    oc = pr * 2 + hf
                    po = ps_o[bk][:, hf * TT:(hf + 1) * TT]
                    mm_group(P, po, [(w2[:, j, oc * 128:(oc + 1) * 128], act[:, j, :]) for j in range(22)],
                             reads=[('w2', j, oc // 4) for j in range(22)] + [('act', j) for j in range(22)], writes=[('bko', bk)])
                for hf in range(2):
                    oc = pr * 2 + hf
                    po = ps_o[bk][:, hf * TT:(hf + 1) * TT]
                    P.op('dve', lambda E, oc=oc, po=po: E.scalar_tensor_tensor(xh[s][:, oc, :], po, 0.5, xh[s][:, oc, :], op0=ALU.mult, op1=ALU.add),
                         reads=[('bko', bk), ('xh', s, oc)], writes=[('xh', s, oc)])

        def ple(i):
            s = i % 3
            d = i % 2
            for oc in range(8):
                b = oc % 2
                pg = ps_g[b]
                mm_group(P, pg[:, 0:TT], [(wpg[:, k, oc * 128:(oc + 1) * 128], un[d][:, k, :]) for k in range(8)],
                         reads=[('wpg', k, oc // 4) for k in range(8)] + [(unk, d, k) for k in range(8)], writes=[('bkg', b)])
                mm_group(P, pg[:, TT:2 * TT], [(wpe[:, k, oc * 128:(oc + 1) * 128], pt[d][:, k, :]) for k in range(2)],
                         reads=[('wpe', k, oc // 4) for k in range(2)] + [('pt', d)], writes=[('bkg', b)])
                P.op('act', lambda E, b=b, pg=pg: E.activation(out=sg[0][:], in_=pg[:, 0:TT], func=AF.Sigmoid),
                     reads=[('bkg', b)], writes=[('sg', 0)])
                P.op('dve', lambda E, b=b, pg=pg: E.tensor_tensor(sg[0][:], sg[0][:], pg[:, TT:2 * TT], op=ALU.mult),
                     reads=[('sg', 0), ('bkg', b)], writes=[('sg', 0)])
                P.op('dve', lambda E, b=b, oc=oc: E.tensor_tensor(xh[s][:, oc, :], sg[0][:], xh[s][:, oc, :], op=ALU.add),
                     reads=[('sg', 0), ('xh', s, oc)], writes=[('xh', s, oc)])
            P.op('sp', lambda E: E.dma_start(out=T['outT'].rearrange("(c p) n -> p c n", p=128)[:, :, i * TT:(i + 1) * TT], in_=xh[s][:]),
                 reads=[('xh', s, oc) for oc in range(8)], dsem="p2_ot%d" % s)

        KSTOP = os.environ.get('KSTOP', '')
        if KSTOP == 'w':
            P.barrier(); return
        load_x(0)
        load_x(1)
        load_p(0)
        if KSTOP == 'x':
            P.barrier(); return
        norm(0, g1, 'g1', xn, 'xn')
        if KSTOP == 'n':
            P.barrier(); return
        if KSTOP == 'in':
            ffn_in(0); P.barrier(); return
        for i in range(ntiles):
            if i + 2 < ntiles:
                load_x(i + 2)
            if i + 1 < ntiles:
                load_p(i + 1)
            ffn_in(i)
            if i + 1 < ntiles:
                norm(i + 1, g1, 'g1', xn, 'xn')
            ffn_out(i)
            s = i % 3
            if first and i >= own0:
                io = i - own0
                P.op('sp', lambda E: E.dma_start(out=T['h1'].rearrange("(c p) n -> p c n", p=128)[:, :, io * TT:(io + 1) * TT], in_=xh[s][:]),
                     reads=[('xh', s, c) for c in range(8)], dsem="p1_h%d" % s)
            norm(i, g2, 'g2', un, unk)
            if first:
                d = i % 2
                P.op('sp', lambda E: E.dma_start(out=T['u'].rearrange("(c p) n -> p c n", p=128)[:, :, i * TT:(i + 1) * TT], in_=un[d][:]),
                     reads=[('un', d, c) for c in range(8)], dsem="p1_u%d" % d)
            else:
                ple(i)
        P.barrier()


def merge_phase(P, nc, T):
    TT = 512
    ntiles = int(os.environ.get('KNT3', OWN // TT))
    with ExitStack() as ph:
        def sb(name, shape, dt):
            return ph.enter_context(nc.sbuf_tensor("p3_" + name, shape, dt))

        def pst(name, shape, dt=F32):
            return ph.enter_context(nc.psum_tensor("p3_" + name, shape, dt))
        wg = sb("wg", [128, 8, 2048], BF16)
        wua = sb("wua", [128, 4, D], BF16)
        wub = sb("wub", [128, 4, D], BF16)
        wo = sb("wo", [128, 8, D], BF16)
        stage = [sb("stg%d" % i, [128, 512], F32) for i in range(3)]
        ut = [sb("ut%d" % i, [128, 8, TT], BF16) for i in range(2)]
        at = [sb("at%d" % i, [128, 4, TT], BF16) for i in range(2)]
        mt = [sb("mt%d" % i, [128, 4, TT], BF16) for i in range(2)]
        ht = [sb("ht%d" % i, [128, 8, TT], F32) for i in range(2)]
        mg = sb("mg", [128, 8, TT], BF16)
        sg = [sb("sg%d" % i, [128, TT], F32) for i in range(2)]
        m1 = [sb("m1%d" % i, [128, TT], F32) for i in range(2)]
        psg = [pst("g%d" % i, [128, 512]) for i in range(2)]
        psy = [pst("y%d" % i, [128, 512]) for i in range(2)]
        pso = [pst("o%d" % i, [128, 512]) for i in range(2)]
        w_in_r = T['w_in'].rearrange("(c p) n -> p c n", p=128)
        pieces = []
        for k in range(8):
            for q in range(4):
                pieces.append((wg[:, k, q * 512:(q + 1) * 512], w_in_r[:, k, C_GA + q * 512:C_GA + (q + 1) * 512], ('wg', k, q)))
        for nm, wt, src in (('wua', wua, T['w_up_att']), ('wub', wub, T['w_up_mlstm'])):
            r = src.rearrange("(c p) n -> p c n", p=128)
            for k in range(4):
                for q in range(2):
                    pieces.append((wt[:, k, q * 512:(q + 1) * 512], r[:, k, q * 512:(q + 1) * 512], (nm, k, q)))
        r = T['w_out'].rearrange("(c p) n -> p c n", p=128)
        for k in range(8):
            for q in range(2):
                pieces.append((wo[:, k, q * 512:(q + 1) * 512], r[:, k, q * 512:(q + 1) * 512], ('wo', k, q)))
        load_cast(P, stage, pieces)
        u_r = T['u'].rearrange("(c p) n -> p c n", p=128)
        a_r = T['att'].rearrange("(c p) n -> p c n", p=128)
        m_r = T['hg'].rearrange("(c p) n -> p c n", p=128)
        h_r = T['h1'].rearrange("(c p) n -> p c n", p=128)
        o_r = T['h2'].rearrange("(c p) n -> p c n", p=128)

        def loads(i):
            d = i % 2
            P.op('sp', lambda E: E.dma_start(out=ut[d][:], in_=u_r[:, :, OWN + i * TT:OWN + (i + 1) * TT]), writes=[('ut', d)], dsem="p3_ut%d" % d)
            P.op('sp', lambda E: E.dma_start(out=at[d][:], in_=a_r[:, :, i * TT:(i + 1) * TT]), writes=[('at', d)], dsem="p3_at%d" % d)
            P.op('sp', lambda E: E.dma_start(out=mt[d][:], in_=m_r[:, :, i * TT:(i + 1) * TT]), writes=[('mt', d)], dsem="p3_mt%d" % d)
            P.op('sp', lambda E: E.dma_start(out=ht[d][:], in_=h_r[:, :, i * TT:(i + 1) * TT]), writes=[('ht', d, c) for c in range(8)], dsem="p3_ht%d" % d)
        loads(0)
        for i in range(ntiles):
            d = i % 2
            if i + 1 < ntiles:
                loads(i + 1)
            for oc in range(8):
                for br in range(2):
                    b = br
                    wy, ykey, yt, ytk = (wua, 'wua', at, 'at') if br == 0 else (wub, 'wub', mt, 'mt')
                    mm_group(P, psg[b][:], [(wg[:, k, br * 1024 + oc * 128:br * 1024 + (oc + 1) * 128], ut[d][:, k, :]) for k in range(8)],
                             reads=[('wg', k, (br * 1024 + oc * 128) // 512) for k in range(8)] + [('ut', d)], writes=[('bkg', b)])
                    mm_group(P, psy[b][:], [(wy[:, k, oc * 128:(oc + 1) * 128], yt[d][:, k, :]) for k in range(4)],
                             reads=[(ykey, k, oc // 4) for k in range(4)] + [(ytk, d)], writes=[('bky', b)])
                    P.op('act', lambda E: E.activation(out=sg[b][:], in_=psg[b][:], func=AF.Sigmoid), reads=[('bkg', b)], writes=[('sg', b)])
                    P.op('dve', lambda E: E.tensor_tensor(m1[b][:], sg[b][:], psy[b][:], op=ALU.mult), reads=[('sg', b), ('bky', b)], writes=[('m1', b)])
                P.op('pool', lambda E: E.tensor_tensor(mg[:, oc, :], m1[0][:], m1[1][:], op=ALU.add), reads=[('m1', 0), ('m1', 1)], writes=[('mg', oc)])
            for oc in range(8):
                b = oc % 2
                mm_group(P, pso[b][:], [(wo[:, k, oc * 128:(oc + 1) * 128], mg[:, k, :]) for k in range(8)],
                         reads=[('wo', k, oc // 4) for k in range(8)] + [('mg', k) for k in range(8)], writes=[('bko', b)])
                P.op('dve', lambda E: E.tensor_tensor(ht[d][:, oc, :], pso[b][:], ht[d][:, oc, :], op=ALU.add),
                     reads=[('bko', b), ('ht', d, oc)], writes=[('ht', d, oc)])
            P.op('sp', lambda E: E.dma_start(out=o_r[:, :, i * TT:(i + 1) * TT], in_=ht[d][:]), reads=[('ht', d, c) for c in range(8)], dsem="p3_o%d" % d)
        P.barrier()


def mlstm_phase(P, nc, T):
    TT = 512
    ntiles = NTOK // TT
    own0 = (NTOK - OWN) // TT
    t_start = int(os.environ.get('KT2A0', 0))
    t_end = int(os.environ.get('KT2A1', ntiles))
    SC = 128 ** -0.5
    with ExitStack() as ph:
        def sb(name, shape, dt):
            return ph.enter_context(nc.sbuf_tensor("p2a_" + name, shape, dt))

        def pst(name, shape, dt=F32):
            return ph.enter_context(nc.psum_tensor("p2a_" + name, shape, dt))
        wm = sb("wm", [128, 8, 2048], BF16)
        wgt = sb("wgt", [128, 8, 8], BF16)
        wgf = sb("wgf", [128, 8, 8], F32)
        stage = [sb("stg%d" % i, [128, 512], F32) for i in range(3)]
        ident_f = sb("identf", [128, 128], F32)
        tri_f = sb("trif", [128, 128], F32)
        ident_bf = sb("identb", [128, 128], BF16)
        mask_bf = sb("maskb", [128, 128], BF16)
        ones_bf = sb("onesb", [128, 128], BF16)
        ones4 = sb("ones4", [4, 128], F32)
        selc = sb("selc", [4, 4, 128], F32)
        flag = sb("flag", [128, 1], F32)
        cw = sb("cw", [128, 8, 4], F32)
        cb = sb("cb", [128, 8], F32)
        ibias = sb("ibias", [4, 1], F32)
        fbias = sb("fbias", [128, 4], F32)
        Cst = sb("Cst", [128, 4, 129], F32)
        mst = sb("mst", [4, 1], F32)
        ut = [sb("ut%d" % i, [128, 8, TT], BF16) for i in range(2)]
        pre = sb("pre", [128, 8, TT + 3], F32)
        cv = sb("cv", [128, 8, TT], F32)
        qT = sb("qT", [128, 4, TT], BF16)
        kT = sb("kT", [128, 4, TT], BF16)
        so = sb("so", [128, 4, TT], F32)
        hgt = [sb("hgt%d" % i, [128, 4, TT], BF16) for i in range(2)]
        vt = sb("vt", [128, 4, 129], BF16)
        kw = sb("kw", [128, 4, 128], BF16)
        pT = sb("pT", [128, 4, 128], BF16)
        Cb = sb("Cb", [128, 4, 128], BF16)
        nbc = sb("nbc", [128, 4, 128], BF16)
        thrS = sb("thrS", [128, 512], F32)
        dd = sb("dd", [128, 512], F32)
        rr = sb("rr", [128, 512], F32)
        hh = sb("hh", [128, 512], F32)
        fx = sb("fx", [128, 4], F32)
        lt = sb("lt", [128, 4], F32)
        nbr = sb("nbr", [4, 128], F32)
        gp = sb("gp", [4, 128], F32)
        wrow = sb("wrow", [4, 128], F32)
        trow = sb("trow", [4, 128], F32)
        gmax = sb("gmax", [4, 1], F32)
        Mv = sb("Mv", [4, 1], F32)
        negM = sb("negM", [4, 1], F32)
        av = sb("av", [4, 1], F32)
        da = sb("da", [4, 4], F32)
        wtok = sb("wtok", [128, 4], F32)
        abc = sb("abc", [128, 4], F32)
        B0 = pst("b0", [128, 512])
        B1 = pst("b1", [128, 512])
        B2 = pst("b2", [128, 512])
        B3 = pst("b3", [128, 512])
        B4 = pst("b4", [128, 1024], BF16)
        B5 = pst("b5", [128, 512])
        B6 = pst("b6", [128, 512])
        B7 = pst("b7", [128, 512])

        P.op('sp', lambda E: E.dma_start(out=ident_f[:], in_=T['c_ident'][:, :]), writes=['identf'], dsem="p2a_c0")
        P.op('sp', lambda E: E.dma_start(out=tri_f[:], in_=T['c_tri'][:, :]), writes=['trif'], dsem="p2a_c1")
        P.op('sp', lambda E: E.dma_start(out=flag[:], in_=T['c_flag'][:, :]), writes=['flag'], dsem="p2a_c2")
        P.op('sp', lambda E: E.dma_start(out=cw[:], in_=T['conv_w'][:, :, :]), writes=['cw'], dsem="p2a_c3")
        P.op('sp', lambda E: E.dma_start(out=cb[:], in_=T['conv_b'][:, :]), writes=['cb'], dsem="p2a_c4")
        P.op('sp', lambda E: E.dma_start(out=ibias[:], in_=T['i_bias'][:, :]), writes=['ibias'], dsem="p2a_c5")
        P.op('sp', lambda E: E.dma_start(out=fbias[:], in_=T['f_bias'][:, :]), writes=['fbias'], dsem="p2a_c6")
        w_in_r = T['w_in'].rearrange("(c p) n -> p c n", p=128)
        P.op('sp', lambda E: E.dma_start(out=wgf[:], in_=w_in_r[:, :, C_MI:C_MI + 8]), writes=['wgf'], dsem="p2a_c7")
        P.op('pool', lambda E: E.tensor_copy(wgt[:], wgf[:]), reads=['wgf'], writes=['wgt'])
        P.op('pool', lambda E: E.tensor_copy(ident_bf[:], ident_f[:]), reads=['identf'], writes=['identb'])
        P.op('pool', lambda E: E.tensor_copy(mask_bf[:], tri_f[:]), reads=['trif'], writes=['maskb'])
        P.op('pool', lambda E: E.memset(ones_bf[:], 1.0), writes=['onesb'])
        P.op('pool', lambda E: E.memset(ones4[:], 1.0), writes=['ones4'])
        P.op('pool', lambda E: E.memset(Cst[:], 0.0), writes=['Cst'])
        P.op('pool', lambda E: E.memset(mst[:], 0.0), writes=['mst'])
        P.op('pool', lambda E: E.memset(vt[:], 1.0), writes=['vt'])
        P.op('pool', lambda E: E.memset(pre[:], 0.0), writes=[('pre', c) for c in range(8)])
        for h in range(4):
            P.op('pool', lambda E, h=h: E.tensor_scalar(selc[:, h, :], ones4[:], ident_f[0:4, h:h + 1], None, op0=ALU.mult),
                 reads=['ones4', 'identf'], writes=['selc'])
        pieces = []
        for k in range(8):
            for q in range(4):
                pieces.append((wm[:, k, q * 512:(q + 1) * 512], w_in_r[:, k, C_MQ + q * 512:C_MQ + (q + 1) * 512], ('wm', k, q)))
        load_cast(P, stage, pieces)
        u_r = T['u'].rearrange("(c p) n -> p c n", p=128)
        hg_r = T['hg'].rearrange("(c p) n -> p c n", p=128)

        def load_u(i):
            d = i % 2
            P.op('sp', lambda E: E.dma_start(out=ut[d][:], in_=u_r[:, :, i * TT:(i + 1) * TT]), writes=[('ut', d)], dsem="p2a_ut%d" % d)

        load_u(t_start)
        for i in range(t_start, t_end):
            d = i % 2
            own = i >= own0
            if i + 1 < t_end:
                load_u(i + 1)
            fcs = ([(0, fc) for fc in range(4)] if (own or i == own0 - 1) else []) + [(1, fc) for fc in range(4)]
            for (kind, fc) in fcs:
                c8 = kind * 4 + fc
                mm_group(P, B0[:], [(wm[:, k, c8 * 128:(c8 + 1) * 128], ut[d][:, k, :]) for k in range(8)],
                         reads=[('wm', k, kind) for k in range(8)] + [('ut', d)], writes=['B0'])
                P.op('act', lambda E: E.copy(out=pre[:, c8, 3:TT + 3], in_=B0[:]), reads=['B0'], writes=[('pre', c8)])
            if own:
                for fc in range(4):
                    mm_group(P, B0[:], [(wm[:, k, 1536 + fc * 128:1536 + (fc + 1) * 128], ut[d][:, k, :]) for k in range(8)],
                             reads=[('wm', k, 3) for k in range(8)] + [('ut', d)], writes=['B0'])
                    P.op('act', lambda E: E.activation(out=so[:, fc, :], in_=B0[:], func=AF.Sigmoid), reads=['B0'], writes=[('so', fc)])
            for (kind, fc) in fcs:
                c8 = kind * 4 + fc
                eng = 'dve'
                P.op(eng, lambda E: E.tensor_scalar(cv[:, c8, :], pre[:, c8, 0:TT], cw[:, c8, 0:1], cb[:, c8:c8 + 1], op0=ALU.mult, op1=ALU.add),
                     reads=[('pre', c8), 'cw', 'cb'], writes=[('cv', c8)])
                for j in range(1, 4):
                    P.op(eng, lambda E: E.scalar_tensor_tensor(cv[:, c8, :], pre[:, c8, j:TT + j], cw[:, c8, j:j + 1], cv[:, c8, :], op0=ALU.mult, op1=ALU.add),
                         reads=[('pre', c8), ('cv', c8), 'cw'], writes=[('cv', c8)])
                P.op(eng, lambda E: E.tensor_copy(pre[:, c8, 0:3], pre[:, c8, TT:TT + 3]), reads=[('pre', c8)], writes=[('pre', c8)])
                if kind == 0:
                    P.op('act', lambda E: E.activation(out=cv[:, c8, :], in_=cv[:, c8, :], func=AF.Silu), reads=[('cv', c8)], writes=[('cv', c8)])
                    P.op('pool', lambda E: E.tensor_scalar(qT[:, fc, :], cv[:, c8, :], SC, None, op0=ALU.mult), reads=[('cv', c8)], writes=[('qT', fc)])
                else:
                    P.op('act', lambda E: E.activation(out=kT[:, fc, :], in_=cv[:, c8, :], func=AF.Silu), reads=[('cv', c8)], writes=[('kT', fc)])
            for ci in range(4):
                c0 = ci * 128
                cs = slice(c0, c0 + 128)
                mm_group(P, B1[:], [(ut[d][:, k, cs], wm[:, k, 1024:1536]) for k in range(8)],
                         reads=[('wm', k, 2) for k in range(8)] + [('ut', d)], writes=['B1'])
                mm_group(P, B2[:, 0:8], [(ut[d][:, k, cs], wgt[:, k, 0:8]) for k in range(8)], reads=['wgt', ('ut', d)], writes=['B2'])
                mm_group(P, B2[0:4, 8:136], [(wgt[:, k, 0:4], ut[d][:, k, cs]) for k in range(8)], reads=['wgt', ('ut', d)], writes=['B2'])
                P.op('act', lambda E: E.copy(out=vt[:, :, 0:128], in_=B1[:].rearrange("p (h e) -> p h e", h=4)), reads=['B1'], writes=['vt'])
                P.op('dve', lambda E: E.tensor_tensor(fx[:], B2[:, 4:8], fbias[:], op=ALU.add), reads=['B2', 'fbias'], writes=['fx'])
                P.op('act', lambda E: E.activation(out=fx[:], in_=fx[:], func=AF.Exp, scale=-1.0), reads=['fx'], writes=['fx'])
                P.op('act', lambda E: E.activation(out=lt[:], in_=fx[:], func=AF.Ln, bias=1.0, scale=1.0), reads=['fx'], writes=['lt'])
                mm_group(P, B3[0:4, 0:128], [(lt[:], tri_f[:])], reads=['lt', 'trif'], writes=['B3'])
                P.op('act', lambda E: E.copy(out=nbr[:], in_=B3[0:4, 0:128]), reads=['B3'], writes=['nbr'])
                P.op('dve', lambda E: E.scalar_tensor_tensor(gp[:], B2[0:4, 8:136], ibias[:, 0:1], nbr[:], op0=ALU.add, op1=ALU.add),
                     reads=['B2', 'ibias', 'nbr'], writes=['gp'])
                P.op('dve', lambda E: E.reduce_max(gmax[:], gp[:], axis=AX.X), reads=['gp'], writes=['gmax'])
                P.op('dve', lambda E: E.tensor_tensor(Mv[:], gmax[:], mst[:], op=ALU.max), reads=['gmax', 'mst'], writes=['Mv'])
                P.op('dve', lambda E: E.tensor_scalar(negM[:], Mv[:], -1.0, None, op0=ALU.mult), reads=['Mv'], writes=['negM'])
                P.op('act', lambda E: E.activation(out=wrow[:], in_=gp[:], func=AF.Exp, bias=negM[:, 0:1], scale=1.0), reads=['gp', 'negM'], writes=['wrow'])
                P.op('act', lambda E: E.activation(out=av[:], in_=mst[:], func=AF.Exp, bias=negM[:, 0:1], scale=1.0), reads=['mst', 'negM'], writes=['av'])
                if own:
                    P.op('act', lambda E: E.activation(out=trow[:], in_=nbr[:], func=AF.Exp, bias=negM[:, 0:1], scale=1.0), reads=['nbr', 'negM'], writes=['trow'])
                P.op('dve', lambda E: E.tensor_tensor(mst[:], Mv[:], nbr[:, 127:128], op=ALU.subtract), reads=['Mv', 'nbr'], writes=['mst'])
                mm_group(P, B3[:, 128:132], [(wrow[:], ident_f[0:4, 0:4])], reads=['wrow', 'identf'], writes=['B3'])
                P.op('dve', lambda E: E.tensor_scalar(da[:], ident_f[0:4, 0:4], av[:, 0:1], None, op0=ALU.mult), reads=['av', 'identf'], writes=['da'])
                mm_group(P, B3[:, 136:140], [(ones4[:], da[:])], reads=['ones4', 'da'], writes=['B3'])
                if own:
                    P.op('dve', lambda E: E.tensor_copy(wtok[:], B3[:, 128:132]), reads=['B3'], writes=['wtok'])
                else:
                    P.op('dve', lambda E: E.tensor_scalar(wtok[:], B3[:, 128:132], flag[:, 0:1], None, op0=ALU.mult), reads=['B3', 'flag'], writes=['wtok'])
                P.op('dve', lambda E: E.tensor_copy(abc[:], B3[:, 136:140]), reads=['B3'], writes=['abc'])
                for h in range(4):
                    P.op('pool', lambda E, h=h: E.tensor_scalar(Cst[:, h, :], Cst[:, h, :], abc[:, h:h + 1], None, op0=ALU.mult), reads=['Cst', 'abc'], writes=['Cst'])
                def trf(E):
                    ins = None
                    for h in range(4):
                        ins = E.transpose(B4[:, h * 128:(h + 1) * 128], kT[:, h, cs], ident_bf[:])
                    return ins
                P.op('pe', trf, reads=[('kT', h) for h in range(4)] + ['identb'], writes=['B4'])
                for h in range(4):
                    P.op('dve', lambda E, h=h: E.tensor_scalar(kw[:, h, :], B4[:, h * 128:(h + 1) * 128], wtok[:, h:h + 1], None, op0=ALU.mult),
                         reads=['B4', 'wtok'], writes=['kw'])
                if own:
                    P.op('act', lambda E: E.copy(out=Cb[:], in_=Cst[:, :, 0:128]), reads=['Cst'], writes=['Cb'])
                    for h in range(4):
                        P.op('pool', lambda E, h=h: E.tensor_scalar(nbc[:, h, :], ones_bf[:], Cst[:, h, 128:129], None, op0=ALU.mult), reads=['Cst', 'onesb'], writes=['nbc'])
                    def qkf(E):
                        ins = None
                        for h in range(4):
                            ins = E.matmul(B5[:, h * 128:(h + 1) * 128], kT[:, h, cs], qT[:, h, cs], start=True, stop=True)
                        return ins
                    P.op('pe', qkf, reads=[('kT', h) for h in range(4)] + [('qT', h) for h in range(4)], writes=['B5'])
                    for h in range(4):
                        P.op('dve', lambda E, h=h: E.scalar_tensor_tensor(pT[:, h, :], B5[:, h * 128:(h + 1) * 128], wtok[:, h:h + 1], mask_bf[:], op0=ALU.mult, op1=ALU.mult),
                             reads=['B5', 'wtok', 'maskb'], writes=['pT'])
                    def numf(E):
                        ins = None
                        for h in range(4):
                            E.matmul(B6[:, h * 128:(h + 1) * 128], Cb[:, h, :], qT[:, h, cs], start=True, stop=False)
                            ins = E.matmul(B6[:, h * 128:(h + 1) * 128], vt[:, h, 0:128], pT[:, h, :], start=False, stop=True)
                        return ins
                    P.op('pe', numf, reads=['Cb', 'vt', 'pT'] + [('qT', h) for h in range(4)], writes=['B6'])

                    def denf(E):
                        ins = None
                        for h in range(4):
                            E.matmul(B1[:, h * 128:(h + 1) * 128], nbc[:, h, :], qT[:, h, cs], start=True, stop=False)
                            ins = E.matmul(B1[:, h * 128:(h + 1) * 128], ones_bf[:], pT[:, h, :], start=False, stop=True)
                        return ins
                    P.op('pe', denf, reads=['nbc', 'onesb', 'pT'] + [('qT', h) for h in range(4)], writes=['B1'])

                    def thrf(E):
                        ins = None
                        for h in range(4):
                            ins = E.matmul(B0[:, h * 128:(h + 1) * 128], selc[:, h, :], trow[:], start=True, stop=True)
                        return ins
                    P.op('pe', thrf, reads=['selc', 'trow'], writes=['B0'])
                    P.op('act', lambda E: E.copy(out=thrS[:], in_=B0[:]), reads=['B0'], writes=['thrS'])
                    P.op('dve', lambda E: E.tensor_tensor(dd[:], B1[:], thrS[:], op=ALU.max), reads=['B1', 'thrS'], writes=['dd'])
                    P.op('dve', lambda E: E.scalar_tensor_tensor(dd[:], B1[:], -1.0, dd[:], op0=ALU.mult, op1=ALU.max), reads=['B1', 'dd'], writes=['dd'])
                    P.op('dve', lambda E: E.reciprocal(rr[:], dd[:]), reads=['dd'], writes=['rr'])
                    P.op('dve', lambda E: E.tensor_tensor(hh[:], B6[:], rr[:], op=ALU.mult), reads=['B6', 'rr'], writes=['hh'])
                    P.op('pool', lambda E: E.tensor_tensor(hgt[d][:, :, cs], hh[:].rearrange("p (h t) -> p h t", h=4), so[:, :, cs], op=ALU.mult),
                         reads=['hh'] + [('so', fc) for fc in range(4)], writes=[('hgt', d)])
                def dcf(E):
                    ins = None
                    for h in range(4):
                        bank = B5 if h < 2 else B7
                        ins = E.matmul(bank[:, (h % 2) * 129:(h % 2 + 1) * 129], kw[:, h, :], vt[:, h, :], start=True, stop=True)
                    return ins
                P.op('pe', dcf, reads=['kw', 'vt'], writes=['B5', 'B7'])
                P.op('dve', lambda E: E.tensor_tensor(Cst[:, 0:2, :], Cst[:, 0:2, :], B5[:, 0:258].rearrange("p (h e) -> p h e", h=2), op=ALU.add), reads=['Cst', 'B5'], writes=['Cst'])
                P.op('dve', lambda E: E.tensor_tensor(Cst[:, 2:4, :], Cst[:, 2:4, :], B7[:, 0:258].rearrange("p (h e) -> p h e", h=2), op=ALU.add), reads=['Cst', 'B7'], writes=['Cst'])
            if own:
                io = i - own0
                P.op('sp', lambda E: E.dma_start(out=hg_r[:, :, io * TT:(io + 1) * TT], in_=hgt[d][:]), reads=[('hgt', d)], dsem="p2a_hg%d" % d)
        P.barrier()


def attn_phase(P, nc, T):
    TT = 512
    L = 6144
    SCALE = 128 ** -0.5
    slots = int(os.environ.get('KSLOTS', 4))
    with ExitStack() as ph:
        def sb(name, shape, dt):
            return ph.enter_context(nc.sbuf_tensor("p2b_" + name, shape, dt))

        def pst(name, shape, dt=F32):
            return ph.enter_context(nc.psum_tensor("p2b_" + name, shape, dt))
        ures = sb("ures", [128, 8, L], BF16)
        wq = sb("wq", [128, 8, 128], BF16)
        wk = sb("wk", [128, 8, 128], BF16)
        wv = sb("wv", [128, 8, 128], BF16)
        stage = [sb("stg%d" % i, [128, 128], F32) for i in range(4)]
        qT = sb("qT", [128, OWN], BF16)
        kT = sb("kT", [128, L], BF16)
        vt = sb("vt", [128, 48, 128], BF16)
        acc = sb("acc", [128, 2, OWN], F32)
        ones_bf = sb("onesb", [128, 128], BF16)
        fones = sb("fones", [128, 128], BF16)
        mask2 = sb("mask2", [128, 512], BF16)
        trif = sb("trif", [128, 128], F32)
        tritf = sb("tritf", [128, 128], F32)
        flag = sb("flag", [128, 1], F32)
        gq = sb("gq", [128, 1], F32)
        gk = sb("gk", [128, 1], F32)
        rm = sb("rm", [32, 32], F32)
        sq = sb("sq", [128, TT], BF16)
        tmp = sb("tmp", [128, TT], F32)
        rstd = sb("rstd", [128, TT], F32)
        qn = sb("qn", [128, TT], F32)
        t1 = sb("t1", [32, TT], F32)
        t2 = sb("t2", [32, TT], F32)
        cosb = [sb("cos%d" % i, [32, TT], F32) for i in range(2)]
        sinb = [sb("sin%d" % i, [32, TT], F32) for i in range(2)]
        pT = [sb("pT%d" % i, [128, 512], BF16) for i in range(2)]
        atto = [sb("atto%d" % i, [128, TT], BF16) for i in range(2)]
        rden = sb("rden", [128, TT], F32)
        BQ = pst("bq", [128, 512])
        BS = pst("bs", [128, 512])
        BR = pst("br", [128, 512])
        BV = pst("bv", [128, 512])
        BSC = [pst("bsc%d" % i, [128, 512]) for i in range(2)]
        BN = [pst("bn%d" % i, [128, 512]) for i in range(2)]

        P.op('sp', lambda E: E.dma_start(out=trif[:], in_=T['c_tri'][:, :]), writes=['trif'], dsem="p2b_c0")
        P.op('sp', lambda E: E.dma_start(out=tritf[:], in_=T['c_trit'][:, :]), writes=['tritf'], dsem="p2b_c1")
        P.op('sp', lambda E: E.dma_start(out=flag[:], in_=T['c_flag'][:, :]), writes=['flag'], dsem="p2b_c2")
        P.op('sp', lambda E: E.dma_start(out=gq[:], in_=T['q_gain'][:, :]), writes=['gq'], dsem="p2b_c3")
        P.op('sp', lambda E: E.dma_start(out=gk[:], in_=T['k_gain'][:, :]), writes=['gk'], dsem="p2b_c4")
        P.op('sp', lambda E: E.dma_start(out=rm[:], in_=T['c_rm'][:, :]), writes=['rm'], dsem="p2b_c5")
        P.op('pool', lambda E: E.memset(ones_bf[:], 1.0), writes=['onesb'])
        P.op('pool', lambda E: E.tensor_scalar(fones[:], ones_bf[:], flag[:, 0:1], None, op0=ALU.mult), reads=['onesb', 'flag'], writes=['fones'])
        for qb in range(2):
            P.op('pool', lambda E, qb=qb: E.tensor_copy(mask2[:, (qb * 2) * 128:(qb * 2 + 1) * 128], tritf[:]), reads=['tritf'], writes=['mask2'])
            P.op('pool', lambda E, qb=qb: E.tensor_copy(mask2[:, (qb * 2 + 1) * 128:(qb * 2 + 2) * 128], trif[:]), reads=['trif'], writes=['mask2'])
        u_r = T['u'].rearrange("(c p) n -> p c n", p=128)
        for tl in range(12):
            P.op('sp', lambda E, tl=tl: E.dma_start(out=ures[:, :, tl * TT:(tl + 1) * TT], in_=u_r[:, :, 2048 + tl * TT:2048 + (tl + 1) * TT]),
                 writes=[('ures', tl)], dsem="p2b_u%d" % tl)
            if tl % 4 == 3:
                pass
        ures_all = [('ures', tl) for tl in range(12)]
        w_in_r = T['w_in'].rearrange("(c p) n -> p c n", p=128)
        att_r = T['att'].rearrange("(c p) n -> p c n", p=128)
        cnt = [0]

        def prep(kind, tl):
            l0 = tl * TT
            w, wkey, gain, gkey = (wq, 'wq', gq, 'gq') if kind == 'q' else (wk, 'wk', gk, 'gk')
            dst = qT[:, l0 - 2048:l0 - 2048 + TT] if kind == 'q' else kT[:, l0:l0 + TT]
            dkey = (kind + 'T', tl)
            cs = cnt[0] % 2
            cnt[0] += 1
            P.op('sp', lambda E: E.dma_start(out=cosb[cs][:], in_=T['c_cos'][:, l0:l0 + TT]), writes=[('cos', cs)], dsem="p2b_cos%d" % cs)
            P.op('sp', lambda E: E.dma_start(out=sinb[cs][:], in_=T['c_sin'][:, l0:l0 + TT]), writes=[('sin', cs)], dsem="p2b_sin%d" % cs)
            mm_group(P, BQ[:], [(w[:, k, :], ures[:, k, l0:l0 + TT]) for k in range(8)], reads=[wkey, ('ures', tl)], writes=['BQ'])
            P.op('act', lambda E: E.activation(out=sq[:], in_=BQ[:], func=AF.Square), reads=['BQ'], writes=['sq'])
            mm_group(P, BS[:], [(ones_bf[:], sq[:])], reads=['onesb', 'sq'], writes=['BS'])
            P.op('dve', lambda E: E.tensor_scalar(tmp[:], BS[:], 1.0 / 128, EPS, op0=ALU.mult, op1=ALU.add), reads=['BS'], writes=['tmp'])
            P.op('act', lambda E: E.activation(out=tmp[:], in_=tmp[:], func=AF.Sqrt), reads=['tmp'], writes=['tmp'])
            P.op('dve', lambda E: E.reciprocal(rstd[:], tmp[:]), reads=['tmp'], writes=['rstd'])
            P.op('dve', lambda E: E.scalar_tensor_tensor(qn[:], BQ[:], gain[:, 0:1], rstd[:], op0=ALU.mult, op1=ALU.mult), reads=['BQ', gkey, 'rstd'], writes=['qn'])
            mm_group(P, BR[0:32, :], [(rm[:], qn[0:32, :])], reads=['rm', 'qn'], writes=['BR'])
            P.op('pool', lambda E: E.tensor_tensor(t1[:], qn[0:32, :], cosb[cs][:], op=ALU.mult), reads=['qn', ('cos', cs)], writes=['t1'])
            P.op('dve', lambda E: E.tensor_tensor(t2[:], BR[0:32, :], sinb[cs][:], op=ALU.mult), reads=['BR', ('sin', cs)], writes=['t2'])
            P.op('pool', lambda E: E.tensor_tensor(dst[0:32, :], t1[:], t2[:], op=ALU.add), reads=['t1', 't2'], writes=[dkey])
            P.op('act', lambda E: E.copy(out=dst[32:64, :], in_=qn[32:64, :]), reads=['qn'], writes=[(kind + 'Tc', tl)])
            P.op('act', lambda E: E.copy(out=dst[64:128, :], in_=qn[64:128, :]), reads=['qn'], writes=[(kind + 'Tb', tl)])

        for s in range(slots):
            for g in range(3):
                d = DILS[g]
                J = OWN // (128 * d)
                hk = g * 4 + s
                pieces = []
                for (wt, nm, c0) in ((wq, 'wq', C_AQ), (wk, 'wk', C_AK), (wv, 'wv', C_AV)):
                    for k in range(8):
                        pieces.append((wt[:, k, :], w_in_r[:, k, c0 + hk * 128:c0 + (hk + 1) * 128], nm))
                load_cast(P, stage, pieces, engs=('pool',))
                ktl0 = 0 if d == 16 else 3
                ktiles = list(range(ktl0, 12))
                for tl in ktiles:
                    prep('k', tl)
                for tl in range(4, 12):
                    prep('q', tl)
                kkeys = [('kT', tl) for tl in ktiles] + [('kTb', tl) for tl in ktiles] + [('kTc', tl) for tl in ktiles]
                qkeys = [('qT', tl) for tl in range(4, 12)] + [('qTb', tl) for tl in range(4, 12)] + [('qTc', tl) for tl in range(4, 12)]
                nblk = d * (J + 1)
                blks = [(r, j) for r in range(d) for j in range(-1, J)]
                for b0 in range(0, nblk, 4):
                    grp = blks[b0:b0 + 4]

                    def vf(E, grp=grp):
                        ins = None
                        for qi, (r, j) in enumerate(grp):
                            u0 = 2048 // d + 128 * j
                            for k in range(8):
                                lhs = ures[:, k, :].rearrange("p (u d) -> p d u", d=d)[:, r, u0:u0 + 128]
                                ins = E.matmul(BV[:, qi * 128:(qi + 1) * 128], lhs, wv[:, k, :], start=(k == 0), stop=(k == 7))
                        return ins
                    P.op('pe', vf, reads=['wv'] + ures_all, writes=['BV'])
                    n = len(grp)
                    P.op('act', lambda E, b0=b0, n=n: E.copy(out=vt[:, b0:b0 + n, :], in_=BV[:, 0:n * 128].rearrange("p (b e) -> p b e", e=128)),
                         reads=['BV'], writes=['vt'])
                kview = kT[:, :].rearrange("p (u d) -> p d u", d=d)
                qview = qT[:, :].rearrange("p (u d) -> p d u", d=d)
                accv = acc[:, :, :].rearrange("p n (u d) -> p n d u", d=d)
                it = 0
                for r in range(d):
                    for jp in range(J // 2):
                        j0 = 2 * jp
                        b = it % 2
                        it += 1

                        def sf(E, r=r, j0=j0, b=b):
                            ins = None
                            for qb in range(2):
                                j = j0 + qb
                                qa = qview[:, r, 128 * j:128 * j + 128]
                                for pc in range(2):
                                    jj = j - 1 + pc
                                    u0 = 2048 // d + 128 * jj
                                    ka = kview[:, r, u0:u0 + 128]
                                    ins = E.matmul(BSC[b][:, (qb * 2 + pc) * 128:(qb * 2 + pc + 1) * 128], ka, qa, start=True, stop=True)
                            return ins
                        P.op('pe', sf, reads=kkeys + qkeys, writes=[('BSC', b)])
                        P.op('act', lambda E, b=b: E.activation(out=pT[b][:], in_=BSC[b][:], func=AF.Exp, scale=SCALE), reads=[('BSC', b)], writes=[('pT', b)])
                        P.op('pool', lambda E, b=b: E.tensor_tensor(pT[b][:], pT[b][:], mask2[:], op=ALU.mult), reads=[('pT', b), 'mask2'], writes=[('pT', b)])

                        def nf(E, r=r, j0=j0, b=b):
                            ins = None
                            for qb in range(2):
                                j = j0 + qb
                                bp = r * (J + 1) + j
                                bc = bp + 1
                                pp = pT[b][:, (qb * 2) * 128:(qb * 2 + 1) * 128]
                                pcur = pT[b][:, (qb * 2 + 1) * 128:(qb * 2 + 2) * 128]
                                E.matmul(BN[b][:, qb * 128:(qb + 1) * 128], vt[:, bp, :], pp, start=True, stop=False)
                                E.matmul(BN[b][:, qb * 128:(qb + 1) * 128], vt[:, bc, :], pcur, start=False, stop=True)
                                E.matmul(BN[b][:, (2 + qb) * 128:(3 + qb) * 128], (fones if j == 0 else ones_bf)[:], pp, start=True, stop=False)
                                ins = E.matmul(BN[b][:, (2 + qb) * 128:(3 + qb) * 128], ones_bf[:], pcur, start=False, stop=True)
                            return ins
                        P.op('pe', nf, reads=['vt', ('pT', b), 'onesb', 'fones'], writes=[('BN', b)])
                        av = accv[:, :, r, 128 * j0:128 * j0 + 256]
                        bnv = BN[b][:].rearrange("p (n x) -> p n x", n=2)
                        if g == 0:
                            P.op('act', lambda E, av=av, bnv=bnv: E.copy(out=av, in_=bnv), reads=[('BN', b)], writes=['acc'])
                        else:
                            P.op('dve', lambda E, av=av, bnv=bnv: E.tensor_tensor(av, av, bnv, op=ALU.add), reads=[('BN', b), 'acc'], writes=['acc'])
            for tl in range(8):
                o = tl % 2
                P.op('dve', lambda E: E.reciprocal(rden[:], acc[:, 1, tl * TT:(tl + 1) * TT]), reads=['acc'], writes=['rden'])
                P.op('pool', lambda E: E.tensor_tensor(atto[o][:], acc[:, 0, tl * TT:(tl + 1) * TT], rden[:], op=ALU.mult), reads=['acc', 'rden'], writes=[('atto', o)])
                P.op('sp', lambda E: E.dma_start(out=att_r[:, s, tl * TT:(tl + 1) * TT], in_=atto[o][:]), reads=[('atto', o)], dsem="p2b_ao%d" % o)
        P.barrier()


def build(debug=0):
    nc = bass.Bass("TRN2", target_bir_lowering=False)
    T = {}

    def din(name, shape, dt=F32):
        T[name] = nc.dram_tensor(name, shape, dt, kind="ExternalInput").ap()

    def scratch(name, shape, dt):
        kind = {"kind": "ExternalOutput"} if debug else {}
        T[name] = nc.dram_tensor(name, shape, dt, **kind).ap()
    din('xT', [D, NTOK])
    din('pT', [256, OWN])
    din('ffn1_norm', [128, 8]); din('mix_norm', [128, 8]); din('ffn2_norm', [128, 8]); din('ple_norm', [128, 8])
    din('ffn1_w_in', [D, 2 * DFF]); din('ffn1_w_out', [DFF, D])
    din('ffn2_w_in', [D, 2 * DFF]); din('ffn2_w_out', [DFF, D])
    din('w_in', [D, DIN])
    din('w_up_att', [512, D]); din('w_up_mlstm', [512, D]); din('w_out', [D, D])
    din('w_ple_gate', [D, D]); din('w_ple_proj', [256, D])
    din('conv_w', [128, 8, 4]); din('conv_b', [128, 8]); din('i_bias', [4, 1]); din('f_bias', [128, 4])
    din('q_gain', [128, 1]); din('k_gain', [128, 1])
    din('c_ident', [128, 128]); din('c_tri', [128, 128]); din('c_trit', [128, 128]); din('c_flag', [128, 1])
    din('c_rm', [32, 32]); din('c_cos', [32, 6144]); din('c_sin', [32, 6144])
    T['outT'] = nc.dram_tensor('outT', [D, OWN], F32, kind="ExternalOutput").ap()
    scratch('h1', [D, OWN], F32)
    scratch('u', [D, NTOK], BF16)
    scratch('att', [512, OWN], BF16)
    scratch('hg', [512, OWN], BF16)
    scratch('h2', [D, OWN], F32)
    stages = os.environ.get('KSTAGES', '1abcd')
    with ExitStack() as es:
        P = Prog(nc, es)
        if '1' in stages:
            ffn_phase(P, nc, T, 1)
        if 'a' in stages:
            mlstm_phase(P, nc, T)
        if 'b' in stages:
            attn_phase(P, nc, T)
        if 'c' in stages:
            merge_phase(P, nc, T)
        if 'd' in stages:
            ffn_phase(P, nc, T, 2)
    return nc


def _chunk_major(v):
    return np.ascontiguousarray(v.reshape(-1, 128).T).astype(np.float32)


def make_in_maps(inputs):
    x = np.asarray(inputs['x'], dtype=np.float32)
    p = np.asarray(inputs['p'], dtype=np.float32)[0]
    shared = {
        'ffn1_norm': _chunk_major(inputs['ffn1_norm'][0]), 'mix_norm': _chunk_major(inputs['mix_norm'][0]),
        'ffn2_norm': _chunk_major(inputs['ffn2_norm'][0]), 'ple_norm': _chunk_major(inputs['ple_norm'][0]),
        'ffn1_w_in': np.ascontiguousarray(inputs['ffn1_w_in'][0]), 'ffn1_w_out': np.ascontiguousarray(inputs['ffn1_w_out'][0]),
        'ffn2_w_in': np.ascontiguousarray(inputs['ffn2_w_in'][0]), 'ffn2_w_out': np.ascontiguousarray(inputs['ffn2_w_out'][0]),
        'w_in': np.ascontiguousarray(inputs['w_in'][0]),
        'w_up_att': np.ascontiguousarray(inputs['w_up_att'][0]), 'w_up_mlstm': np.ascontiguousarray(inputs['w_up_mlstm'][0]),
        'w_out': np.ascontiguousarray(inputs['w_out'][0]),
        'w_ple_gate': np.ascontiguousarray(inputs['w_ple_gate'][0]), 'w_ple_proj': np.ascontiguousarray(inputs['w_ple_proj'][0]),
    }
    cw = np.asarray(inputs['conv_w'][0], np.float32)
    shared['conv_w'] = np.ascontiguousarray(cw.reshape(4, 8, 128).transpose(2, 1, 0))
    shared['conv_b'] = _chunk_major(inputs['conv_b'][0])
    shared['i_bias'] = np.asarray(inputs['i_bias'][0], np.float32).reshape(4, 1).copy()
    shared['f_bias'] = np.ascontiguousarray(np.broadcast_to(np.asarray(inputs['f_bias'][0], np.float32)[None, :], (128, 4)))
    shared['q_gain'] = np.asarray(inputs['q_gain'][0], np.float32).reshape(128, 1).copy()
    shared['k_gain'] = np.asarray(inputs['k_gain'][0], np.float32).reshape(128, 1).copy()
    shared['c_ident'] = np.eye(128, dtype=np.float32)
    tri = np.triu(np.ones((128, 128), np.float32))
    shared['c_tri'] = tri
    shared['c_trit'] = np.ascontiguousarray(tri.T)
    rmm = np.zeros((32, 32), np.float32)
    for m_ in range(32):
        rmm[(m_ + 16) % 32, m_] = 1.0
    shared['c_rm'] = rmm
    half = 16
    inv_freq = (1.0 / (np.float32(500000.0) ** (np.arange(half, dtype=np.float32) / np.float32(half)))).astype(np.float32)
    maps = []
    for c in range(8):
        b, h = c // 2, c % 2
        xT = np.zeros((D, NTOK), np.float32)
        xT[:, OWN:] = x[b, h * OWN:(h + 1) * OWN].T
        if h == 1:
            xT[:, :OWN] = x[b, :OWN].T
        m = dict(shared)
        m['xT'] = xT
        m['pT'] = np.ascontiguousarray(p[b, h * OWN:(h + 1) * OWN].T)
        m['c_flag'] = np.full((128, 1), float(h), np.float32)
        pos = (np.arange(2048, 8192) - 4096 + 4096 * h).astype(np.float32)
        ang = (pos[None, :] * inv_freq[:, None]).astype(np.float32)
        cs_, sn_ = np.cos(ang).astype(np.float32), np.sin(ang).astype(np.float32)
        m['c_cos'] = np.ascontiguousarray(np.concatenate([cs_, cs_], axis=0))
        m['c_sin'] = np.ascontiguousarray(np.concatenate([-sn_, sn_], axis=0))
        maps.append(m)
    return maps


def kernel(**inputs):
    nc = build(0)
    maps = make_in_maps(inputs)
    res = run_bass_kernel_spmd(nc, maps, core_ids=list(range(8)))
    out = np.empty((4, 8192, D), np.float32)
    for c in range(8):
        b, h = c // 2, c % 2
        out[b, h * OWN:(h + 1) * OWN] = res.results[c]['outT'].T
    return out
```

```python
import os
import numpy as np
from contextlib import ExitStack
import concourse.bass as bass
import concourse.mybir as mybir
from concourse.bass_utils import run_bass_kernel_spmd

F32 = mybir.dt.float32
BF16 = mybir.dt.bfloat16
AF = mybir.ActivationFunctionType
ALU = mybir.AluOpType
AX = mybir.AxisListType

D = 1024
DFF = 2816
NTOK = 8192
OWN = 4096
DIN = 8712
EPS = 1e-6
C_AQ, C_AK, C_AV = 0, 1536, 3072
C_MQ, C_MK, C_MV, C_MO, C_MI, C_MF, C_GA, C_GB = 4608, 5120, 5632, 6144, 6656, 6660, 6664, 7688
DILS = (1, 4, 16)


class Sem:
    def __init__(self, h):
        self.h = h
        self.n = 0


class Prog:
    ENG = ('pe', 'act', 'dve', 'pool', 'sp')

    def __init__(self, nc, es):
        self.nc, self.es = nc, es
        self.e = {'pe': nc.tensor, 'act': nc.scalar, 'dve': nc.vector, 'pool': nc.gpsimd, 'sp': nc.sync}
        self.prog = {}
        self.seen = {k: {} for k in self.ENG}
        self.lastw = {}
        self.rd = {}
        self.nsem = 0
        self.allsems = []
        self.dsems = {}
        self.new_epoch()

    def mksem(self):
        self.nsem += 1
        s = Sem(self.es.enter_context(self.nc.semaphore("s%d" % self.nsem)))
        self.allsems.append(s)
        return s

    def new_epoch(self):
        for k in self.ENG:
            self.prog[k] = self.mksem()

    def dsem(self, name):
        if name not in self.dsems:
            self.dsems[name] = self.mksem()
        return self.dsems[name]

    def op(self, eng, fn, reads=(), writes=(), dsem=None):
        need = {}

        def add(t):
            if t is None:
                return
            sem, v = t
            if need.get(sem, 0) < v:
                need[sem] = v
        for k in reads:
            add(self.lastw.get(k))
        for k in writes:
            add(self.lastw.get(k))
            for sem, v in self.rd.get(k, {}).items():
                add((sem, v))
        E = self.e[eng]
        for sem, v in need.items():
            if eng == 'pe' and sem is self.prog['pe']:
                continue
            if self.seen[eng].get(sem, 0) >= v:
                continue
            E.wait_ge(sem.h, v)
            self.seen[eng][sem] = v
        ins = fn(E)
        if dsem is not None:
            sem = self.dsem(dsem) if isinstance(dsem, str) else dsem
            sem.n += 16
            ins.then_inc(sem.h, 16)
        else:
            sem = self.prog[eng]
            sem.n += 1
            ins.then_inc(sem.h, 1)
        tok = (sem, sem.n)
        for k in reads:
            d = self.rd.setdefault(k, {})
            if d.get(sem, 0) < sem.n:
                d[sem] = sem.n
        for k in writes:
            self.lastw[k] = tok
            self.rd[k] = {}
        return tok

    def barrier(self):
        for eng in self.ENG:
            E = self.e[eng]
            for sem in self.allsems:
                if sem.n > self.seen[eng].get(sem, 0):
                    E.wait_ge(sem.h, sem.n)
                    self.seen[eng][sem] = sem.n
        self.lastw = {}
        self.rd = {}
        self.new_epoch()


def mm_group(P, out, pairs, reads, writes):
    n = len(pairs)

    def fn(E):
        ins = None
        for i, (l, r) in enumerate(pairs):
            ins = E.matmul(out, l, r, start=(i == 0), stop=(i == n - 1))
        return ins
    return P.op('pe', fn, reads=reads, writes=writes)


def load_w_groups(P, dst, src, ngroups, nj, key, eng='pool'):
    bounds = [round(i * nj / ngroups) for i in range(ngroups + 1)]
    for g in range(ngroups):
        j0, j1 = bounds[g], bounds[g + 1]
        if j1 == j0:
            continue
        ks = [(key, j) for j in range(j0, j1)]
        P.op(eng, lambda E, j0=j0, j1=j1: E.dma_start(out=dst[:, :, j0 * 128:j1 * 128], in_=src[:, :, j0 * 128:j1 * 128]),
             writes=ks, dsem="%s_g%d" % (key, g))


def load_cast(P, stage, pieces, engs=('pool', 'act')):
    for (dst, src, key) in pieces:
        i = P.lc_i = getattr(P, 'lc_i', -1) + 1
        sl = i % len(stage)
        n = dst.shape[-1]
        st = stage[sl][:, 0:n]
        P.op('sp', lambda E: E.dma_start(out=st, in_=src), writes=[('stg', sl)], dsem="stg%d" % sl)
        eng = engs[i % len(engs)]
        if eng == 'act':
            P.op('act', lambda E: E.copy(out=dst, in_=st), reads=[('stg', sl)], writes=[key])
        else:
            P.op(eng, lambda E: E.tensor_copy(dst, st), reads=[('stg', sl)], writes=[key])


def rms_stats(P, nc, src_aps, skeys, sq, sqk, ones_bf, ps_ap, psk, tmp, tmpk, rstd, rstdk, inv_n, eps_ap=None):
    nchunk = len(src_aps)
    for c in range(nchunk):
        s = sq[c % len(sq)]
        sk = sqk[c % len(sq)]
        P.op('act', lambda E, s=s, c=c: E.activation(out=s, in_=src_aps[c], func=AF.Square),
             reads=[skeys[c]], writes=[sk])
        P.op('pe', lambda E, s=s, c=c: E.matmul(ps_ap, ones_bf, s, start=(c == 0), stop=(c == nchunk - 1)),
             reads=[sk, 'ones'], writes=[psk])
    P.op('act', lambda E: E.activation(out=tmp, in_=ps_ap, func=AF.Ln, bias=eps_ap, scale=inv_n), reads=[psk, 'epsc'], writes=[tmpk])
    P.op('act', lambda E: E.activation(out=rstd, in_=tmp, func=AF.Exp, scale=-0.5), reads=[tmpk], writes=[rstdk])


def ffn_phase(P, nc, T, which):
    TT = 256
    first = (which == 1)
    ntiles = (NTOK if first else OWN) // TT
    ntiles = int(os.environ.get('KNT', ntiles))
    own0 = (NTOK - OWN) // TT if first else 0
    w_in = T['ffn1_w_in'] if first else T['ffn2_w_in']
    w_out = T['ffn1_w_out'] if first else T['ffn2_w_out']
    src = T['xT'] if first else T['h2']
    with ExitStack() as ph:
        def sb(name, shape, dt):
            return ph.enter_context(nc.sbuf_tensor("p%d_%s" % (which, name), shape, dt))

        def pst(name, shape, dt=F32):
            return ph.enter_context(nc.psum_tensor("p%d_ps_%s" % (which, name), shape, dt))
        w1 = sb("w1", [128, 8, 2 * DFF], BF16)
        w2 = sb("w2", [128, 22, D], BF16)
        g1 = sb("g1", [128, 8], F32)
        g2 = sb("g2", [128, 8], F32)
        ones_bf = sb("ones", [128, 128], BF16)
        epsc = sb("epsc", [128, 1], F32)
        xh = [sb("xh%d" % i, [128, 8, TT], F32) for i in range(3)]
        xn = [sb("xn%d" % i, [128, 8, TT], BF16) for i in range(2)]
        un = [sb("un%d" % i, [128, 8, TT], BF16) for i in range(2)] if first else xn
        unk = 'un' if first else 'xn'
        sq = [sb("sq%d" % i, [128, TT], BF16) for i in range(2)]
        act = sb("act", [128, 22, TT], BF16)
        sil = [sb("sil%d" % i, [128, TT], BF16) for i in range(2)]
        tmp = [sb("tmp%d" % i, [128, TT], F32) for i in range(2 if first else 1)]
        rstd = [sb("rstd%d" % i, [128, TT], F32) for i in range(2)]
        stage = [sb("stg%d" % i, [128, 512], F32) for i in range(3 if first else 2)]
        ps_ss = pst("ss", [128, 512])
        ps_a = [pst("a%d" % i, [128, 512]) for i in range(2)]
        ps_o = [pst("o%d" % i, [128, 512]) for i in range(2)]
        if not first:
            wpg = sb("wpg", [128, 8, D], BF16)
            wpe = sb("wpe", [128, 2, D], BF16)
            pt = [sb("pt%d" % i, [128, 2, TT], BF16) for i in range(2)]
            sg = [sb("sg%d" % i, [128, TT], F32) for i in range(1)]
            ps_g = [pst("g%d" % i, [128, 512]) for i in range(2)]

        P.op('pool', lambda E: E.memset(ones_bf[:], 1.0), writes=['ones'])
        P.op('pool', lambda E: E.memset(epsc[:], EPS), writes=['epsc'])
        P.op('sp', lambda E: E.dma_start(out=g1[:], in_=T['ffn1_norm' if first else 'ffn2_norm'][:, :]), writes=['g1'], dsem="p%d_g1" % which)
        P.op('sp', lambda E: E.dma_start(out=g2[:], in_=T['mix_norm' if first else 'ple_norm'][:, :]), writes=['g2'], dsem="p%d_g2" % which)
        w_in_r = w_in.rearrange("(c p) n -> p c n", p=128)
        w_out_r = w_out.rearrange("(j p) n -> p j n", p=128)
        pieces = []
        for jg in range(0, 22, 4):
            j1 = min(jg + 4, 22)
            for part in range(2):
                for k in range(8):
                    c0 = part * DFF + jg * 128
                    c1 = part * DFF + j1 * 128
                    pieces.append((w1[:, k, c0:c1], w_in_r[:, k, c0:c1], ('w1', part, k, jg // 4)))
        for j in range(22):
            for hh in range(2):
                pieces.append((w2[:, j, hh * 512:(hh + 1) * 512], w_out_r[:, j, hh * 512:(hh + 1) * 512], ('w2', j, hh)))
        if not first:
            wpg_r = T['w_ple_gate'].rearrange("(c p) n -> p c n", p=128)
            wpe_r = T['w_ple_proj'].rearrange("(c p) n -> p c n", p=128)
            for k in range(8):
                for hh in range(2):
                    pieces.append((wpg[:, k, hh * 512:(hh + 1) * 512], wpg_r[:, k, hh * 512:(hh + 1) * 512], ('wpg', k, hh)))
            for k in range(2):
                for hh in range(2):
                    pieces.append((wpe[:, k, hh * 512:(hh + 1) * 512], wpe_r[:, k, hh * 512:(hh + 1) * 512], ('wpe', k, hh)))
        load_cast(P, stage, pieces)

        src_r = src.rearrange("(c p) n -> p c n", p=128)

        def load_x(i):
            s = i % 3
            P.op('sp', lambda E: E.dma_start(out=xh[s][:], in_=src_r[:, :, i * TT:(i + 1) * TT]),
                 writes=[('xh', s, c) for c in range(8)], dsem="p%d_xh%d" % (which, s))
        def load_p(i):
            if not first:
                s2 = i % 2
                for kk in range(2):
                    P.op('pool', lambda E, kk=kk: E.dma_start(out=pt[s2][:, kk, :], in_=T['pT'][kk * 128:(kk + 1) * 128, i * TT:(i + 1) * TT]),
                         writes=[('pt', s2)], dsem="p2_pt%d" % s2)

        def norm(i, gain, gkey, dst, dkey):
            s = i % 3
            d = i % 2
            rms_stats(P, nc, [xh[s][:, c, :] for c in range(8)], [('xh', s, c) for c in range(8)],
                      [q[:] for q in sq], [('sq', 0), ('sq', 1)], ones_bf[:], ps_ss[:, 0:TT], 'ps_ss',
                      tmp[d % len(tmp)][:], ('tmp', d % len(tmp)), rstd[d][:], ('rstd', d), 1.0 / D, eps_ap=epsc[:, 0:1])
            for c in range(8):
                P.op('dve', lambda E, c=c: E.scalar_tensor_tensor(dst[d][:, c, :], xh[s][:, c, :], gain[:, c:c + 1], rstd[d][:],
                                                                   op0=ALU.mult, op1=ALU.mult),
                     reads=[('xh', s, c), gkey, ('rstd', d)], writes=[(dkey, d, c)])

        def ffn_in(i):
            d = i % 2
            for j in range(22):
                b = j % 2
                pa = ps_a[b]
                mm_group(P, pa[:, 0:TT], [(w1[:, k, j * 128:(j + 1) * 128], xn[d][:, k, :]) for k in range(8)],
                         reads=[('w1', 0, k, j // 4) for k in range(8)] + [('xn', d, k) for k in range(8)], writes=[('bka', b)])
                mm_group(P, pa[:, TT:2 * TT], [(w1[:, k, DFF + j * 128:DFF + (j + 1) * 128], xn[d][:, k, :]) for k in range(8)],
                         reads=[('w1', 1, k, j // 4) for k in range(8)] + [('xn', d, k) for k in range(8)], writes=[('bka', b)])
                P.op('act', lambda E, b=b, pa=pa: E.activation(out=sil[b][:], in_=pa[:, 0:TT], func=AF.Silu),
                     reads=[('bka', b)], writes=[('sil', b)])
                P.op('dve', lambda E, b=b, pa=pa, j=j: E.tensor_tensor(act[:, j, :], sil[b][:], pa[:, TT:2 * TT], op=ALU.mult),
                     reads=[('sil', b), ('bka', b)], writes=[('act', j)])

        def ffn_out(i):
            s = i % 3
            for pr in range(4):
                bk = pr % 2
                for hf in range(2):
                    oc = pr * 2 + hf
                    po = ps_o[bk][:, hf * TT:(hf + 1) * TT]
                    mm_group(P, po, [(w2[:, j, oc * 128:(oc + 1) * 128], act[:, j, :]) for j in range(22)],
                             reads=[('w2', j, oc // 4) for j in range(22)] + [('act', j) for j in range(22)], writes=[('bko', bk)])
                for hf in range(2):
                    oc = pr * 2 + hf
                    po = ps_o[bk][:, hf * TT:(hf + 1) * TT]
                    P.op('dve', lambda E, oc=oc, po=po: E.scalar_tensor_tensor(xh[s][:, oc, :], po, 0.5, xh[s][:, oc, :], op0=ALU.mult, op1=ALU.add),
                         reads=[('bko', bk), ('xh', s, oc)], writes=[('xh', s, oc)])

        def ple(i):
            s = i % 3
            d = i % 2
            for oc in range(8):
                b = oc % 2
                pg = ps_g[b]
                mm_group(P, pg[:, 0:TT], [(wpg[:, k, oc * 128:(oc + 1) * 128], un[d][:, k, :]) for k in range(8)],
                         reads=[('wpg', k, oc // 4) for k in range(8)] + [(unk, d, k) for k in range(8)], writes=[('bkg', b)])
                mm_group(P, pg[:, TT:2 * TT], [(wpe[:, k, oc * 128:(oc + 1) * 128], pt[d][:, k, :]) for k in range(2)],
                         reads=[('wpe', k, oc // 4) for k in range(2)] + [('pt', d)], writes=[('bkg', b)])
                P.op('act', lambda E, b=b, pg=pg: E.activation(out=sg[0][:], in_=pg[:, 0:TT], func=AF.Sigmoid),
                     reads=[('bkg', b)], writes=[('sg', 0)])
                P.op('dve', lambda E, b=b, pg=pg: E.tensor_tensor(sg[0][:], sg[0][:], pg[:, TT:2 * TT], op=ALU.mult),
                     reads=[('sg', 0), ('bkg', b)], writes=[('sg', 0)])
                P.op('dve', lambda E, b=b, oc=oc: E.tensor_tensor(xh[s][:, oc, :], sg[0][:], xh[s][:, oc, :], op=ALU.add),
                     reads=[('sg', 0), ('xh', s, oc)], writes=[('xh', s, oc)])
            P.op('sp', lambda E: E.dma_start(out=T['outT'].rearrange("(c p) n -> p c n", p=128)[:, :, i * TT:(i + 1) * TT], in_=xh[s][:]),
                 reads=[('xh', s, oc) for oc in range(8)], dsem="p2_ot%d" % s)

        KSTOP = os.environ.get('KSTOP', '')
        if KSTOP == 'w':
            P.barrier(); return
        load_x(0)
        load_x(1)
        load_p(0)
        if KSTOP == 'x':
            P.barrier(); return
        norm(0, g1, 'g1', xn, 'xn')
        if KSTOP == 'n':
            P.barrier(); return
        if KSTOP == 'in':
            ffn_in(0); P.barrier(); return
        for i in range(ntiles):
            if i + 2 < ntiles:
                load_x(i + 2)
            if i + 1 < ntiles:
                load_p(i + 1)
            ffn_in(i)
            if i + 1 < ntiles:
                norm(i + 1, g1, 'g1', xn, 'xn')
            ffn_out(i)
            s = i % 3
            if first and i >= own0:
                io = i - own0
                P.op('sp', lambda E: E.dma_start(out=T['h1'].rearrange("(c p) n -> p c n", p=128)[:, :, io * TT:(io + 1) * TT], in_=xh[s][:]),
                     reads=[('xh', s, c) for c in range(8)], dsem="p1_h%d" % s)
            norm(i, g2, 'g2', un, unk)
            if first:
                d = i % 2
                P.op('sp', lambda E: E.dma_start(out=T['u'].rearrange("(c p) n -> p c n", p=128)[:, :, i * TT:(i + 1) * TT], in_=un[d][:]),
                     reads=[('un', d, c) for c in range(8)], dsem="p1_u%d" % d)
            else:
                ple(i)
        P.barrier()


def merge_phase(P, nc, T):
    TT = 512
    ntiles = int(os.environ.get('KNT3', OWN // TT))
    with ExitStack() as ph:
        def sb(name, shape, dt):
            return ph.enter_context(nc.sbuf_tensor("p3_" + name, shape, dt))

        def pst(name, shape, dt=F32):
            return ph.enter_context(nc.psum_tensor("p3_" + name, shape, dt))
        wg = sb("wg", [128, 8, 2048], BF16)
        wua = sb("wua", [128, 4, D], BF16)
        wub = sb("wub", [128, 4, D], BF16)
        wo = sb("wo", [128, 8, D], BF16)
        stage = [sb("stg%d" % i, [128, 512], F32) for i in range(3)]
        ut = [sb("ut%d" % i, [128, 8, TT], BF16) for i in range(2)]
        at = [sb("at%d" % i, [128, 4, TT], BF16) for i in range(2)]
        mt = [sb("mt%d" % i, [128, 4, TT], BF16) for i in range(2)]
        ht = [sb("ht%d" % i, [128, 8, TT], F32) for i in range(2)]
        mg = sb("mg", [128, 8, TT], BF16)
        sg = [sb("sg%d" % i, [128, TT], F32) for i in range(2)]
        m1 = [sb("m1%d" % i, [128, TT], F32) for i in range(2)]
        psg = [pst("g%d" % i, [128, 512]) for i in range(2)]
        psy = [pst("y%d" % i, [128, 512]) for i in range(2)]
        pso = [pst("o%d" % i, [128, 512]) for i in range(2)]
        w_in_r = T['w_in'].rearrange("(c p) n -> p c n", p=128)
        pieces = []
        for k in range(8):
            for q in range(4):
                pieces.append((wg[:, k, q * 512:(q + 1) * 512], w_in_r[:, k, C_GA + q * 512:C_GA + (q + 1) * 512], ('wg', k, q)))
        for nm, wt, src in (('wua', wua, T['w_up_att']), ('wub', wub, T['w_up_mlstm'])):
            r = src.rearrange("(c p) n -> p c n", p=128)
            for k in range(4):
                for q in range(2):
                    pieces.append((wt[:, k, q * 512:(q + 1) * 512], r[:, k, q * 512:(q + 1) * 512], (nm, k, q)))
        r = T['w_out'].rearrange("(c p) n -> p c n", p=128)
        for k in range(8):
            for q in range(2):
                pieces.append((wo[:, k, q * 512:(q + 1) * 512], r[:, k, q * 512:(q + 1) * 512], ('wo', k, q)))
        load_cast(P, stage, pieces)
        u_r = T['u'].rearrange("(c p) n -> p c n", p=128)
        a_r = T['att'].rearrange("(c p) n -> p c n", p=128)
        m_r = T['hg'].rearrange("(c p) n -> p c n", p=128)
        h_r = T['h1'].rearrange("(c p) n -> p c n", p=128)
        o_r = T['h2'].rearrange("(c p) n -> p c n", p=128)

        def loads(i):
            d = i % 2
            P.op('sp', lambda E: E.dma_start(out=ut[d][:], in_=u_r[:, :, OWN + i * TT:OWN + (i + 1) * TT]), writes=[('ut', d)], dsem="p3_ut%d" % d)
            P.op('sp', lambda E: E.dma_start(out=at[d][:], in_=a_r[:, :, i * TT:(i + 1) * TT]), writes=[('at', d)], dsem="p3_at%d" % d)
            P.op('sp', lambda E: E.dma_start(out=mt[d][:], in_=m_r[:, :, i * TT:(i + 1) * TT]), writes=[('mt', d)], dsem="p3_mt%d" % d)
            P.op('sp', lambda E: E.dma_start(out=ht[d][:], in_=h_r[:, :, i * TT:(i + 1) * TT]), writes=[('ht', d, c) for c in range(8)], dsem="p3_ht%d" % d)
        loads(0)
        for i in range(ntiles):
            d = i % 2
            if i + 1 < ntiles:
                loads(i + 1)
            for oc in range(8):
                for br in range(2):
                    b = br
                    wy, ykey, yt, ytk = (wua, 'wua', at, 'at') if br == 0 else (wub, 'wub', mt, 'mt')
                    mm_group(P, psg[b][:], [(wg[:, k, br * 1024 + oc * 128:br * 1024 + (oc + 1) * 128], ut[d][:, k, :]) for k in range(8)],
                             reads=[('wg', k, (br * 1024 + oc * 128) // 512) for k in range(8)] + [('ut', d)], writes=[('bkg', b)])
                    mm_group(P, psy[b][:], [(wy[:, k, oc * 128:(oc + 1) * 128], yt[d][:, k, :]) for k in range(4)],
                             reads=[(ykey, k, oc // 4) for k in range(4)] + [(ytk, d)], writes=[('bky', b)])
                    P.op('act', lambda E: E.activation(out=sg[b][:], in_=psg[b][:], func=AF.Sigmoid), reads=[('bkg', b)], writes=[('sg', b)])
                    P.op('dve', lambda E: E.tensor_tensor(m1[b][:], sg[b][:], psy[b][:], op=ALU.mult), reads=[('sg', b), ('bky', b)], writes=[('m1', b)])
                P.op('pool', lambda E: E.tensor_tensor(mg[:, oc, :], m1[0][:], m1[1][:], op=ALU.add), reads=[('m1', 0), ('m1', 1)], writes=[('mg', oc)])
            for oc in range(8):
                b = oc % 2
                mm_group(P, pso[b][:], [(wo[:, k, oc * 128:(oc + 1) * 128], mg[:, k, :]) for k in range(8)],
                         reads=[('wo', k, oc // 4) for k in range(8)] + [('mg', k) for k in range(8)], writes=[('bko', b)])
                P.op('dve', lambda E: E.tensor_tensor(ht[d][:, oc, :], pso[b][:], ht[d][:, oc, :], op=ALU.add),
                     reads=[('bko', b), ('ht', d, oc)], writes=[('ht', d, oc)])
            P.op('sp', lambda E: E.dma_start(out=o_r[:, :, i * TT:(i + 1) * TT], in_=ht[d][:]), reads=[('ht', d, c) for c in range(8)], dsem="p3_o%d" % d)
        P.barrier()


def mlstm_phase(P, nc, T):
    TT = 512
    ntiles = NTOK // TT
    own0 = (NTOK - OWN) // TT
    t_start = int(os.environ.get('KT2A0', 0))
    t_end = int(os.environ.get('KT2A1', ntiles))
    SC = 128 ** -0.5
    with ExitStack() as ph:
        def sb(name, shape, dt):
            return ph.enter_context(nc.sbuf_tensor("p2a_" + name, shape, dt))

        def pst(name, shape, dt=F32):
            return ph.enter_context(nc.psum_tensor("p2a_" + name, shape, dt))
        wm = sb("wm", [128, 8, 2048], BF16)
        wgt = sb("wgt", [128, 8, 8], BF16)
        wgf = sb("wgf", [128, 8, 8], F32)
        stage = [sb("stg%d" % i, [128, 512], F32) for i in range(3)]
        ident_f = sb("identf", [128, 128], F32)
        tri_f = sb("trif", [128, 128], F32)
        ident_bf = sb("identb", [128, 128], BF16)
        mask_bf = sb("maskb", [128, 128], BF16)
        ones_bf = sb("onesb", [128, 128], BF16)
        ones4 = sb("ones4", [4, 128], F32)
        selc = sb("selc", [4, 4, 128], F32)
        flag = sb("flag", [128, 1], F32)
        cw = sb("cw", [128, 8, 4], F32)
        cb = sb("cb", [128, 8], F32)
        ibias = sb("ibias", [4, 1], F32)
        fbias = sb("fbias", [128, 4], F32)
        Cst = sb("Cst", [128, 4, 129], F32)
        mst = sb("mst", [4, 1], F32)
        ut = [sb("ut%d" % i, [128, 8, TT], BF16) for i in range(2)]
        pre = sb("pre", [128, 8, TT + 3], F32)
        cv = sb("cv", [128, 8, TT], F32)
        qT = sb("qT", [128, 4, TT], BF16)
        kT = sb("kT", [128, 4, TT], BF16)
        so = sb("so", [128, 4, TT], F32)
        hgt = [sb("hgt%d" % i, [128, 4, TT], BF16) for i in range(2)]
        vt = sb("vt", [128, 4, 129], BF16)
        kw = sb("kw", [128, 4, 128], BF16)
        pT = sb("pT", [128, 4, 128], BF16)
        Cb = sb("Cb", [128, 4, 128], BF16)
        nbc = sb("nbc", [128, 4, 128], BF16)
        thrS = sb("thrS", [128, 512], F32)
        dd = sb("dd", [128, 512], F32)
        rr = sb("rr", [128, 512], F32)
        hh = sb("hh", [128, 512], F32)
        fx = sb("fx", [128, 4], F32)
        lt = sb("lt", [128, 4], F32)
        nbr = sb("nbr", [4, 128], F32)
        gp = sb("gp", [4, 128], F32)
        wrow = sb("wrow", [4, 128], F32)
        trow = sb("trow", [4, 128], F32)
        gmax = sb("gmax", [4, 1], F32)
        Mv = sb("Mv", [4, 1], F32)
        negM = sb("negM", [4, 1], F32)
        av = sb("av", [4, 1], F32)
        da = sb("da", [4, 4], F32)
        wtok = sb("wtok", [128, 4], F32)
        abc = sb("abc", [128, 4], F32)
        B0 = pst("b0", [128, 512])
        B1 = pst("b1", [128, 512])
        B2 = pst("b2", [128, 512])
        B3 = pst("b3", [128, 512])
        B4 = pst("b4", [128, 1024], BF16)
        B5 = pst("b5", [128, 512])
        B6 = pst("b6", [128, 512])
        B7 = pst("b7", [128, 512])

        P.op('sp', lambda E: E.dma_start(out=ident_f[:], in_=T['c_ident'][:, :]), writes=['identf'], dsem="p2a_c0")
        P.op('sp', lambda E: E.dma_start(out=tri_f[:], in_=T['c_tri'][:, :]), writes=['trif'], dsem="p2a_c1")
        P.op('sp', lambda E: E.dma_start(out=flag[:], in_=T['c_flag'][:, :]), writes=['flag'], dsem="p2a_c2")
        P.op('sp', lambda E: E.dma_start(out=cw[:], in_=T['conv_w'][:, :, :]), writes=['cw'], dsem="p2a_c3")
        P.op('sp', lambda E: E.dma_start(out=cb[:], in_=T['conv_b'][:, :]), writes=['cb'], dsem="p2a_c4")
        P.op('sp', lambda E: E.dma_start(out=ibias[:], in_=T['i_bias'][:, :]), writes=['ibias'], dsem="p2a_c5")
        P.op('sp', lambda E: E.dma_start(out=fbias[:], in_=T['f_bias'][:, :]), writes=['fbias'], dsem="p2a_c6")
        w_in_r = T['w_in'].rearrange("(c p) n -> p c n", p=128)
        P.op('sp', lambda E: E.dma_start(out=wgf[:], in_=w_in_r[:, :, C_MI:C_MI + 8]), writes=['wgf'], dsem="p2a_c7")
        P.op('pool', lambda E: E.tensor_copy(wgt[:], wgf[:]), reads=['wgf'], writes=['wgt'])
        P.op('pool', lambda E: E.tensor_copy(ident_bf[:], ident_f[:]), reads=['identf'], writes=['identb'])
        P.op('pool', lambda E: E.tensor_copy(mask_bf[:], tri_f[:]), reads=['trif'], writes=['maskb'])
        P.op('pool', lambda E: E.memset(ones_bf[:], 1.0), writes=['onesb'])
        P.op('pool', lambda E: E.memset(ones4[:], 1.0), writes=['ones4'])
        P.op('pool', lambda E: E.memset(Cst[:], 0.0), writes=['Cst'])
        P.op('pool', lambda E: E.memset(mst[:], 0.0), writes=['mst'])
        P.op('pool', lambda E: E.memset(vt[:], 1.0), writes=['vt'])
        P.op('pool', lambda E: E.memset(pre[:], 0.0), writes=[('pre', c) for c in range(8)])
        for h in range(4):
            P.op('pool', lambda E, h=h: E.tensor_scalar(selc[:, h, :], ones4[:], ident_f[0:4, h:h + 1], None, op0=ALU.mult),
                 reads=['ones4', 'identf'], writes=['selc'])
        pieces = []
        for k in range(8):
            for q in range(4):
                pieces.append((wm[:, k, q * 512:(q + 1) * 512], w_in_r[:, k, C_MQ + q * 512:C_MQ + (q + 1) * 512], ('wm', k, q)))
        load_cast(P, stage, pieces)
        u_r = T['u'].rearrange("(c p) n -> p c n", p=128)
        hg_r = T['hg'].rearrange("(c p) n -> p c n", p=128)

        def load_u(i):
            d = i % 2
            P.op('sp', lambda E: E.dma_start(out=ut[d][:], in_=u_r[:, :, i * TT:(i + 1) * TT]), writes=[('ut', d)], dsem="p2a_ut%d" % d)

        load_u(t_start)
        for i in range(t_start, t_end):
            d = i % 2
            own = i >= own0
            if i + 1 < t_end:
                load_u(i + 1)
            fcs = ([(0, fc) for fc in range(4)] if (own or i == own0 - 1) else []) + [(1, fc) for fc in range(4)]
            for (kind, fc) in fcs:
                c8 = kind * 4 + fc
                mm_group(P, B0[:], [(wm[:, k, c8 * 128:(c8 + 1) * 128], ut[d][:, k, :]) for k in range(8)],
                         reads=[('wm', k, kind) for k in range(8)] + [('ut', d)], writes=['B0'])
                P.op('act', lambda E: E.copy(out=pre[:, c8, 3:TT + 3], in_=B0[:]), reads=['B0'], writes=[('pre', c8)])
            if own:
                for fc in range(4):
                    mm_group(P, B0[:], [(wm[:, k, 1536 + fc * 128:1536 + (fc + 1) * 128], ut[d][:, k, :]) for k in range(8)],
                             reads=[('wm', k, 3) for k in range(8)] + [('ut', d)], writes=['B0'])
                    P.op('act', lambda E: E.activation(out=so[:, fc, :], in_=B0[:], func=AF.Sigmoid), reads=['B0'], writes=[('so', fc)])
            for (kind, fc) in fcs:
                c8 = kind * 4 + fc
                eng = 'dve'
                P.op(eng, lambda E: E.tensor_scalar(cv[:, c8, :], pre[:, c8, 0:TT], cw[:, c8, 0:1], cb[:, c8:c8 + 1], op0=ALU.mult, op1=ALU.add),
                     reads=[('pre', c8), 'cw', 'cb'], writes=[('cv', c8)])
                for j in range(1, 4):
                    P.op(eng, lambda E: E.scalar_tensor_tensor(cv[:, c8, :], pre[:, c8, j:TT + j], cw[:, c8, j:j + 1], cv[:, c8, :], op0=ALU.mult, op1=ALU.add),
                         reads=[('pre', c8), ('cv', c8), 'cw'], writes=[('cv', c8)])
                P.op(eng, lambda E: E.tensor_copy(pre[:, c8, 0:3], pre[:, c8, TT:TT + 3]), reads=[('pre', c8)], writes=[('pre', c8)])
                if kind == 0:
                    P.op('act', lambda E: E.activation(out=cv[:, c8, :], in_=cv[:, c8, :], func=AF.Silu), reads=[('cv', c8)], writes=[('cv', c8)])
                    P.op('pool', lambda E: E.tensor_scalar(qT[:, fc, :], cv[:, c8, :], SC, None, op0=ALU.mult), reads=[('cv', c8)], writes=[('qT', fc)])
                else:
                    P.op('act', lambda E: E.activation(out=kT[:, fc, :], in_=cv[:, c8, :], func=AF.Silu), reads=[('cv', c8)], writes=[('kT', fc)])
            for ci in range(4):
                c0 = ci * 128
                cs = slice(c0, c0 + 128)
                mm_group(P, B1[:], [(ut[d][:, k, cs], wm[:, k, 1024:1536]) for k in range(8)],
                         reads=[('wm', k, 2) for k in range(8)] + [('ut', d)], writes=['B1'])
                mm_group(P, B2[:, 0:8], [(ut[d][:, k, cs], wgt[:, k, 0:8]) for k in range(8)], reads=['wgt', ('ut', d)], writes=['B2'])
                mm_group(P, B2[0:4, 8:136], [(wgt[:, k, 0:4], ut[d][:, k, cs]) for k in range(8)], reads=['wgt', ('ut', d)], writes=['B2'])
                P.op('act', lambda E: E.copy(out=vt[:, :, 0:128], in_=B1[:].rearrange("p (h e) -> p h e", h=4)), reads=['B1'], writes=['vt'])
                P.op('dve', lambda E: E.tensor_tensor(fx[:], B2[:, 4:8], fbias[:], op=ALU.add), reads=['B2', 'fbias'], writes=['fx'])
                P.op('act', lambda E: E.activation(out=fx[:], in_=fx[:], func=AF.Exp, scale=-1.0), reads=['fx'], writes=['fx'])
                P.op('act', lambda E: E.activation(out=lt[:], in_=fx[:], func=AF.Ln, bias=1.0, scale=1.0), reads=['fx'], writes=['lt'])
                mm_group(P, B3[0:4, 0:128], [(lt[:], tri_f[:])], reads=['lt', 'trif'], writes=['B3'])
                P.op('act', lambda E: E.copy(out=nbr[:], in_=B3[0:4, 0:128]), reads=['B3'], writes=['nbr'])
                P.op('dve', lambda E: E.scalar_tensor_tensor(gp[:], B2[0:4, 8:136], ibias[:, 0:1], nbr[:], op0=ALU.add, op1=ALU.add),
                     reads=['B2', 'ibias', 'nbr'], writes=['gp'])
                P.op('dve', lambda E: E.reduce_max(gmax[:], gp[:], axis=AX.X), reads=['gp'], writes=['gmax'])
                P.op('dve', lambda E: E.tensor_tensor(Mv[:], gmax[:], mst[:], op=ALU.max), reads=['gmax', 'mst'], writes=['Mv'])
                P.op('dve', lambda E: E.tensor_scalar(negM[:], Mv[:], -1.0, None, op0=ALU.mult), reads=['Mv'], writes=['negM'])
                P.op('act', lambda E: E.activation(out=wrow[:], in_=gp[:], func=AF.Exp, bias=negM[:, 0:1], scale=1.0), reads=['gp', 'negM'], writes=['wrow'])
                P.op('act', lambda E: E.activation(out=av[:], in_=mst[:], func=AF.Exp, bias=negM[:, 0:1], scale=1.0), reads=['mst', 'negM'], writes=['av'])
                if own:
                    P.op('act', lambda E: E.activation(out=trow[:], in_=nbr[:], func=AF.Exp, bias=negM[:, 0:1], scale=1.0), reads=['nbr', 'negM'], writes=['trow'])
                P.op('dve', lambda E: E.tensor_tensor(mst[:], Mv[:], nbr[:, 127:128], op=ALU.subtract), reads=['Mv', 'nbr'], writes=['mst'])
                mm_group(P, B3[:, 128:132], [(wrow[:], ident_f[0:4, 0:4])], reads=['wrow', 'identf'], writes=['B3'])
                P.op('dve', lambda E: E.tensor_scalar(da[:], ident_f[0:4, 0:4], av[:, 0:1], None, op0=ALU.mult), reads=['av', 'identf'], writes=['da'])
                mm_group(P, B3[:, 136:140], [(ones4[:], da[:])], reads=['ones4', 'da'], writes=['B3'])
                if own:
                    P.op('dve', lambda E: E.tensor_copy(wtok[:], B3[:, 128:132]), reads=['B3'], writes=['wtok'])
                else:
                    P.op('dve', lambda E: E.tensor_scalar(wtok[:], B3[:, 128:132], flag[:, 0:1], None, op0=ALU.mult), reads=['B3', 'flag'], writes=['wtok'])
                P.op('dve', lambda E: E.tensor_copy(abc[:], B3[:, 136:140]), reads=['B3'], writes=['abc'])
                for h in range(4):
                    P.op('pool', lambda E, h=h: E.tensor_scalar(Cst[:, h, :], Cst[:, h, :], abc[:, h:h + 1], None, op0=ALU.mult), reads=['Cst', 'abc'], writes=['Cst'])
                def trf(E):
                    ins = None
                    for h in range(4):
                        ins = E.transpose(B4[:, h * 128:(h + 1) * 128], kT[:, h, cs], ident_bf[:])
                    return ins
                P.op('pe', trf, reads=[('kT', h) for h in range(4)] + ['identb'], writes=['B4'])
                for h in range(4):
                    P.op('dve', lambda E, h=h: E.tensor_scalar(kw[:, h, :], B4[:, h * 128:(h + 1) * 128], wtok[:, h:h + 1], None, op0=ALU.mult),
                         reads=['B4', 'wtok'], writes=['kw'])
                if own:
                    P.op('act', lambda E: E.copy(out=Cb[:], in_=Cst[:, :, 0:128]), reads=['Cst'], writes=['Cb'])
                    for h in range(4):
                        P.op('pool', lambda E, h=h: E.tensor_scalar(nbc[:, h, :], ones_bf[:], Cst[:, h, 128:129], None, op0=ALU.mult), reads=['Cst', 'onesb'], writes=['nbc'])
                    def qkf(E):
                        ins = None
                        for h in range(4):
                            ins = E.matmul(B5[:, h * 128:(h + 1) * 128], kT[:, h, cs], qT[:, h, cs], start=True, stop=True)
                        return ins
                    P.op('pe', qkf, reads=[('kT', h) for h in range(4)] + [('qT', h) for h in range(4)], writes=['B5'])
                    for h in range(4):
                        P.op('dve', lambda E, h=h: E.scalar_tensor_tensor(pT[:, h, :], B5[:, h * 128:(h + 1) * 128], wtok[:, h:h + 1], mask_bf[:], op0=ALU.mult, op1=ALU.mult),
                             reads=['B5', 'wtok', 'maskb'], writes=['pT'])
                    def numf(E):
                        ins = None
                        for h in range(4):
                            E.matmul(B6[:, h * 128:(h + 1) * 128], Cb[:, h, :], qT[:, h, cs], start=True, stop=False)
                            ins = E.matmul(B6[:, h * 128:(h + 1) * 128], vt[:, h, 0:128], pT[:, h, :], start=False, stop=True)
                        return ins
                    P.op('pe', numf, reads=['Cb', 'vt', 'pT'] + [('qT', h) for h in range(4)], writes=['B6'])

                    def denf(E):
                        ins = None
                        for h in range(4):
                            E.matmul(B1[:, h * 128:(h + 1) * 128], nbc[:, h, :], qT[:, h, cs], start=True, stop=False)
                            ins = E.matmul(B1[:, h * 128:(h + 1) * 128], ones_bf[:], pT[:, h, :], start=False, stop=True)
                        return ins
                    P.op('pe', denf, reads=['nbc', 'onesb', 'pT'] + [('qT', h) for h in range(4)], writes=['B1'])

                    def thrf(E):
                        ins = None
                        for h in range(4):
                            ins = E.matmul(B0[:, h * 128:(h + 1) * 128], selc[:, h, :], trow[:], start=True, stop=True)
                        return ins
                    P.op('pe', thrf, reads=['selc', 'trow'], writes=['B0'])
                    P.op('act', lambda E: E.copy(out=thrS[:], in_=B0[:]), reads=['B0'], writes=['thrS'])
                    P.op('dve', lambda E: E.tensor_tensor(dd[:], B1[:], thrS[:], op=ALU.max), reads=['B1', 'thrS'], writes=['dd'])
                    P.op('dve', lambda E: E.scalar_tensor_tensor(dd[:], B1[:], -1.0, dd[:], op0=ALU.mult, op1=ALU.max), reads=['B1', 'dd'], writes=['dd'])
                    P.op('dve', lambda E: E.reciprocal(rr[:], dd[:]), reads=['dd'], writes=['rr'])
                    P.op('dve', lambda E: E.tensor_tensor(hh[:], B6[:], rr[:], op=ALU.mult), reads=['B6', 'rr'], writes=['hh'])
                    P.op('pool', lambda E: E.tensor_tensor(hgt[d][:, :, cs], hh[:].rearrange("p (h t) -> p h t", h=4), so[:, :, cs], op=ALU.mult),
                         reads=['hh'] + [('so', fc) for fc in range(4)], writes=[('hgt', d)])
                def dcf(E):
                    ins = None
                    for h in range(4):
                        bank = B5 if h < 2 else B7
                        ins = E.matmul(bank[:, (h % 2) * 129:(h % 2 + 1) * 129], kw[:, h, :], vt[:, h, :], start=True, stop=True)
                    return ins
                P.op('pe', dcf, reads=['kw', 'vt'], writes=['B5', 'B7'])
                P.op('dve', lambda E: E.tensor_tensor(Cst[:, 0:2, :], Cst[:, 0:2, :], B5[:, 0:258].rearrange("p (h e) -> p h e", h=2), op=ALU.add), reads=['Cst', 'B5'], writes=['Cst'])
                P.op('dve', lambda E: E.tensor_tensor(Cst[:, 2:4, :], Cst[:, 2:4, :], B7[:, 0:258].rearrange("p (h e) -> p h e", h=2), op=ALU.add), reads=['Cst', 'B7'], writes=['Cst'])
            if own:
                io = i - own0
                P.op('sp', lambda E: E.dma_start(out=hg_r[:, :, io * TT:(io + 1) * TT], in_=hgt[d][:]), reads=[('hgt', d)], dsem="p2a_hg%d" % d)
        P.barrier()


def attn_phase(P, nc, T):
    TT = 512
    L = 6144
    SCALE = 128 ** -0.5
    slots = int(os.environ.get('KSLOTS', 4))
    with ExitStack() as ph:
        def sb(name, shape, dt):
            return ph.enter_context(nc.sbuf_tensor("p2b_" + name, shape, dt))

        def pst(name, shape, dt=F32):
            return ph.enter_context(nc.psum_tensor("p2b_" + name, shape, dt))
        ures = sb("ures", [128, 8, L], BF16)
        wq = sb("wq", [128, 8, 128], BF16)
        wk = sb("wk", [128, 8, 128], BF16)
        wv = sb("wv", [128, 8, 128], BF16)
        stage = [sb("stg%d" % i, [128, 128], F32) for i in range(4)]
        qT = sb("qT", [128, OWN], BF16)
        kT = sb("kT", [128, L], BF16)
        vt = sb("vt", [128, 48, 128], BF16)
        acc = sb("acc", [128, 2, OWN], F32)
        ones_bf = sb("onesb", [128, 128], BF16)
        fones = sb("fones", [128, 128], BF16)
        mask2 = sb("mask2", [128, 512], BF16)
        trif = sb("trif", [128, 128], F32)
        tritf = sb("tritf", [128, 128], F32)
        flag = sb("flag", [128, 1], F32)
        gq = sb("gq", [128, 1], F32)
        gk = sb("gk", [128, 1], F32)
        rm = sb("rm", [32, 32], F32)
        sq = [sb("sq%d" % i, [128, TT], BF16) for i in range(2)]
        tmp = [sb("tmp%d" % i, [128, TT], F32) for i in range(2)]
        rstd = [sb("rstd%d" % i, [128, TT], F32) for i in range(2)]
        qn = [sb("qn%d" % i, [128, TT], F32) for i in range(2)]
        t1 = [sb("t1%d" % i, [32, TT], F32) for i in range(2)]
        t2 = [sb("t2%d" % i, [32, TT], F32) for i in range(2)]
        epsc = sb("epsc", [128, 1], F32)
        cosb = [sb("cos%d" % i, [32, TT], F32) for i in range(2)]
        sinb = [sb("sin%d" % i, [32, TT], F32) for i in range(2)]
        pT = [sb("pT%d" % i, [128, 512], BF16) for i in range(2)]
        atto = [sb("atto%d" % i, [128, TT], BF16) for i in range(2)]
        rden = sb("rden", [128, TT], F32)
        BK = [pst("bk%d" % i, [128, 512]) for i in range(8)]
        BSC = [BK[0], BK[1]]
        BN = [BK[2], BK[3]]

        P.op('sp', lambda E: E.dma_start(out=trif[:], in_=T['c_tri'][:, :]), writes=['trif'], dsem="p2b_c0")
        P.op('sp', lambda E: E.dma_start(out=tritf[:], in_=T['c_trit'][:, :]), writes=['tritf'], dsem="p2b_c1")
        P.op('sp', lambda E: E.dma_start(out=flag[:], in_=T['c_flag'][:, :]), writes=['flag'], dsem="p2b_c2")
        P.op('sp', lambda E: E.dma_start(out=gq[:], in_=T['q_gain'][:, :]), writes=['gq'], dsem="p2b_c3")
        P.op('sp', lambda E: E.dma_start(out=gk[:], in_=T['k_gain'][:, :]), writes=['gk'], dsem="p2b_c4")
        P.op('sp', lambda E: E.dma_start(out=rm[:], in_=T['c_rm'][:, :]), writes=['rm'], dsem="p2b_c5")
        P.op('pool', lambda E: E.memset(ones_bf[:], 1.0), writes=['onesb'])
        P.op('pool', lambda E: E.memset(epsc[:], EPS), writes=['epsc'])
        P.op('pool', lambda E: E.tensor_scalar(fones[:], ones_bf[:], flag[:, 0:1], None, op0=ALU.mult), reads=['onesb', 'flag'], writes=['fones'])
        for qb in range(2):
            P.op('pool', lambda E, qb=qb: E.tensor_copy(mask2[:, (qb * 2) * 128:(qb * 2 + 1) * 128], tritf[:]), reads=['tritf'], writes=['mask2'])
            P.op('pool', lambda E, qb=qb: E.tensor_copy(mask2[:, (qb * 2 + 1) * 128:(qb * 2 + 2) * 128], trif[:]), reads=['trif'], writes=['mask2'])
        u_r = T['u'].rearrange("(c p) n -> p c n", p=128)
        for tl in range(12):
            P.op('sp', lambda E, tl=tl: E.dma_start(out=ures[:, :, tl * TT:(tl + 1) * TT], in_=u_r[:, :, 2048 + tl * TT:2048 + (tl + 1) * TT]),
                 writes=[('ures', tl)], dsem="p2b_u%d" % tl)
            if tl % 4 == 3:
                pass
        ures_all = [('ures', tl) for tl in range(12)]
        w_in_r = T['w_in'].rearrange("(c p) n -> p c n", p=128)
        att_r = T['att'].rearrange("(c p) n -> p c n", p=128)
        cnt = [0]

        def prep(kind, tl):
            l0 = tl * TT
            w, wkey, gain, gkey = (wq, 'wq', gq, 'gq') if kind == 'q' else (wk, 'wk', gk, 'gk')
            dst = qT[:, l0 - 2048:l0 - 2048 + TT] if kind == 'q' else kT[:, l0:l0 + TT]
            dkey = (kind + 'T', tl)
            cs = cnt[0] % 2
            cnt[0] += 1
            P.op('sp', lambda E: E.dma_start(out=cosb[cs][:], in_=T['c_cos'][:, l0:l0 + TT]), writes=[('cos', cs)], dsem="p2b_cos%d" % cs)
            P.op('sp', lambda E: E.dma_start(out=sinb[cs][:], in_=T['c_sin'][:, l0:l0 + TT]), writes=[('sin', cs)], dsem="p2b_sin%d" % cs)
            BQ, BS, BR = BK[0 + cs], BK[4 + cs], BK[6 + cs]
            kq, ks_, kr = ('bk', 0 + cs), ('bk', 4 + cs), ('bk', 6 + cs)
            mm_group(P, BQ[:], [(w[:, k, :], ures[:, k, l0:l0 + TT]) for k in range(8)], reads=[wkey, ('ures', tl)], writes=[kq])
            P.op('act', lambda E: E.activation(out=sq[cs][:], in_=BQ[:], func=AF.Square), reads=[kq], writes=[('sq', cs)])
            mm_group(P, BS[:], [(ones_bf[:], sq[cs][:])], reads=['onesb', ('sq', cs)], writes=[ks_])
            P.op('act', lambda E: E.activation(out=tmp[cs][:], in_=BS[:], func=AF.Ln, bias=epsc[:, 0:1], scale=1.0 / 128), reads=[ks_, 'epsc'], writes=[('tmp', cs)])
            P.op('act', lambda E: E.activation(out=rstd[cs][:], in_=tmp[cs][:], func=AF.Exp, scale=-0.5), reads=[('tmp', cs)], writes=[('rstd', cs)])
            P.op('dve', lambda E: E.scalar_tensor_tensor(qn[cs][:], BQ[:], gain[:, 0:1], rstd[cs][:], op0=ALU.mult, op1=ALU.mult), reads=[kq, gkey, ('rstd', cs)], writes=[('qn', cs)])
            mm_group(P, BR[0:32, :], [(rm[:], qn[cs][0:32, :])], reads=['rm', ('qn', cs)], writes=[kr])
            P.op('pool', lambda E: E.tensor_tensor(t1[cs][:], qn[cs][0:32, :], cosb[cs][:], op=ALU.mult), reads=[('qn', cs), ('cos', cs)], writes=[('t1', cs)])
            P.op('dve', lambda E: E.tensor_tensor(t2[cs][:], BR[0:32, :], sinb[cs][:], op=ALU.mult), reads=[kr, ('sin', cs)], writes=[('t2', cs)])
            P.op('pool', lambda E: E.tensor_tensor(dst[0:32, :], t1[cs][:], t2[cs][:], op=ALU.add), reads=[('t1', cs), ('t2', cs)], writes=[dkey])
            P.op('pool', lambda E: E.tensor_copy(dst[32:64, :], qn[cs][32:64, :]), reads=[('qn', cs)], writes=[(kind + 'Tc', tl)])
            P.op('act', lambda E: E.copy(out=dst[64:128, :], in_=qn[cs][64:128, :]), reads=[('qn', cs)], writes=[(kind + 'Tb', tl)])

        for s in range(slots):
            for g in range(3):
                d = DILS[g]
                J = OWN // (128 * d)
                hk = g * 4 + s
                pieces = []
                for (wt, nm, c0) in ((wq, 'wq', C_AQ), (wk, 'wk', C_AK), (wv, 'wv', C_AV)):
                    for k in range(8):
                        pieces.append((wt[:, k, :], w_in_r[:, k, c0 + hk * 128:c0 + (hk + 1) * 128], nm))
                load_cast(P, stage, pieces, engs=('pool',))
                ktl0 = 0 if d == 16 else 3
                ktiles = list(range(ktl0, 12))
                for tl in ktiles:
                    prep('k', tl)
                for tl in range(4, 12):
                    prep('q', tl)
                kkeys = [('kT', tl) for tl in ktiles] + [('kTb', tl) for tl in ktiles] + [('kTc', tl) for tl in ktiles]
                qkeys = [('qT', tl) for tl in range(4, 12)] + [('qTb', tl) for tl in range(4, 12)] + [('qTc', tl) for tl in range(4, 12)]
                nblk = d * (J + 1)
                blks = [(r, j) for r in range(d) for j in range(-1, J)]
                for b0 in range(0, nblk, 4):
                    grp = blks[b0:b0 + 4]

                    vb = (b0 // 4) % 2
                    BVb = BK[2 + vb]

                    def vf(E, grp=grp, BVb=BVb):
                        ins = None
                        for qi, (r, j) in enumerate(grp):
                            u0 = 2048 // d + 128 * j
                            for k in range(8):
                                lhs = ures[:, k, :].rearrange("p (u d) -> p d u", d=d)[:, r, u0:u0 + 128]
                                ins = E.matmul(BVb[:, qi * 128:(qi + 1) * 128], lhs, wv[:, k, :], start=(k == 0), stop=(k == 7))
                        return ins
                    P.op('pe', vf, reads=['wv'] + ures_all, writes=[('bk', 2 + vb)])
                    n = len(grp)
                    P.op('act' if vb == 0 else 'dve', (lambda E, b0=b0, n=n, BVb=BVb: E.copy(out=vt[:, b0:b0 + n, :], in_=BVb[:, 0:n * 128].rearrange("p (b e) -> p b e", e=128))) if vb == 0 else
                         (lambda E, b0=b0, n=n, BVb=BVb: E.tensor_copy(vt[:, b0:b0 + n, :], BVb[:, 0:n * 128].rearrange("p (b e) -> p b e", e=128))),
                         reads=[('bk', 2 + vb)], writes=[('vt', b0)])
                kview = kT[:, :].rearrange("p (u d) -> p d u", d=d)
                qview = qT[:, :].rearrange("p (u d) -> p d u", d=d)
                accv = acc[:, :, :].rearrange("p n (u d) -> p n d u", d=d)
                it = 0
                for r in range(d):
                    for jp in range(J // 2):
                        j0 = 2 * jp
                        b = it % 2
                        it += 1

                        def sf(E, r=r, j0=j0, b=b):
                            ins = None
                            for qb in range(2):
                                j = j0 + qb
                                qa = qview[:, r, 128 * j:128 * j + 128]
                                for pc in range(2):
                                    jj = j - 1 + pc
                                    u0 = 2048 // d + 128 * jj
                                    ka = kview[:, r, u0:u0 + 128]
                                    ins = E.matmul(BSC[b][:, (qb * 2 + pc) * 128:(qb * 2 + pc + 1) * 128], ka, qa, start=True, stop=True)
                            return ins
                        P.op('pe', sf, reads=kkeys + qkeys, writes=[('bk', b)])
                        P.op('act', lambda E, b=b: E.activation(out=pT[b][:], in_=BSC[b][:], func=AF.Exp, scale=SCALE), reads=[('bk', b)], writes=[('pT', b)])
                        P.op('pool', lambda E, b=b: E.tensor_tensor(pT[b][:], pT[b][:], mask2[:], op=ALU.mult), reads=[('pT', b), 'mask2'], writes=[('pT', b)])

                        def nf(E, r=r, j0=j0, b=b):
                            ins = None
                            for qb in range(2):
                                j = j0 + qb
                                bp = r * (J + 1) + j
                                bc = bp + 1
                                pp = pT[b][:, (qb * 2) * 128:(qb * 2 + 1) * 128]
                                pcur = pT[b][:, (qb * 2 + 1) * 128:(qb * 2 + 2) * 128]
                                E.matmul(BN[b][:, qb * 128:(qb + 1) * 128], vt[:, bp, :], pp, start=True, stop=False)
                                E.matmul(BN[b][:, qb * 128:(qb + 1) * 128], vt[:, bc, :], pcur, start=False, stop=True)
                                E.matmul(BN[b][:, (2 + qb) * 128:(3 + qb) * 128], (fones if j == 0 else ones_bf)[:], pp, start=True, stop=False)
                                ins = E.matmul(BN[b][:, (2 + qb) * 128:(3 + qb) * 128], ones_bf[:], pcur, start=False, stop=True)
                            return ins
                        P.op('pe', nf, reads=[('vt', (bb // 4) * 4) for bb in (r * (J + 1) + j0, r * (J + 1) + j0 + 1, r * (J + 1) + j0 + 2)] + [('pT', b), 'onesb', 'fones'], writes=[('bk', 2 + b)])
                        av = accv[:, :, r, 128 * j0:128 * j0 + 256]
                        bnv = BN[b][:].rearrange("p (n x) -> p n x", n=2)
                        if g == 0:
                            P.op('act', lambda E, av=av, bnv=bnv: E.copy(out=av, in_=bnv), reads=[('bk', 2 + b)], writes=['acc'])
                        else:
                            P.op('dve', lambda E, av=av, bnv=bnv: E.tensor_tensor(av, av, bnv, op=ALU.add), reads=[('bk', 2 + b), 'acc'], writes=['acc'])
            for tl in range(8):
                o = tl % 2
                P.op('dve', lambda E: E.reciprocal(rden[:], acc[:, 1, tl * TT:(tl + 1) * TT]), reads=['acc'], writes=['rden'])
                P.op('pool', lambda E: E.tensor_tensor(atto[o][:], acc[:, 0, tl * TT:(tl + 1) * TT], rden[:], op=ALU.mult), reads=['acc', 'rden'], writes=[('atto', o)])
                P.op('sp', lambda E: E.dma_start(out=att_r[:, s, tl * TT:(tl + 1) * TT], in_=atto[o][:]), reads=[('atto', o)], dsem="p2b_ao%d" % o)
        P.barrier()


def build(debug=0):
    nc = bass.Bass("TRN2", target_bir_lowering=False)
    T = {}

    def din(name, shape, dt=F32):
        T[name] = nc.dram_tensor(name, shape, dt, kind="ExternalInput").ap()

    def scratch(name, shape, dt):
        kind = {"kind": "ExternalOutput"} if debug else {}
        T[name] = nc.dram_tensor(name, shape, dt, **kind).ap()
    din('xT', [D, NTOK])
    din('pT', [256, OWN])
    din('ffn1_norm', [128, 8]); din('mix_norm', [128, 8]); din('ffn2_norm', [128, 8]); din('ple_norm', [128, 8])
    din('ffn1_w_in', [D, 2 * DFF]); din('ffn1_w_out', [DFF, D])
    din('ffn2_w_in', [D, 2 * DFF]); din('ffn2_w_out', [DFF, D])
    din('w_in', [D, DIN])
    din('w_up_att', [512, D]); din('w_up_mlstm', [512, D]); din('w_out', [D, D])
    din('w_ple_gate', [D, D]); din('w_ple_proj', [256, D])
    din('conv_w', [128, 8, 4]); din('conv_b', [128, 8]); din('i_bias', [4, 1]); din('f_bias', [128, 4])
    din('q_gain', [128, 1]); din('k_gain', [128, 1])
    din('c_ident', [128, 128]); din('c_tri', [128, 128]); din('c_trit', [128, 128]); din('c_flag', [128, 1])
    din('c_rm', [32, 32]); din('c_cos', [32, 6144]); din('c_sin', [32, 6144])
    T['outT'] = nc.dram_tensor('outT', [D, OWN], F32, kind="ExternalOutput").ap()
    scratch('h1', [D, OWN], F32)
    scratch('u', [D, NTOK], BF16)
    scratch('att', [512, OWN], BF16)
    scratch('hg', [512, OWN], BF16)
    scratch('h2', [D, OWN], F32)
    stages = os.environ.get('KSTAGES', '1abcd')
    with ExitStack() as es:
        P = Prog(nc, es)
        if '1' in stages:
            ffn_phase(P, nc, T, 1)
        if 'a' in stages:
            mlstm_phase(P, nc, T)
        if 'b' in stages:
            attn_phase(P, nc, T)
        if 'c' in stages:
            merge_phase(P, nc, T)
        if 'd' in stages:
            ffn_phase(P, nc, T, 2)
    return nc


def _chunk_major(v):
    return np.ascontiguousarray(v.reshape(-1, 128).T).astype(np.float32)


def make_in_maps(inputs):
    x = np.asarray(inputs['x'], dtype=np.float32)
    p = np.asarray(inputs['p'], dtype=np.float32)[0]
    shared = {
        'ffn1_norm': _chunk_major(inputs['ffn1_norm'][0]), 'mix_norm': _chunk_major(inputs['mix_norm'][0]),
        'ffn2_norm': _chunk_major(inputs['ffn2_norm'][0]), 'ple_norm': _chunk_major(inputs['ple_norm'][0]),
        'ffn1_w_in': np.ascontiguousarray(inputs['ffn1_w_in'][0]), 'ffn1_w_out': np.ascontiguousarray(inputs['ffn1_w_out'][0]),
        'ffn2_w_in': np.ascontiguousarray(inputs['ffn2_w_in'][0]), 'ffn2_w_out': np.ascontiguousarray(inputs['ffn2_w_out'][0]),
        'w_in': np.ascontiguousarray(inputs['w_in'][0]),
        'w_up_att': np.ascontiguousarray(inputs['w_up_att'][0]), 'w_up_mlstm': np.ascontiguousarray(inputs['w_up_mlstm'][0]),
        'w_out': np.ascontiguousarray(inputs['w_out'][0]),
        'w_ple_gate': np.ascontiguousarray(inputs['w_ple_gate'][0]), 'w_ple_proj': np.ascontiguousarray(inputs['w_ple_proj'][0]),
    }
    cw = np.asarray(inputs['conv_w'][0], np.float32)
    shared['conv_w'] = np.ascontiguousarray(cw.reshape(4, 8, 128).transpose(2, 1, 0))
    shared['conv_b'] = _chunk_major(inputs['conv_b'][0])
    shared['i_bias'] = np.asarray(inputs['i_bias'][0], np.float32).reshape(4, 1).copy()
    shared['f_bias'] = np.ascontiguousarray(np.broadcast_to(np.asarray(inputs['f_bias'][0], np.float32)[None, :], (128, 4)))
    shared['q_gain'] = np.asarray(inputs['q_gain'][0], np.float32).reshape(128, 1).copy()
    shared['k_gain'] = np.asarray(inputs['k_gain'][0], np.float32).reshape(128, 1).copy()
    shared['c_ident'] = np.eye(128, dtype=np.float32)
    tri = np.triu(np.ones((128, 128), np.float32))
    shared['c_tri'] = tri
    shared['c_trit'] = np.ascontiguousarray(tri.T)
    rmm = np.zeros((32, 32), np.float32)
    for m_ in range(32):
        rmm[(m_ + 16) % 32, m_] = 1.0
    shared['c_rm'] = rmm
    half = 16
    inv_freq = (1.0 / (np.float32(500000.0) ** (np.arange(half, dtype=np.float32) / np.float32(half)))).astype(np.float32)
    maps = []
    for c in range(8):
        b, h = c // 2, c % 2
        xT = np.zeros((D, NTOK), np.float32)
        xT[:, OWN:] = x[b, h * OWN:(h + 1) * OWN].T
        if h == 1:
            xT[:, :OWN] = x[b, :OWN].T
        m = dict(shared)
        m['xT'] = xT
        m['pT'] = np.ascontiguousarray(p[b, h * OWN:(h + 1) * OWN].T)
        m['c_flag'] = np.full((128, 1), float(h), np.float32)
        pos = (np.arange(2048, 8192) - 4096 + 4096 * h).astype(np.float32)
        ang = (pos[None, :] * inv_freq[:, None]).astype(np.float32)
        cs_, sn_ = np.cos(ang).astype(np.float32), np.sin(ang).astype(np.float32)
        m['c_cos'] = np.ascontiguousarray(np.concatenate([cs_, cs_], axis=0))
        m['c_sin'] = np.ascontiguousarray(np.concatenate([-sn_, sn_], axis=0))
        maps.append(m)
    return maps


def kernel(**inputs):
    nc = build(0)
    maps = make_in_maps(inputs)
    res = run_bass_kernel_spmd(nc, maps, core_ids=list(range(8)))
    out = np.empty((4, 8192, D), np.float32)
    for c in range(8):
        b, h = c // 2, c % 2
        out[b, h * OWN:(h + 1) * OWN] = res.results[c]['outT'].T
    return out
```

```python
import os
import numpy as np
from contextlib import ExitStack
import concourse.bass as bass
import concourse.mybir as mybir
from concourse.bass_utils import run_bass_kernel_spmd

F32 = mybir.dt.float32
BF16 = mybir.dt.bfloat16
AF = mybir.ActivationFunctionType
ALU = mybir.AluOpType
AX = mybir.AxisListType

D = 1024
DFF = 2816
NTOK = 8192
OWN = 4096
DIN = 8712
EPS = 1e-6
C_AQ, C_AK, C_AV = 0, 1536, 3072
C_MQ, C_MK, C_MV, C_MO, C_MI, C_MF, C_GA, C_GB = 4608, 5120, 5632, 6144, 6656, 6660, 6664, 7688
DILS = (1, 4, 16)


class Sem:
    def __init__(self, h):
        self.h = h
        self.n = 0


class Prog:
    ENG = ('pe', 'act', 'dve', 'pool', 'sp')

    def __init__(self, nc, es):
        self.nc, self.es = nc, es
        self.e = {'pe': nc.tensor, 'act': nc.scalar, 'dve': nc.vector, 'pool': nc.gpsimd, 'sp': nc.sync}
        self.prog = {}
        self.seen = {k: {} for k in self.ENG}
        self.lastw = {}
        self.rd = {}
        self.nsem = 0
        self.allsems = []
        self.dsems = {}
        self.new_epoch()

    def mksem(self):
        self.nsem += 1
        s = Sem(self.es.enter_context(self.nc.semaphore("s%d" % self.nsem)))
        self.allsems.append(s)
        return s

    def new_epoch(self):
        for k in self.ENG:
            self.prog[k] = self.mksem()

    def dsem(self, name):
        if name not in self.dsems:
            self.dsems[name] = self.mksem()
        return self.dsems[name]

    def op(self, eng, fn, reads=(), writes=(), dsem=None):
        need = {}

        def add(t):
            if t is None:
                return
            sem, v = t
            if need.get(sem, 0) < v:
                need[sem] = v
        for k in reads:
            add(self.lastw.get(k))
        for k in writes:
            add(self.lastw.get(k))
            for sem, v in self.rd.get(k, {}).items():
                add((sem, v))
        E = self.e[eng]
        for sem, v in need.items():
            if eng == 'pe' and sem is self.prog['pe']:
                continue
            if self.seen[eng].get(sem, 0) >= v:
                continue
            E.wait_ge(sem.h, v)
            self.seen[eng][sem] = v
        ins = fn(E)
        if dsem is not None:
            sem = self.dsem(dsem) if isinstance(dsem, str) else dsem
            sem.n += 16
            ins.then_inc(sem.h, 16)
        else:
            sem = self.prog[eng]
            sem.n += 1
            ins.then_inc(sem.h, 1)
        tok = (sem, sem.n)
        for k in reads:
            d = self.rd.setdefault(k, {})
            if d.get(sem, 0) < sem.n:
                d[sem] = sem.n
        for k in writes:
            self.lastw[k] = tok
            self.rd[k] = {}
        return tok

    def barrier(self):
        for eng in self.ENG:
            E = self.e[eng]
            for sem in self.allsems:
                if sem.n > self.seen[eng].get(sem, 0):
                    E.wait_ge(sem.h, sem.n)
                    self.seen[eng][sem] = sem.n
        self.lastw = {}
        self.rd = {}
        self.new_epoch()


def mm_group(P, out, pairs, reads, writes):
    n = len(pairs)

    def fn(E):
        ins = None
        for i, (l, r) in enumerate(pairs):
            ins = E.matmul(out, l, r, start=(i == 0), stop=(i == n - 1))
        return ins
    return P.op('pe', fn, reads=reads, writes=writes)


def load_w_groups(P, dst, src, ngroups, nj, key, eng='pool'):
    bounds = [round(i * nj / ngroups) for i in range(ngroups + 1)]
    for g in range(ngroups):
        j0, j1 = bounds[g], bounds[g + 1]
        if j1 == j0:
            continue
        ks = [(key, j) for j in range(j0, j1)]
        P.op(eng, lambda E, j0=j0, j1=j1: E.dma_start(out=dst[:, :, j0 * 128:j1 * 128], in_=src[:, :, j0 * 128:j1 * 128]),
             writes=ks, dsem="%s_g%d" % (key, g))


def load_cast(P, stage, pieces, engs=('pool', 'act')):
    for (dst, src, key) in pieces:
        i = P.lc_i = getattr(P, 'lc_i', -1) + 1
        sl = i % len(stage)
        n = dst.shape[-1]
        st = stage[sl][:, 0:n]
        P.op('sp', lambda E: E.dma_start(out=st, in_=src), writes=[('stg', sl)], dsem="stg%d" % sl)
        eng = engs[i % len(engs)]
        if eng == 'act':
            P.op('act', lambda E: E.copy(out=dst, in_=st), reads=[('stg', sl)], writes=[key])
        else:
            P.op(eng, lambda E: E.tensor_copy(dst, st), reads=[('stg', sl)], writes=[key])


def rms_stats(P, nc, src_aps, skeys, sq, sqk, ones_bf, ps_ap, psk, tmp, tmpk, rstd, rstdk, inv_n, eps_ap=None):
    nchunk = len(src_aps)
    for c in range(nchunk):
        s = sq[c % len(sq)]
        sk = sqk[c % len(sq)]
        P.op('act', lambda E, s=s, c=c: E.activation(out=s, in_=src_aps[c], func=AF.Square),
             reads=[skeys[c]], writes=[sk])
        P.op('pe', lambda E, s=s, c=c: E.matmul(ps_ap, ones_bf, s, start=(c == 0), stop=(c == nchunk - 1)),
             reads=[sk, 'ones'], writes=[psk])
    P.op('act', lambda E: E.activation(out=tmp, in_=ps_ap, func=AF.Ln, bias=eps_ap, scale=inv_n), reads=[psk, 'epsc'], writes=[tmpk])
    P.op('act', lambda E: E.activation(out=rstd, in_=tmp, func=AF.Exp, scale=-0.5), reads=[tmpk], writes=[rstdk])


def ffn_phase(P, nc, T, which):
    TT = 256
    first = (which == 1)
    ntiles = (NTOK if first else OWN) // TT
    ntiles = int(os.environ.get('KNT', ntiles))
    own0 = (NTOK - OWN) // TT if first else 0
    w_in = T['ffn1_w_in'] if first else T['ffn2_w_in']
    w_out = T['ffn1_w_out'] if first else T['ffn2_w_out']
    src = T['xT'] if first else T['h2']
    with ExitStack() as ph:
        def sb(name, shape, dt):
            return ph.enter_context(nc.sbuf_tensor("p%d_%s" % (which, name), shape, dt))

        def pst(name, shape, dt=F32):
            return ph.enter_context(nc.psum_tensor("p%d_ps_%s" % (which, name), shape, dt))
        w1 = sb("w1", [128, 8, 2 * DFF], BF16)
        w2 = sb("w2", [128, 22, D], BF16)
        g1 = sb("g1", [128, 8], F32)
        g2 = sb("g2", [128, 8], F32)
        ones_bf = sb("ones", [128, 128], BF16)
        epsc = sb("epsc", [128, 1], F32)
        xh = [sb("xh%d" % i, [128, 8, TT], F32) for i in range(3)]
        xn = [sb("xn%d" % i, [128, 8, TT], BF16) for i in range(2)]
        un = [sb("un%d" % i, [128, 8, TT], BF16) for i in range(2)] if first else xn
        unk = 'un' if first else 'xn'
        sq = [sb("sq%d" % i, [128, TT], BF16) for i in range(2)]
        act = sb("act", [128, 22, TT], BF16)
        sil = [sb("sil%d" % i, [128, TT], BF16) for i in range(2)]
        tmp = [sb("tmp%d" % i, [128, TT], F32) for i in range(2 if first else 1)]
        rstd = [sb("rstd%d" % i, [128, TT], F32) for i in range(2)]
        stage = [sb("stg%d" % i, [128, 512], F32) for i in range(3 if first else 2)]
        ps_ss = pst("ss", [128, 512])
        ps_a = [pst("a%d" % i, [128, 512]) for i in range(2)]
        ps_o = [pst("o%d" % i, [128, 512]) for i in range(2)]
        if not first:
            wpg = sb("wpg", [128, 8, D], BF16)
            wpe = sb("wpe", [128, 2, D], BF16)
            pt = [sb("pt%d" % i, [128, 2, TT], BF16) for i in range(2)]
            sg = [sb("sg%d" % i, [128, TT], F32) for i in range(1)]
            ps_g = [pst("g%d" % i, [128, 512]) for i in range(2)]

        P.op('pool', lambda E: E.memset(ones_bf[:], 1.0), writes=['ones'])
        P.op('pool', lambda E: E.memset(epsc[:], EPS), writes=['epsc'])
        P.op('sp', lambda E: E.dma_start(out=g1[:], in_=T['ffn1_norm' if first else 'ffn2_norm'][:, :]), writes=['g1'], dsem="p%d_g1" % which)
        P.op('sp', lambda E: E.dma_start(out=g2[:], in_=T['mix_norm' if first else 'ple_norm'][:, :]), writes=['g2'], dsem="p%d_g2" % which)
        w_in_r = w_in.rearrange("(c p) n -> p c n", p=128)
        w_out_r = w_out.rearrange("(j p) n -> p j n", p=128)
        pieces = []
        for jg in range(0, 22, 4):
            j1 = min(jg + 4, 22)
            for part in range(2):
                for k in range(8):
                    c0 = part * DFF + jg * 128
                    c1 = part * DFF + j1 * 128
                    pieces.append((w1[:, k, c0:c1], w_in_r[:, k, c0:c1], ('w1', part, k, jg // 4)))
        for j in range(22):
            for hh in range(2):
                pieces.append((w2[:, j, hh * 512:(hh + 1) * 512], w_out_r[:, j, hh * 512:(hh + 1) * 512], ('w2', j, hh)))
        if not first:
            wpg_r = T['w_ple_gate'].rearrange("(c p) n -> p c n", p=128)
            wpe_r = T['w_ple_proj'].rearrange("(c p) n -> p c n", p=128)
            for k in range(8):
                for hh in range(2):
                    pieces.append((wpg[:, k, hh * 512:(hh + 1) * 512], wpg_r[:, k, hh * 512:(hh + 1) * 512], ('wpg', k, hh)))
            for k in range(2):
                for hh in range(2):
                    pieces.append((wpe[:, k, hh * 512:(hh + 1) * 512], wpe_r[:, k, hh * 512:(hh + 1) * 512], ('wpe', k, hh)))
        load_cast(P, stage, pieces)

        src_r = src.rearrange("(c p) n -> p c n", p=128)

        def load_x(i):
            s = i % 3
            P.op('sp', lambda E: E.dma_start(out=xh[s][:], in_=src_r[:, :, i * TT:(i + 1) * TT]),
                 writes=[('xh', s, c) for c in range(8)], dsem="p%d_xh%d" % (which, s))
        def load_p(i):
            if not first:
                s2 = i % 2
                for kk in range(2):
                    P.op('pool', lambda E, kk=kk: E.dma_start(out=pt[s2][:, kk, :], in_=T['pT'][kk * 128:(kk + 1) * 128, i * TT:(i + 1) * TT]),
                         writes=[('pt', s2)], dsem="p2_pt%d" % s2)

        def norm(i, gain, gkey, dst, dkey):
            s = i % 3
            d = i % 2
            rms_stats(P, nc, [xh[s][:, c, :] for c in range(8)], [('xh', s, c) for c in range(8)],
                      [q[:] for q in sq], [('sq', 0), ('sq', 1)], ones_bf[:], ps_ss[:, 0:TT], 'ps_ss',
                      tmp[d % len(tmp)][:], ('tmp', d % len(tmp)), rstd[d][:], ('rstd', d), 1.0 / D, eps_ap=epsc[:, 0:1])
            for c in range(8):
                P.op('dve', lambda E, c=c: E.scalar_tensor_tensor(dst[d][:, c, :], xh[s][:, c, :], gain[:, c:c + 1], rstd[d][:],
                                                                   op0=ALU.mult, op1=ALU.mult),
                     reads=[('xh', s, c), gkey, ('rstd', d)], writes=[(dkey, d, c)])

        def ffn_in(i):
            d = i % 2
            for j in range(22):
                b = j % 2
                pa = ps_a[b]
                mm_group(P, pa[:, 0:TT], [(w1[:, k, j * 128:(j + 1) * 128], xn[d][:, k, :]) for k in range(8)],
                         reads=[('w1', 0, k, j // 4) for k in range(8)] + [('xn', d, k) for k in range(8)], writes=[('bka', b)])
                mm_group(P, pa[:, TT:2 * TT], [(w1[:, k, DFF + j * 128:DFF + (j + 1) * 128], xn[d][:, k, :]) for k in range(8)],
                         reads=[('w1', 1, k, j // 4) for k in range(8)] + [('xn', d, k) for k in range(8)], writes=[('bka', b)])
                P.op('act', lambda E, b=b, pa=pa: E.activation(out=sil[b][:], in_=pa[:, 0:TT], func=AF.Silu),
                     reads=[('bka', b)], writes=[('sil', b)])
                P.op('dve', lambda E, b=b, pa=pa, j=j: E.tensor_tensor(act[:, j, :], sil[b][:], pa[:, TT:2 * TT], op=ALU.mult),
                     reads=[('sil', b), ('bka', b)], writes=[('act', j)])

        def ffn_out(i):
            s = i % 3
            for pr in range(4):
                bk = pr % 2
                for hf in range(2):
                    oc = pr * 2 + hf
                    po = ps_o[bk][:, hf * TT:(hf + 1) * TT]
                    mm_group(P, po, [(w2[:, j, oc * 128:(oc + 1) * 128], act[:, j, :]) for j in range(22)],
                             reads=[('w2', j, oc // 4) for j in range(22)] + [('act', j) for j in range(22)], writes=[('bko', bk)])
                for hf in range(2):
                    oc = pr * 2 + hf
                    po = ps_o[bk][:, hf * TT:(hf + 1) * TT]
                    P.op('dve', lambda E, oc=oc, po=po: E.scalar_tensor_tensor(xh[s][:, oc, :], po, 0.5, xh[s][:, oc, :], op0=ALU.mult, op1=ALU.add),
                         reads=[('bko', bk), ('xh', s, oc)], writes=[('xh', s, oc)])

        def ple(i):
            s = i % 3
            d = i % 2
            for oc in range(8):
                b = oc % 2
                pg = ps_g[b]
                mm_group(P, pg[:, 0:TT], [(wpg[:, k, oc * 128:(oc + 1) * 128], un[d][:, k, :]) for k in range(8)],
                         reads=[('wpg', k, oc // 4) for k in range(8)] + [(unk, d, k) for k in range(8)], writes=[('bkg', b)])
                mm_group(P, pg[:, TT:2 * TT], [(wpe[:, k, oc * 128:(oc + 1) * 128], pt[d][:, k, :]) for k in range(2)],
                         reads=[('wpe', k, oc // 4) for k in range(2)] + [('pt', d)], writes=[('bkg', b)])
                P.op('act', lambda E, b=b, pg=pg: E.activation(out=sg[0][:], in_=pg[:, 0:TT], func=AF.Sigmoid),
                     reads=[('bkg', b)], writes=[('sg', 0)])
                P.op('dve', lambda E, b=b, pg=pg: E.tensor_tensor(sg[0][:], sg[0][:], pg[:, TT:2 * TT], op=ALU.mult),
                     reads=[('sg', 0), ('bkg', b)], writes=[('sg', 0)])
                P.op('dve', lambda E, b=b, oc=oc: E.tensor_tensor(xh[s][:, oc, :], sg[0][:], xh[s][:, oc, :], op=ALU.add),
                     reads=[('sg', 0), ('xh', s, oc)], writes=[('xh', s, oc)])
            P.op('sp', lambda E: E.dma_start(out=T['outT'].rearrange("(c p) n -> p c n", p=128)[:, :, i * TT:(i + 1) * TT], in_=xh[s][:]),
                 reads=[('xh', s, oc) for oc in range(8)], dsem="p2_ot%d" % s)

        KSTOP = os.environ.get('KSTOP', '')
        if KSTOP == 'w':
            P.barrier(); return
        load_x(0)
        load_x(1)
        load_p(0)
        norm(0, g1, 'g1', xn, 'xn')
        if first:
            for i in range(ntiles):
                if i + 2 < ntiles:
                    load_x(i + 2)
                ffn_in(i)
                if i + 1 < ntiles:
                    norm(i + 1, g1, 'g1', xn, 'xn')
                ffn_out(i)
                s = i % 3
                if i >= own0:
                    io = i - own0
                    P.op('sp', lambda E: E.dma_start(out=T['h1'].rearrange("(c p) n -> p c n", p=128)[:, :, io * TT:(io + 1) * TT], in_=xh[s][:]),
                         reads=[('xh', s, c) for c in range(8)], dsem="p1_h%d" % s)
                norm(i, g2, 'g2', un, unk)
                d = i % 2
                P.op('sp', lambda E: E.dma_start(out=T['u'].rearrange("(c p) n -> p c n", p=128)[:, :, i * TT:(i + 1) * TT], in_=un[d][:]),
                     reads=[('un', d, c) for c in range(8)], dsem="p1_u%d" % d)
        else:
            for i in range(ntiles):
                ffn_in(i)
                if i > 0:
                    ple(i - 1)
                if i + 2 < ntiles:
                    load_x(i + 2)
                if i + 1 < ntiles:
                    load_p(i + 1)
                    norm(i + 1, g1, 'g1', xn, 'xn')
                ffn_out(i)
                norm(i, g2, 'g2', un, unk)
            ple(ntiles - 1)
        P.barrier()


def merge_phase(P, nc, T):
    TT = 512
    ntiles = int(os.environ.get('KNT3', OWN // TT))
    with ExitStack() as ph:
        def sb(name, shape, dt):
            return ph.enter_context(nc.sbuf_tensor("p3_" + name, shape, dt))

        def pst(name, shape, dt=F32):
            return ph.enter_context(nc.psum_tensor("p3_" + name, shape, dt))
        wg = sb("wg", [128, 8, 2048], BF16)
        wua = sb("wua", [128, 4, D], BF16)
        wub = sb("wub", [128, 4, D], BF16)
        wo = sb("wo", [128, 8, D], BF16)
        stage = [sb("stg%d" % i, [128, 512], F32) for i in range(3)]
        ut = [sb("ut%d" % i, [128, 8, TT], BF16) for i in range(2)]
        at = [sb("at%d" % i, [128, 4, TT], BF16) for i in range(2)]
        mt = [sb("mt%d" % i, [128, 4, TT], BF16) for i in range(2)]
        ht = [sb("ht%d" % i, [128, 8, TT], F32) for i in range(2)]
        mg = sb("mg", [128, 8, TT], BF16)
        sg = [sb("sg%d" % i, [128, TT], F32) for i in range(2)]
        m1 = [sb("m1%d" % i, [128, TT], F32) for i in range(2)]
        psg = [pst("g%d" % i, [128, 512]) for i in range(2)]
        psy = [pst("y%d" % i, [128, 512]) for i in range(2)]
        pso = [pst("o%d" % i, [128, 512]) for i in range(2)]
        w_in_r = T['w_in'].rearrange("(c p) n -> p c n", p=128)
        pieces = []
        for k in range(8):
            for q in range(4):
                pieces.append((wg[:, k, q * 512:(q + 1) * 512], w_in_r[:, k, C_GA + q * 512:C_GA + (q + 1) * 512], ('wg', k, q)))
        for nm, wt, src in (('wua', wua, T['w_up_att']), ('wub', wub, T['w_up_mlstm'])):
            r = src.rearrange("(c p) n -> p c n", p=128)
            for k in range(4):
                for q in range(2):
                    pieces.append((wt[:, k, q * 512:(q + 1) * 512], r[:, k, q * 512:(q + 1) * 512], (nm, k, q)))
        r = T['w_out'].rearrange("(c p) n -> p c n", p=128)
        for k in range(8):
            for q in range(2):
                pieces.append((wo[:, k, q * 512:(q + 1) * 512], r[:, k, q * 512:(q + 1) * 512], ('wo', k, q)))
        load_cast(P, stage, pieces)
        u_r = T['u'].rearrange("(c p) n -> p c n", p=128)
        a_r = T['att'].rearrange("(c p) n -> p c n", p=128)
        m_r = T['hg'].rearrange("(c p) n -> p c n", p=128)
        h_r = T['h1'].rearrange("(c p) n -> p c n", p=128)
        o_r = T['h2'].rearrange("(c p) n -> p c n", p=128)

        def loads(i):
            d = i % 2
            P.op('sp', lambda E: E.dma_start(out=ut[d][:], in_=u_r[:, :, OWN + i * TT:OWN + (i + 1) * TT]), writes=[('ut', d)], dsem="p3_ut%d" % d)
            P.op('sp', lambda E: E.dma_start(out=at[d][:], in_=a_r[:, :, i * TT:(i + 1) * TT]), writes=[('at', d)], dsem="p3_at%d" % d)
            P.op('sp', lambda E: E.dma_start(out=mt[d][:], in_=m_r[:, :, i * TT:(i + 1) * TT]), writes=[('mt', d)], dsem="p3_mt%d" % d)
            P.op('sp', lambda E: E.dma_start(out=ht[d][:], in_=h_r[:, :, i * TT:(i + 1) * TT]), writes=[('ht', d, c) for c in range(8)], dsem="p3_ht%d" % d)
        loads(0)
        for i in range(ntiles):
            d = i % 2
            if i + 1 < ntiles:
                loads(i + 1)
            for oc in range(8):
                for br in range(2):
                    b = br
                    wy, ykey, yt, ytk = (wua, 'wua', at, 'at') if br == 0 else (wub, 'wub', mt, 'mt')
                    mm_group(P, psg[b][:], [(wg[:, k, br * 1024 + oc * 128:br * 1024 + (oc + 1) * 128], ut[d][:, k, :]) for k in range(8)],
                             reads=[('wg', k, (br * 1024 + oc * 128) // 512) for k in range(8)] + [('ut', d)], writes=[('bkg', b)])
                    mm_group(P, psy[b][:], [(wy[:, k, oc * 128:(oc + 1) * 128], yt[d][:, k, :]) for k in range(4)],
                             reads=[(ykey, k, oc // 4) for k in range(4)] + [(ytk, d)], writes=[('bky', b)])
                    P.op('act', lambda E: E.activation(out=sg[b][:], in_=psg[b][:], func=AF.Sigmoid), reads=[('bkg', b)], writes=[('sg', b)])
                    P.op('dve', lambda E: E.tensor_tensor(m1[b][:], sg[b][:], psy[b][:], op=ALU.mult), reads=[('sg', b), ('bky', b)], writes=[('m1', b)])
                P.op('pool', lambda E: E.tensor_tensor(mg[:, oc, :], m1[0][:], m1[1][:], op=ALU.add), reads=[('m1', 0), ('m1', 1)], writes=[('mg', oc)])
            for oc in range(8):
                b = oc % 2
                mm_group(P, pso[b][:], [(wo[:, k, oc * 128:(oc + 1) * 128], mg[:, k, :]) for k in range(8)],
                         reads=[('wo', k, oc // 4) for k in range(8)] + [('mg', k) for k in range(8)], writes=[('bko', b)])
                P.op('dve', lambda E: E.tensor_tensor(ht[d][:, oc, :], pso[b][:], ht[d][:, oc, :], op=ALU.add),
                     reads=[('bko', b), ('ht', d, oc)], writes=[('ht', d, oc)])
            P.op('sp', lambda E: E.dma_start(out=o_r[:, :, i * TT:(i + 1) * TT], in_=ht[d][:]), reads=[('ht', d, c) for c in range(8)], dsem="p3_o%d" % d)
        P.barrier()


def mlstm_phase(P, nc, T):
    TT = 512
    ntiles = NTOK // TT
    own0 = (NTOK - OWN) // TT
    t_start = int(os.environ.get('KT2A0', 0))
    t_end = int(os.environ.get('KT2A1', ntiles))
    SC = 128 ** -0.5
    with ExitStack() as ph:
        def sb(name, shape, dt):
            return ph.enter_context(nc.sbuf_tensor("p2a_" + name, shape, dt))

        def pst(name, shape, dt=F32):
            return ph.enter_context(nc.psum_tensor("p2a_" + name, shape, dt))
        wm = sb("wm", [128, 8, 2048], BF16)
        wgt = sb("wgt", [128, 8, 8], BF16)
        wgf = sb("wgf", [128, 8, 8], F32)
        stage = [sb("stg%d" % i, [128, 512], F32) for i in range(3)]
        ident_f = sb("identf", [128, 128], F32)
        tri_f = sb("trif", [128, 128], F32)
        ident_bf = sb("identb", [128, 128], BF16)
        mask_bf = sb("maskb", [128, 128], BF16)
        ones_bf = sb("onesb", [128, 128], BF16)
        ones4 = sb("ones4", [4, 128], F32)
        selc = sb("selc", [4, 4, 128], F32)
        flag = sb("flag", [128, 1], F32)
        cw = sb("cw", [128, 8, 4], F32)
        cb = sb("cb", [128, 8], F32)
        ibias = sb("ibias", [4, 1], F32)
        fbias = sb("fbias", [128, 4], F32)
        Cst = sb("Cst", [128, 4, 129], F32)
        mst = sb("mst", [4, 1], F32)
        ut = [sb("ut%d" % i, [128, 8, TT], BF16) for i in range(2)]
        pre = sb("pre", [128, 8, TT + 3], F32)
        cv = sb("cv", [128, 8, TT], F32)
        qT = sb("qT", [128, 4, TT], BF16)
        kT = sb("kT", [128, 4, TT], BF16)
        so = sb("so", [128, 4, TT], F32)
        hgt = [sb("hgt%d" % i, [128, 4, TT], BF16) for i in range(2)]
        vt = sb("vt", [128, 4, 129], BF16)
        kw = sb("kw", [128, 4, 128], BF16)
        pT = sb("pT", [128, 4, 128], BF16)
        Cb = sb("Cb", [128, 4, 128], BF16)
        nbc = sb("nbc", [128, 4, 128], BF16)
        thrS = sb("thrS", [128, 512], F32)
        dd = sb("dd", [128, 512], F32)
        rr = sb("rr", [128, 512], F32)
        hh = sb("hh", [128, 512], F32)
        fx = sb("fx", [128, 4], F32)
        lt = sb("lt", [128, 4], F32)
        nbr = sb("nbr", [4, 128], F32)
        gp = sb("gp", [4, 128], F32)
        wrow = sb("wrow", [4, 128], F32)
        trow = sb("trow", [4, 128], F32)
        gmax = sb("gmax", [4, 1], F32)
        Mv = sb("Mv", [4, 1], F32)
        negM = sb("negM", [4, 1], F32)
        av = sb("av", [4, 1], F32)
        da = sb("da", [4, 4], F32)
        wtok = sb("wtok", [128, 4], F32)
        abc = sb("abc", [128, 4], F32)
        B0 = pst("b0", [128, 512])
        B1 = pst("b1", [128, 512])
        B2 = pst("b2", [128, 512])
        B3 = pst("b3", [128, 512])
        B4 = pst("b4", [128, 1024], BF16)
        B5 = pst("b5", [128, 512])
        B6 = pst("b6", [128, 512])
        B7 = pst("b7", [128, 512])

        P.op('sp', lambda E: E.dma_start(out=ident_f[:], in_=T['c_ident'][:, :]), writes=['identf'], dsem="p2a_c0")
        P.op('sp', lambda E: E.dma_start(out=tri_f[:], in_=T['c_tri'][:, :]), writes=['trif'], dsem="p2a_c1")
        P.op('sp', lambda E: E.dma_start(out=flag[:], in_=T['c_flag'][:, :]), writes=['flag'], dsem="p2a_c2")
        P.op('sp', lambda E: E.dma_start(out=cw[:], in_=T['conv_w'][:, :, :]), writes=['cw'], dsem="p2a_c3")
        P.op('sp', lambda E: E.dma_start(out=cb[:], in_=T['conv_b'][:, :]), writes=['cb'], dsem="p2a_c4")
        P.op('sp', lambda E: E.dma_start(out=ibias[:], in_=T['i_bias'][:, :]), writes=['ibias'], dsem="p2a_c5")
        P.op('sp', lambda E: E.dma_start(out=fbias[:], in_=T['f_bias'][:, :]), writes=['fbias'], dsem="p2a_c6")
        w_in_r = T['w_in'].rearrange("(c p) n -> p c n", p=128)
        P.op('sp', lambda E: E.dma_start(out=wgf[:], in_=w_in_r[:, :, C_MI:C_MI + 8]), writes=['wgf'], dsem="p2a_c7")
        P.op('pool', lambda E: E.tensor_copy(wgt[:], wgf[:]), reads=['wgf'], writes=['wgt'])
        P.op('pool', lambda E: E.tensor_copy(ident_bf[:], ident_f[:]), reads=['identf'], writes=['identb'])
        P.op('pool', lambda E: E.tensor_copy(mask_bf[:], tri_f[:]), reads=['trif'], writes=['maskb'])
        P.op('pool', lambda E: E.memset(ones_bf[:], 1.0), writes=['onesb'])
        P.op('pool', lambda E: E.memset(ones4[:], 1.0), writes=['ones4'])
        P.op('pool', lambda E: E.memset(Cst[:], 0.0), writes=['Cst'])
        P.op('pool', lambda E: E.memset(mst[:], 0.0), writes=['mst'])
        P.op('pool', lambda E: E.memset(vt[:], 1.0), writes=['vt'])
        P.op('pool', lambda E: E.memset(pre[:], 0.0), writes=[('pre', c) for c in range(8)])
        for h in range(4):
            P.op('pool', lambda E, h=h: E.tensor_scalar(selc[:, h, :], ones4[:], ident_f[0:4, h:h + 1], None, op0=ALU.mult),
                 reads=['ones4', 'identf'], writes=['selc'])
        pieces = []
        for k in range(8):
            for q in range(4):
                pieces.append((wm[:, k, q * 512:(q + 1) * 512], w_in_r[:, k, C_MQ + q * 512:C_MQ + (q + 1) * 512], ('wm', k, q)))
        load_cast(P, stage, pieces)
        u_r = T['u'].rearrange("(c p) n -> p c n", p=128)
        hg_r = T['hg'].rearrange("(c p) n -> p c n", p=128)

        def load_u(i):
            d = i % 2
            P.op('sp', lambda E: E.dma_start(out=ut[d][:], in_=u_r[:, :, i * TT:(i + 1) * TT]), writes=[('ut', d)], dsem="p2a_ut%d" % d)

        load_u(t_start)
        for i in range(t_start, t_end):
            d = i % 2
            own = i >= own0
            if i + 1 < t_end:
                load_u(i + 1)
            fcs = ([(0, fc) for fc in range(4)] if (own or i == own0 - 1) else []) + [(1, fc) for fc in range(4)]
            for (kind, fc) in fcs:
                c8 = kind * 4 + fc
                mm_group(P, B0[:], [(wm[:, k, c8 * 128:(c8 + 1) * 128], ut[d][:, k, :]) for k in range(8)],
                         reads=[('wm', k, kind) for k in range(8)] + [('ut', d)], writes=['B0'])
                P.op('act', lambda E: E.copy(out=pre[:, c8, 3:TT + 3], in_=B0[:]), reads=['B0'], writes=[('pre', c8)])
            if own:
                for fc in range(4):
                    mm_group(P, B0[:], [(wm[:, k, 1536 + fc * 128:1536 + (fc + 1) * 128], ut[d][:, k, :]) for k in range(8)],
                             reads=[('wm', k, 3) for k in range(8)] + [('ut', d)], writes=['B0'])
                    P.op('act', lambda E: E.activation(out=so[:, fc, :], in_=B0[:], func=AF.Sigmoid), reads=['B0'], writes=[('so', fc)])
            for (kind, fc) in fcs:
                c8 = kind * 4 + fc
                eng = 'dve'
                P.op(eng, lambda E: E.tensor_scalar(cv[:, c8, :], pre[:, c8, 0:TT], cw[:, c8, 0:1], cb[:, c8:c8 + 1], op0=ALU.mult, op1=ALU.add),
                     reads=[('pre', c8), 'cw', 'cb'], writes=[('cv', c8)])
                for j in range(1, 4):
                    P.op(eng, lambda E: E.scalar_tensor_tensor(cv[:, c8, :], pre[:, c8, j:TT + j], cw[:, c8, j:j + 1], cv[:, c8, :], op0=ALU.mult, op1=ALU.add),
                         reads=[('pre', c8), ('cv', c8), 'cw'], writes=[('cv', c8)])
                P.op(eng, lambda E: E.tensor_copy(pre[:, c8, 0:3], pre[:, c8, TT:TT + 3]), reads=[('pre', c8)], writes=[('pre', c8)])
                if kind == 0:
                    P.op('act', lambda E: E.activation(out=cv[:, c8, :], in_=cv[:, c8, :], func=AF.Silu), reads=[('cv', c8)], writes=[('cv', c8)])
                    P.op('pool', lambda E: E.tensor_scalar(qT[:, fc, :], cv[:, c8, :], SC, None, op0=ALU.mult), reads=[('cv', c8)], writes=[('qT', fc)])
                else:
                    P.op('act', lambda E: E.activation(out=kT[:, fc, :], in_=cv[:, c8, :], func=AF.Silu), reads=[('cv', c8)], writes=[('kT', fc)])
            for ci in range(4):
                c0 = ci * 128
                cs = slice(c0, c0 + 128)
                mm_group(P, B1[:], [(ut[d][:, k, cs], wm[:, k, 1024:1536]) for k in range(8)],
                         reads=[('wm', k, 2) for k in range(8)] + [('ut', d)], writes=['B1'])
                mm_group(P, B2[:, 0:8], [(ut[d][:, k, cs], wgt[:, k, 0:8]) for k in range(8)], reads=['wgt', ('ut', d)], writes=['B2'])
                mm_group(P, B2[0:4, 8:136], [(wgt[:, k, 0:4], ut[d][:, k, cs]) for k in range(8)], reads=['wgt', ('ut', d)], writes=['B2'])
                P.op('act', lambda E: E.copy(out=vt[:, :, 0:128], in_=B1[:].rearrange("p (h e) -> p h e", h=4)), reads=['B1'], writes=['vt'])
                P.op('dve', lambda E: E.tensor_tensor(fx[:], B2[:, 4:8], fbias[:], op=ALU.add), reads=['B2', 'fbias'], writes=['fx'])
                P.op('act', lambda E: E.activation(out=fx[:], in_=fx[:], func=AF.Exp, scale=-1.0), reads=['fx'], writes=['fx'])
                P.op('act', lambda E: E.activation(out=lt[:], in_=fx[:], func=AF.Ln, bias=1.0, scale=1.0), reads=['fx'], writes=['lt'])
                mm_group(P, B3[0:4, 0:128], [(lt[:], tri_f[:])], reads=['lt', 'trif'], writes=['B3'])
                P.op('act', lambda E: E.copy(out=nbr[:], in_=B3[0:4, 0:128]), reads=['B3'], writes=['nbr'])
                P.op('dve', lambda E: E.scalar_tensor_tensor(gp[:], B2[0:4, 8:136], ibias[:, 0:1], nbr[:], op0=ALU.add, op1=ALU.add),
                     reads=['B2', 'ibias', 'nbr'], writes=['gp'])
                P.op('dve', lambda E: E.reduce_max(gmax[:], gp[:], axis=AX.X), reads=['gp'], writes=['gmax'])
                P.op('dve', lambda E: E.tensor_tensor(Mv[:], gmax[:], mst[:], op=ALU.max), reads=['gmax', 'mst'], writes=['Mv'])
                P.op('dve', lambda E: E.tensor_scalar(negM[:], Mv[:], -1.0, None, op0=ALU.mult), reads=['Mv'], writes=['negM'])
                P.op('act', lambda E: E.activation(out=wrow[:], in_=gp[:], func=AF.Exp, bias=negM[:, 0:1], scale=1.0), reads=['gp', 'negM'], writes=['wrow'])
                P.op('act', lambda E: E.activation(out=av[:], in_=mst[:], func=AF.Exp, bias=negM[:, 0:1], scale=1.0), reads=['mst', 'negM'], writes=['av'])
                if own:
                    P.op('act', lambda E: E.activation(out=trow[:], in_=nbr[:], func=AF.Exp, bias=negM[:, 0:1], scale=1.0), reads=['nbr', 'negM'], writes=['trow'])
                P.op('dve', lambda E: E.tensor_tensor(mst[:], Mv[:], nbr[:, 127:128], op=ALU.subtract), reads=['Mv', 'nbr'], writes=['mst'])
                mm_group(P, B3[:, 128:132], [(wrow[:], ident_f[0:4, 0:4])], reads=['wrow', 'identf'], writes=['B3'])
                P.op('dve', lambda E: E.tensor_scalar(da[:], ident_f[0:4, 0:4], av[:, 0:1], None, op0=ALU.mult), reads=['av', 'identf'], writes=['da'])
                mm_group(P, B3[:, 136:140], [(ones4[:], da[:])], reads=['ones4', 'da'], writes=['B3'])
                if own:
                    P.op('dve', lambda E: E.tensor_copy(wtok[:], B3[:, 128:132]), reads=['B3'], writes=['wtok'])
                else:
                    P.op('dve', lambda E: E.tensor_scalar(wtok[:], B3[:, 128:132], flag[:, 0:1], None, op0=ALU.mult), reads=['B3', 'flag'], writes=['wtok'])
                P.op('dve', lambda E: E.tensor_copy(abc[:], B3[:, 136:140]), reads=['B3'], writes=['abc'])
                for h in range(4):
                    P.op('pool', lambda E, h=h: E.tensor_scalar(Cst[:, h, :], Cst[:, h, :], abc[:, h:h + 1], None, op0=ALU.mult), reads=['Cst', 'abc'], writes=['Cst'])
                def trf(E):
                    ins = None
                    for h in range(4):
                        ins = E.transpose(B4[:, h * 128:(h + 1) * 128], kT[:, h, cs], ident_bf[:])
                    return ins
                P.op('pe', trf, reads=[('kT', h) for h in range(4)] + ['identb'], writes=['B4'])
                for h in range(4):
                    P.op('dve', lambda E, h=h: E.tensor_scalar(kw[:, h, :], B4[:, h * 128:(h + 1) * 128], wtok[:, h:h + 1], None, op0=ALU.mult),
                         reads=['B4', 'wtok'], writes=['kw'])
                if own:
                    P.op('act', lambda E: E.copy(out=Cb[:], in_=Cst[:, :, 0:128]), reads=['Cst'], writes=['Cb'])
                    for h in range(4):
                        P.op('pool', lambda E, h=h: E.tensor_scalar(nbc[:, h, :], ones_bf[:], Cst[:, h, 128:129], None, op0=ALU.mult), reads=['Cst', 'onesb'], writes=['nbc'])
                    def qkf(E):
                        ins = None
                        for h in range(4):
                            ins = E.matmul(B5[:, h * 128:(h + 1) * 128], kT[:, h, cs], qT[:, h, cs], start=True, stop=True)
                        return ins
                    P.op('pe', qkf, reads=[('kT', h) for h in range(4)] + [('qT', h) for h in range(4)], writes=['B5'])
                    for h in range(4):
                        P.op('dve', lambda E, h=h: E.scalar_tensor_tensor(pT[:, h, :], B5[:, h * 128:(h + 1) * 128], wtok[:, h:h + 1], mask_bf[:], op0=ALU.mult, op1=ALU.mult),
                             reads=['B5', 'wtok', 'maskb'], writes=['pT'])
                    def numf(E):
                        ins = None
                        for h in range(4):
                            E.matmul(B6[:, h * 128:(h + 1) * 128], Cb[:, h, :], qT[:, h, cs], start=True, stop=False)
                            ins = E.matmul(B6[:, h * 128:(h + 1) * 128], vt[:, h, 0:128], pT[:, h, :], start=False, stop=True)
                        return ins
                    P.op('pe', numf, reads=['Cb', 'vt', 'pT'] + [('qT', h) for h in range(4)], writes=['B6'])

                    def denf(E):
                        ins = None
                        for h in range(4):
                            E.matmul(B1[:, h * 128:(h + 1) * 128], nbc[:, h, :], qT[:, h, cs], start=True, stop=False)
                            ins = E.matmul(B1[:, h * 128:(h + 1) * 128], ones_bf[:], pT[:, h, :], start=False, stop=True)
                        return ins
                    P.op('pe', denf, reads=['nbc', 'onesb', 'pT'] + [('qT', h) for h in range(4)], writes=['B1'])

                    def thrf(E):
                        ins = None
                        for h in range(4):
                            ins = E.matmul(B0[:, h * 128:(h + 1) * 128], selc[:, h, :], trow[:], start=True, stop=True)
                        return ins
                    P.op('pe', thrf, reads=['selc', 'trow'], writes=['B0'])
                    P.op('act', lambda E: E.copy(out=thrS[:], in_=B0[:]), reads=['B0'], writes=['thrS'])
                    P.op('dve', lambda E: E.tensor_tensor(dd[:], B1[:], thrS[:], op=ALU.max), reads=['B1', 'thrS'], writes=['dd'])
                    P.op('dve', lambda E: E.scalar_tensor_tensor(dd[:], B1[:], -1.0, dd[:], op0=ALU.mult, op1=ALU.max), reads=['B1', 'dd'], writes=['dd'])
                    P.op('dve', lambda E: E.reciprocal(rr[:], dd[:]), reads=['dd'], writes=['rr'])
                    P.op('dve', lambda E: E.tensor_tensor(hh[:], B6[:], rr[:], op=ALU.mult), reads=['B6', 'rr'], writes=['hh'])
                    P.op('pool', lambda E: E.tensor_tensor(hgt[d][:, :, cs], hh[:].rearrange("p (h t) -> p h t", h=4), so[:, :, cs], op=ALU.mult),
                         reads=['hh'] + [('so', fc) for fc in range(4)], writes=[('hgt', d)])
                def dcf(E):
                    ins = None
                    for h in range(4):
                        bank = B5 if h < 2 else B7
                        ins = E.matmul(bank[:, (h % 2) * 129:(h % 2 + 1) * 129], kw[:, h, :], vt[:, h, :], start=True, stop=True)
                    return ins
                P.op('pe', dcf, reads=['kw', 'vt'], writes=['B5', 'B7'])
                P.op('dve', lambda E: E.tensor_tensor(Cst[:, 0:2, :], Cst[:, 0:2, :], B5[:, 0:258].rearrange("p (h e) -> p h e", h=2), op=ALU.add), reads=['Cst', 'B5'], writes=['Cst'])
                P.op('dve', lambda E: E.tensor_tensor(Cst[:, 2:4, :], Cst[:, 2:4, :], B7[:, 0:258].rearrange("p (h e) -> p h e", h=2), op=ALU.add), reads=['Cst', 'B7'], writes=['Cst'])
            if own:
                io = i - own0
                P.op('sp', lambda E: E.dma_start(out=hg_r[:, :, io * TT:(io + 1) * TT], in_=hgt[d][:]), reads=[('hgt', d)], dsem="p2a_hg%d" % d)
        P.barrier()


def attn_phase(P, nc, T):
    TT = 512
    L = 6144
    SCALE = 128 ** -0.5
    slots = int(os.environ.get('KSLOTS', 4))
    with ExitStack() as ph:
        def sb(name, shape, dt):
            return ph.enter_context(nc.sbuf_tensor("p2b_" + name, shape, dt))

        def pst(name, shape, dt=F32):
            return ph.enter_context(nc.psum_tensor("p2b_" + name, shape, dt))
        ures = sb("ures", [128, 8, L], BF16)
        wq = sb("wq", [128, 8, 128], BF16)
        wk = sb("wk", [128, 8, 128], BF16)
        wv = sb("wv", [128, 8, 128], BF16)
        stage = [sb("stg%d" % i, [128, 128], F32) for i in range(4)]
        qT = sb("qT", [128, OWN], BF16)
        kT = sb("kT", [128, L], BF16)
        vt = sb("vt", [128, 48, 128], BF16)
        acc = sb("acc", [128, 2, OWN], F32)
        ones_bf = sb("onesb", [128, 128], BF16)
        fones = sb("fones", [128, 128], BF16)
        mask2 = sb("mask2", [128, 512], BF16)
        trif = sb("trif", [128, 128], F32)
        tritf = sb("tritf", [128, 128], F32)
        flag = sb("flag", [128, 1], F32)
        gq = sb("gq", [128, 1], F32)
        gk = sb("gk", [128, 1], F32)
        rm = sb("rm", [32, 32], F32)
        sq = [sb("sq%d" % i, [128, TT], BF16) for i in range(2)]
        tmp = [sb("tmp%d" % i, [128, TT], F32) for i in range(2)]
        rstd = [sb("rstd%d" % i, [128, TT], F32) for i in range(2)]
        qn = [sb("qn%d" % i, [128, TT], F32) for i in range(2)]
        t2 = [sb("t2%d" % i, [32, TT], F32) for i in range(2)]
        epsc = sb("epsc", [128, 1], F32)
        qnb = [sb("qnb%d" % i, [32, TT], BF16) for i in range(2)]
        rmb = sb("rmb", [32, 32], BF16)
        cosb = [sb("cos%d" % i, [32, TT], F32) for i in range(2)]
        sinb = [sb("sin%d" % i, [32, TT], F32) for i in range(2)]
        pT = [sb("pT%d" % i, [128, 512], BF16) for i in range(2)]
        atto = [sb("atto%d" % i, [128, TT], BF16) for i in range(2)]
        rden = sb("rden", [128, TT], F32)
        BK = [pst("bk%d" % i, [128, 512]) for i in range(8)]
        BSC = [BK[0], BK[1]]
        BN = [BK[2], BK[3]]

        P.op('sp', lambda E: E.dma_start(out=trif[:], in_=T['c_tri'][:, :]), writes=['trif'], dsem="p2b_c0")
        P.op('sp', lambda E: E.dma_start(out=tritf[:], in_=T['c_trit'][:, :]), writes=['tritf'], dsem="p2b_c1")
        P.op('sp', lambda E: E.dma_start(out=flag[:], in_=T['c_flag'][:, :]), writes=['flag'], dsem="p2b_c2")
        P.op('sp', lambda E: E.dma_start(out=gq[:], in_=T['q_gain'][:, :]), writes=['gq'], dsem="p2b_c3")
        P.op('sp', lambda E: E.dma_start(out=gk[:], in_=T['k_gain'][:, :]), writes=['gk'], dsem="p2b_c4")
        P.op('sp', lambda E: E.dma_start(out=rm[:], in_=T['c_rm'][:, :]), writes=['rm'], dsem="p2b_c5")
        P.op('pool', lambda E: E.memset(ones_bf[:], 1.0), writes=['onesb'])
        P.op('pool', lambda E: E.memset(epsc[:], EPS), writes=['epsc'])
        P.op('pool', lambda E: E.tensor_copy(rmb[:], rm[:]), reads=['rm'], writes=['rmb'])
        P.op('pool', lambda E: E.tensor_scalar(fones[:], ones_bf[:], flag[:, 0:1], None, op0=ALU.mult), reads=['onesb', 'flag'], writes=['fones'])
        for qb in range(2):
            P.op('pool', lambda E, qb=qb: E.tensor_copy(mask2[:, (qb * 2) * 128:(qb * 2 + 1) * 128], tritf[:]), reads=['tritf'], writes=['mask2'])
            P.op('pool', lambda E, qb=qb: E.tensor_copy(mask2[:, (qb * 2 + 1) * 128:(qb * 2 + 2) * 128], trif[:]), reads=['trif'], writes=['mask2'])
        u_r = T['u'].rearrange("(c p) n -> p c n", p=128)
        for tl in range(12):
            P.op('sp', lambda E, tl=tl: E.dma_start(out=ures[:, :, tl * TT:(tl + 1) * TT], in_=u_r[:, :, 2048 + tl * TT:2048 + (tl + 1) * TT]),
                 writes=[('ures', tl)], dsem="p2b_u%d" % tl)
            if tl % 4 == 3:
                pass
        ures_all = [('ures', tl) for tl in range(12)]
        w_in_r = T['w_in'].rearrange("(c p) n -> p c n", p=128)
        att_r = T['att'].rearrange("(c p) n -> p c n", p=128)
        cnt = [0]

        def prep(kind, tl):
            l0 = tl * TT
            w, wkey, gain, gkey = (wq, 'wq', gq, 'gq') if kind == 'q' else (wk, 'wk', gk, 'gk')
            dst = qT[:, l0 - 2048:l0 - 2048 + TT] if kind == 'q' else kT[:, l0:l0 + TT]
            dkey = (kind + 'T', tl)
            cs = cnt[0] % 2
            cnt[0] += 1
            P.op('sp', lambda E: E.dma_start(out=cosb[cs][:], in_=T['c_cos'][:, l0:l0 + TT]), writes=[('cos', cs)], dsem="p2b_cos%d" % cs)
            P.op('sp', lambda E: E.dma_start(out=sinb[cs][:], in_=T['c_sin'][:, l0:l0 + TT]), writes=[('sin', cs)], dsem="p2b_sin%d" % cs)
            BQ, BS, BR = BK[0 + cs], BK[4 + cs], BK[6 + cs]
            kq, ks_, kr = ('bk', 0 + cs), ('bk', 4 + cs), ('bk', 6 + cs)
            mm_group(P, BQ[:], [(w[:, k, :], ures[:, k, l0:l0 + TT]) for k in range(8)], reads=[wkey, ('ures', tl)], writes=[kq])
            P.op('act', lambda E: E.activation(out=sq[cs][:], in_=BQ[:], func=AF.Square), reads=[kq], writes=[('sq', cs)])
            mm_group(P, BS[:], [(ones_bf[:], sq[cs][:])], reads=['onesb', ('sq', cs)], writes=[ks_])
            P.op('act', lambda E: E.activation(out=tmp[cs][:], in_=BS[:], func=AF.Ln, bias=epsc[:, 0:1], scale=1.0 / 128), reads=[ks_, 'epsc'], writes=[('tmp', cs)])
            P.op('act', lambda E: E.activation(out=rstd[cs][:], in_=tmp[cs][:], func=AF.Exp, scale=-0.5), reads=[('tmp', cs)], writes=[('rstd', cs)])
            P.op('dve', lambda E: E.scalar_tensor_tensor(qn[cs][:], BQ[:], gain[:, 0:1], rstd[cs][:], op0=ALU.mult, op1=ALU.mult), reads=[kq, gkey, ('rstd', cs)], writes=[('qn', cs)])
            P.op('dve', lambda E: E.tensor_copy(qnb[cs][:], qn[cs][0:32, :]), reads=[('qn', cs)], writes=[('qnb', cs)])
            mm_group(P, BR[0:32, :], [(rmb[:], qnb[cs][:])], reads=['rmb', ('qnb', cs)], writes=[kr])
            P.op('dve', lambda E: E.tensor_tensor(qn[cs][0:32, :], qn[cs][0:32, :], cosb[cs][:], op=ALU.mult), reads=[('qn', cs), ('qnb', cs), ('cos', cs)], writes=[('qn', cs)])
            P.op('dve', lambda E: E.tensor_tensor(t2[cs][:], BR[0:32, :], sinb[cs][:], op=ALU.mult), reads=[kr, ('sin', cs)], writes=[('t2', cs)])
            P.op('dve', lambda E: E.tensor_tensor(dst[0:32, :], qn[cs][0:32, :], t2[cs][:], op=ALU.add), reads=[('qn', cs), ('t2', cs)], writes=[dkey])
            P.op('dve', lambda E: E.tensor_copy(dst[32:64, :], qn[cs][32:64, :]), reads=[('qn', cs)], writes=[(kind + 'Tc', tl)])
            P.op('act', lambda E: E.copy(out=dst[64:128, :], in_=qn[cs][64:128, :]), reads=[('qn', cs)], writes=[(kind + 'Tb', tl)])

        for s in range(slots):
            for g in range(3):
                d = DILS[g]
                J = OWN // (128 * d)
                hk = g * 4 + s
                pieces = []
                for (wt, nm, c0) in ((wq, 'wq', C_AQ), (wk, 'wk', C_AK), (wv, 'wv', C_AV)):
                    for k in range(8):
                        pieces.append((wt[:, k, :], w_in_r[:, k, c0 + hk * 128:c0 + (hk + 1) * 128], nm))
                load_cast(P, stage, pieces, engs=('pool',))
                ktl0 = 0 if d == 16 else 3
                ktiles = list(range(ktl0, 12))
                for tl in ktiles:
                    prep('k', tl)
                for tl in range(4, 12):
                    prep('q', tl)
                kkeys = [('kT', tl) for tl in ktiles] + [('kTb', tl) for tl in ktiles] + [('kTc', tl) for tl in ktiles]
                qkeys = [('qT', tl) for tl in range(4, 12)] + [('qTb', tl) for tl in range(4, 12)] + [('qTc', tl) for tl in range(4, 12)]
                nblk = d * (J + 1)
                blks = [(r, j) for r in range(d) for j in range(-1, J)]
                for b0 in range(0, nblk, 4):
                    grp = blks[b0:b0 + 4]

                    vb = (b0 // 4) % 2
                    BVb = BK[2 + vb]

                    def vf(E, grp=grp, BVb=BVb):
                        ins = None
                        for qi, (r, j) in enumerate(grp):
                            u0 = 2048 // d + 128 * j
                            for k in range(8):
                                lhs = ures[:, k, :].rearrange("p (u d) -> p d u", d=d)[:, r, u0:u0 + 128]
                                ins = E.matmul(BVb[:, qi * 128:(qi + 1) * 128], lhs, wv[:, k, :], start=(k == 0), stop=(k == 7))
                        return ins
                    P.op('pe', vf, reads=['wv'] + ures_all, writes=[('bk', 2 + vb)])
                    n = len(grp)
                    P.op('act' if vb == 0 else 'dve', (lambda E, b0=b0, n=n, BVb=BVb: E.copy(out=vt[:, b0:b0 + n, :], in_=BVb[:, 0:n * 128].rearrange("p (b e) -> p b e", e=128))) if vb == 0 else
                         (lambda E, b0=b0, n=n, BVb=BVb: E.tensor_copy(vt[:, b0:b0 + n, :], BVb[:, 0:n * 128].rearrange("p (b e) -> p b e", e=128))),
                         reads=[('bk', 2 + vb)], writes=[('vt', b0)])
                kview = kT[:, :].rearrange("p (u d) -> p d u", d=d)
                qview = qT[:, :].rearrange("p (u d) -> p d u", d=d)
                accv = acc[:, :, :].rearrange("p n (u d) -> p n d u", d=d)
                it = 0
                for r in range(d):
                    for jp in range(J // 2):
                        j0 = 2 * jp
                        b = it % 2
                        it += 1

                        def sf(E, r=r, j0=j0, b=b):
                            ins = None
                            for qb in range(2):
                                j = j0 + qb
                                qa = qview[:, r, 128 * j:128 * j + 128]
                                for pc in range(2):
                                    jj = j - 1 + pc
                                    u0 = 2048 // d + 128 * jj
                                    ka = kview[:, r, u0:u0 + 128]
                                    ins = E.matmul(BSC[b][:, (qb * 2 + pc) * 128:(qb * 2 + pc + 1) * 128], ka, qa, start=True, stop=True)
                            return ins
                        P.op('pe', sf, reads=kkeys + qkeys, writes=[('bk', b)])
                        P.op('act', lambda E, b=b: E.activation(out=pT[b][:], in_=BSC[b][:], func=AF.Exp, scale=SCALE), reads=[('bk', b)], writes=[('pT', b)])
                        P.op('pool' if b == 0 else 'dve', lambda E, b=b: E.tensor_tensor(pT[b][:], pT[b][:], mask2[:], op=ALU.mult), reads=[('pT', b), 'mask2'], writes=[('pT', b)])

                        def nf(E, r=r, j0=j0, b=b):
                            ins = None
                            for qb in range(2):
                                j = j0 + qb
                                bp = r * (J + 1) + j
                                bc = bp + 1
                                pp = pT[b][:, (qb * 2) * 128:(qb * 2 + 1) * 128]
                                pcur = pT[b][:, (qb * 2 + 1) * 128:(qb * 2 + 2) * 128]
                                E.matmul(BN[b][:, qb * 128:(qb + 1) * 128], vt[:, bp, :], pp, start=True, stop=False)
                                E.matmul(BN[b][:, qb * 128:(qb + 1) * 128], vt[:, bc, :], pcur, start=False, stop=True)
                                E.matmul(BN[b][:, (2 + qb) * 128:(3 + qb) * 128], (fones if j == 0 else ones_bf)[:], pp, start=True, stop=False)
                                ins = E.matmul(BN[b][:, (2 + qb) * 128:(3 + qb) * 128], ones_bf[:], pcur, start=False, stop=True)
                            return ins
                        P.op('pe', nf, reads=[('vt', (bb // 4) * 4) for bb in (r * (J + 1) + j0, r * (J + 1) + j0 + 1, r * (J + 1) + j0 + 2)] + [('pT', b), 'onesb', 'fones'], writes=[('bk', 2 + b)])
                        av = accv[:, :, r, 128 * j0:128 * j0 + 256]
                        bnv = BN[b][:].rearrange("p (n x) -> p n x", n=2)
                        if g == 0:
                            P.op('act', lambda E, av=av, bnv=bnv: E.copy(out=av, in_=bnv), reads=[('bk', 2 + b)], writes=['acc'])
                        else:
                            P.op('dve', lambda E, av=av, bnv=bnv: E.tensor_tensor(av, av, bnv, op=ALU.add), reads=[('bk', 2 + b), 'acc'], writes=['acc'])
            for tl in range(8):
                o = tl % 2
                P.op('dve', lambda E: E.reciprocal(rden[:], acc[:, 1, tl * TT:(tl + 1) * TT]), reads=['acc'], writes=['rden'])
                P.op('pool', lambda E: E.tensor_tensor(atto[o][:], acc[:, 0, tl * TT:(tl + 1) * TT], rden[:], op=ALU.mult), reads=['acc', 'rden'], writes=[('atto', o)])
                P.op('sp', lambda E: E.dma_start(out=att_r[:, s, tl * TT:(tl + 1) * TT], in_=atto[o][:]), reads=[('atto', o)], dsem="p2b_ao%d" % o)
        P.barrier()


def build(debug=0):
    nc = bass.Bass("TRN2", target_bir_lowering=False)
    T = {}

    def din(name, shape, dt=F32):
        T[name] = nc.dram_tensor(name, shape, dt, kind="ExternalInput").ap()

    def scratch(name, shape, dt):
        kind = {"kind": "ExternalOutput"} if debug else {}
        T[name] = nc.dram_tensor(name, shape, dt, **kind).ap()
    din('xT', [D, NTOK])
    din('pT', [256, OWN])
    din('ffn1_norm', [128, 8]); din('mix_norm', [128, 8]); din('ffn2_norm', [128, 8]); din('ple_norm', [128, 8])
    din('ffn1_w_in', [D, 2 * DFF]); din('ffn1_w_out', [DFF, D])
    din('ffn2_w_in', [D, 2 * DFF]); din('ffn2_w_out', [DFF, D])
    din('w_in', [D, DIN])
    din('w_up_att', [512, D]); din('w_up_mlstm', [512, D]); din('w_out', [D, D])
    din('w_ple_gate', [D, D]); din('w_ple_proj', [256, D])
    din('conv_w', [128, 8, 4]); din('conv_b', [128, 8]); din('i_bias', [4, 1]); din('f_bias', [128, 4])
    din('q_gain', [128, 1]); din('k_gain', [128, 1])
    din('c_ident', [128, 128]); din('c_tri', [128, 128]); din('c_trit', [128, 128]); din('c_flag', [128, 1])
    din('c_rm', [32, 32]); din('c_cos', [32, 6144]); din('c_sin', [32, 6144])
    T['outT'] = nc.dram_tensor('outT', [D, OWN], F32, kind="ExternalOutput").ap()
    scratch('h1', [D, OWN], F32)
    scratch('u', [D, NTOK], BF16)
    scratch('att', [512, OWN], BF16)
    scratch('hg', [512, OWN], BF16)
    scratch('h2', [D, OWN], F32)
    stages = os.environ.get('KSTAGES', '1abcd')
    with ExitStack() as es:
        P = Prog(nc, es)
        if '1' in stages:
            ffn_phase(P, nc, T, 1)
        if 'a' in stages:
            mlstm_phase(P, nc, T)
        if 'b' in stages:
            attn_phase(P, nc, T)
        if 'c' in stages:
            merge_phase(P, nc, T)
        if 'd' in stages:
            ffn_phase(P, nc, T, 2)
    return nc


def _chunk_major(v):
    return np.ascontiguousarray(v.reshape(-1, 128).T).astype(np.float32)


def make_in_maps(inputs):
    x = np.asarray(inputs['x'], dtype=np.float32)
    p = np.asarray(inputs['p'], dtype=np.float32)[0]
    shared = {
        'ffn1_norm': _chunk_major(inputs['ffn1_norm'][0]), 'mix_norm': _chunk_major(inputs['mix_norm'][0]),
        'ffn2_norm': _chunk_major(inputs['ffn2_norm'][0]), 'ple_norm': _chunk_major(inputs['ple_norm'][0]),
        'ffn1_w_in': np.ascontiguousarray(inputs['ffn1_w_in'][0]), 'ffn1_w_out': np.ascontiguousarray(inputs['ffn1_w_out'][0]),
        'ffn2_w_in': np.ascontiguousarray(inputs['ffn2_w_in'][0]), 'ffn2_w_out': np.ascontiguousarray(inputs['ffn2_w_out'][0]),
        'w_in': np.ascontiguousarray(inputs['w_in'][0]),
        'w_up_att': np.ascontiguousarray(inputs['w_up_att'][0]), 'w_up_mlstm': np.ascontiguousarray(inputs['w_up_mlstm'][0]),
        'w_out': np.ascontiguousarray(inputs['w_out'][0]),
        'w_ple_gate': np.ascontiguousarray(inputs['w_ple_gate'][0]), 'w_ple_proj': np.ascontiguousarray(inputs['w_ple_proj'][0]),
    }
    cw = np.asarray(inputs['conv_w'][0], np.float32)
    shared['conv_w'] = np.ascontiguousarray(cw.reshape(4, 8, 128).transpose(2, 1, 0))
    shared['conv_b'] = _chunk_major(inputs['conv_b'][0])
    shared['i_bias'] = np.asarray(inputs['i_bias'][0], np.float32).reshape(4, 1).copy()
    shared['f_bias'] = np.ascontiguousarray(np.broadcast_to(np.asarray(inputs['f_bias'][0], np.float32)[None, :], (128, 4)))
    shared['q_gain'] = np.asarray(inputs['q_gain'][0], np.float32).reshape(128, 1).copy()
    shared['k_gain'] = np.asarray(inputs['k_gain'][0], np.float32).reshape(128, 1).copy()
    shared['c_ident'] = np.eye(128, dtype=np.float32)
    tri = np.triu(np.ones((128, 128), np.float32))
    shared['c_tri'] = tri
    shared['c_trit'] = np.ascontiguousarray(tri.T)
    rmm = np.zeros((32, 32), np.float32)
    for m_ in range(32):
        rmm[(m_ + 16) % 32, m_] = 1.0
    shared['c_rm'] = rmm
    half = 16
    inv_freq = (1.0 / (np.float32(500000.0) ** (np.arange(half, dtype=np.float32) / np.float32(half)))).astype(np.float32)
    maps = []
    for c in range(8):
        b, h = c // 2, c % 2
        xT = np.zeros((D, NTOK), np.float32)
        xT[:, OWN:] = x[b, h * OWN:(h + 1) * OWN].T
        if h == 1:
            xT[:, :OWN] = x[b, :OWN].T
        m = dict(shared)
        m['xT'] = xT
        m['pT'] = np.ascontiguousarray(p[b, h * OWN:(h + 1) * OWN].T)
        m['c_flag'] = np.full((128, 1), float(h), np.float32)
        pos = (np.arange(2048, 8192) - 4096 + 4096 * h).astype(np.float32)
        ang = (pos[None, :] * inv_freq[:, None]).astype(np.float32)
        cs_, sn_ = np.cos(ang).astype(np.float32), np.sin(ang).astype(np.float32)
        m['c_cos'] = np.ascontiguousarray(np.concatenate([cs_, cs_], axis=0))
        m['c_sin'] = np.ascontiguousarray(np.concatenate([-sn_, sn_], axis=0))
        maps.append(m)
    return maps


def kernel(**inputs):
    nc = build(0)
    maps = make_in_maps(inputs)
    res = run_bass_kernel_spmd(nc, maps, core_ids=list(range(8)))
    out = np.empty((4, 8192, D), np.float32)
    for c in range(8):
        b, h = c // 2, c % 2
        out[b, h * OWN:(h + 1) * OWN] = res.results[c]['outT'].T
    return out
```

```python
import os
import numpy as np
from contextlib import ExitStack
import concourse.bass as bass
import concourse.mybir as mybir
from concourse.bass_utils import run_bass_kernel_spmd

F32 = mybir.dt.float32
BF16 = mybir.dt.bfloat16
AF = mybir.ActivationFunctionType
ALU = mybir.AluOpType
AX = mybir.AxisListType

D = 1024
DFF = 2816
NTOK = 8192
OWN = 4096
DIN = 8712
EPS = 1e-6
C_AQ, C_AK, C_AV = 0, 1536, 3072
C_MQ, C_MK, C_MV, C_MO, C_MI, C_MF, C_GA, C_GB = 4608, 5120, 5632, 6144, 6656, 6660, 6664, 7688
DILS = (1, 4, 16)


class Sem:
    def __init__(self, h):
        self.h = h
        self.n = 0


class Prog:
    ENG = ('pe', 'act', 'dve', 'pool', 'sp')

    def __init__(self, nc, es):
        self.nc, self.es = nc, es
        self.e = {'pe': nc.tensor, 'act': nc.scalar, 'dve': nc.vector, 'pool': nc.gpsimd, 'sp': nc.sync}
        self.prog = {}
        self.seen = {k: {} for k in self.ENG}
        self.lastw = {}
        self.rd = {}
        self.nsem = 0
        self.allsems = []
        self.dsems = {}
        self.new_epoch()

    def mksem(self):
        self.nsem += 1
        s = Sem(self.es.enter_context(self.nc.semaphore("s%d" % self.nsem)))
        self.allsems.append(s)
        return s

    def new_epoch(self):
        for k in self.ENG:
            self.prog[k] = self.mksem()

    def dsem(self, name):
        if name not in self.dsems:
            self.dsems[name] = self.mksem()
        return self.dsems[name]

    def op(self, eng, fn, reads=(), writes=(), dsem=None):
        need = {}

        def add(t):
            if t is None:
                return
            sem, v = t
            if need.get(sem, 0) < v:
                need[sem] = v
        for k in reads:
            add(self.lastw.get(k))
        for k in writes:
            add(self.lastw.get(k))
            for sem, v in self.rd.get(k, {}).items():
                add((sem, v))
        E = self.e[eng]
        for sem, v in need.items():
            if eng == 'pe' and sem is self.prog['pe']:
                continue
            if self.seen[eng].get(sem, 0) >= v:
                continue
            E.wait_ge(sem.h, v)
            self.seen[eng][sem] = v
        ins = fn(E)
        if dsem is not None:
            sem = self.dsem(dsem) if isinstance(dsem, str) else dsem
            sem.n += 16
            ins.then_inc(sem.h, 16)
        else:
            sem = self.prog[eng]
            sem.n += 1
            ins.then_inc(sem.h, 1)
        tok = (sem, sem.n)
        for k in reads:
            d = self.rd.setdefault(k, {})
            if d.get(sem, 0) < sem.n:
                d[sem] = sem.n
        for k in writes:
            self.lastw[k] = tok
            self.rd[k] = {}
        return tok

    def barrier(self):
        for eng in self.ENG:
            E = self.e[eng]
            for sem in self.allsems:
                if sem.n > self.seen[eng].get(sem, 0):
                    E.wait_ge(sem.h, sem.n)
                    self.seen[eng][sem] = sem.n
        self.lastw = {}
        self.rd = {}
        self.new_epoch()


def mm_group(P, out, pairs, reads, writes):
    n = len(pairs)

    def fn(E):
        ins = None
        for i, (l, r) in enumerate(pairs):
            ins = E.matmul(out, l, r, start=(i == 0), stop=(i == n - 1))
        return ins
    return P.op('pe', fn, reads=reads, writes=writes)


def load_w_groups(P, dst, src, ngroups, nj, key, eng='pool'):
    bounds = [round(i * nj / ngroups) for i in range(ngroups + 1)]
    for g in range(ngroups):
        j0, j1 = bounds[g], bounds[g + 1]
        if j1 == j0:
            continue
        ks = [(key, j) for j in range(j0, j1)]
        P.op(eng, lambda E, j0=j0, j1=j1: E.dma_start(out=dst[:, :, j0 * 128:j1 * 128], in_=src[:, :, j0 * 128:j1 * 128]),
             writes=ks, dsem="%s_g%d" % (key, g))


def load_cast(P, stage, pieces, engs=('pool', 'act')):
    for (dst, src, key) in pieces:
        i = P.lc_i = getattr(P, 'lc_i', -1) + 1
        sl = i % len(stage)
        n = dst.shape[-1]
        st = stage[sl][:, 0:n]
        P.op('sp', lambda E: E.dma_start(out=st, in_=src), writes=[('stg', sl)], dsem="stg%d" % sl)
        eng = engs[i % len(engs)]
        if eng == 'act':
            P.op('act', lambda E: E.copy(out=dst, in_=st), reads=[('stg', sl)], writes=[key])
        else:
            P.op(eng, lambda E: E.tensor_copy(dst, st), reads=[('stg', sl)], writes=[key])


def rms_stats(P, nc, src_aps, skeys, sq, sqk, ones_bf, ps_ap, psk, tmp, tmpk, rstd, rstdk, inv_n, eps_ap=None):
    nchunk = len(src_aps)
    for c in range(nchunk):
        s = sq[c % len(sq)]
        sk = sqk[c % len(sq)]
        P.op('act', lambda E, s=s, c=c: E.activation(out=s, in_=src_aps[c], func=AF.Square),
             reads=[skeys[c]], writes=[sk])
        P.op('pe', lambda E, s=s, c=c: E.matmul(ps_ap, ones_bf, s, start=(c == 0), stop=(c == nchunk - 1)),
             reads=[sk, 'ones'], writes=[psk])
    P.op('act', lambda E: E.activation(out=tmp, in_=ps_ap, func=AF.Ln, bias=eps_ap, scale=inv_n), reads=[psk, 'epsc'], writes=[tmpk])
    P.op('act', lambda E: E.activation(out=rstd, in_=tmp, func=AF.Exp, scale=-0.5), reads=[tmpk], writes=[rstdk])


def ffn_phase(P, nc, T, which):
    TT = 256
    first = (which == 1)
    ntiles = (NTOK if first else OWN) // TT
    ntiles = int(os.environ.get('KNT', ntiles))
    own0 = (NTOK - OWN) // TT if first else 0
    w_in = T['ffn1_w_in'] if first else T['ffn2_w_in']
    w_out = T['ffn1_w_out'] if first else T['ffn2_w_out']
    src = T['xT'] if first else T['h2']
    with ExitStack() as ph:
        def sb(name, shape, dt):
            return ph.enter_context(nc.sbuf_tensor("p%d_%s" % (which, name), shape, dt))

        def pst(name, shape, dt=F32):
            return ph.enter_context(nc.psum_tensor("p%d_ps_%s" % (which, name), shape, dt))
        w1 = sb("w1", [128, 8, 2 * DFF], BF16)
        w2 = sb("w2", [128, 22, D], BF16)
        g1 = sb("g1", [128, 8], F32)
        g2 = sb("g2", [128, 8], F32)
        ones_bf = sb("ones", [128, 128], BF16)
        epsc = sb("epsc", [128, 1], F32)
        xh = [sb("xh%d" % i, [128, 8, TT], F32) for i in range(3)]
        xn = [sb("xn%d" % i, [128, 8, TT], BF16) for i in range(2)]
        un = [sb("un%d" % i, [128, 8, TT], BF16) for i in range(2)] if first else xn
        unk = 'un' if first else 'xn'
        sq = [sb("sq%d" % i, [128, TT], BF16) for i in range(2)]
        act = sb("act", [128, 22, TT], BF16)
        sil = [sb("sil%d" % i, [128, TT], BF16) for i in range(2)]
        tmp = [sb("tmp%d" % i, [128, TT], F32) for i in range(2 if first else 1)]
        rstd = [sb("rstd%d" % i, [128, TT], F32) for i in range(2)]
        stage = [sb("stg%d" % i, [128, 512], F32) for i in range(3 if first else 2)]
        ps_ss = pst("ss", [128, 512])
        ps_a = [pst("a%d" % i, [128, 512]) for i in range(2)]
        ps_o = [pst("o%d" % i, [128, 512]) for i in range(2)]
        if not first:
            wpg = sb("wpg", [128, 8, D], BF16)
            wpe = sb("wpe", [128, 2, D], BF16)
            pt = [sb("pt%d" % i, [128, 2, TT], BF16) for i in range(2)]
            sg = [sb("sg%d" % i, [128, TT], F32) for i in range(1)]
            ps_g = [pst("g%d" % i, [128, 512]) for i in range(2)]

        P.op('pool', lambda E: E.memset(ones_bf[:], 1.0), writes=['ones'])
        P.op('pool', lambda E: E.memset(epsc[:], EPS), writes=['epsc'])
        P.op('sp', lambda E: E.dma_start(out=g1[:], in_=T['ffn1_norm' if first else 'ffn2_norm'][:, :]), writes=['g1'], dsem="p%d_g1" % which)
        P.op('sp', lambda E: E.dma_start(out=g2[:], in_=T['mix_norm' if first else 'ple_norm'][:, :]), writes=['g2'], dsem="p%d_g2" % which)
        w_in_r = w_in.rearrange("(c p) n -> p c n", p=128)
        w_out_r = w_out.rearrange("(j p) n -> p j n", p=128)
        pieces = []
        for jg in range(0, 22, 4):
            j1 = min(jg + 4, 22)
            for part in range(2):
                for k in range(8):
                    c0 = part * DFF + jg * 128
                    c1 = part * DFF + j1 * 128
                    pieces.append((w1[:, k, c0:c1], w_in_r[:, k, c0:c1], ('w1', part, k, jg // 4)))
        for j in range(22):
            for hh in range(2):
                pieces.append((w2[:, j, hh * 512:(hh + 1) * 512], w_out_r[:, j, hh * 512:(hh + 1) * 512], ('w2', j, hh)))
        if not first:
            wpg_r = T['w_ple_gate'].rearrange("(c p) n -> p c n", p=128)
            wpe_r = T['w_ple_proj'].rearrange("(c p) n -> p c n", p=128)
            for k in range(8):
                for hh in range(2):
                    pieces.append((wpg[:, k, hh * 512:(hh + 1) * 512], wpg_r[:, k, hh * 512:(hh + 1) * 512], ('wpg', k, hh)))
            for k in range(2):
                for hh in range(2):
                    pieces.append((wpe[:, k, hh * 512:(hh + 1) * 512], wpe_r[:, k, hh * 512:(hh + 1) * 512], ('wpe', k, hh)))
        load_cast(P, stage, pieces)

        src_r = src.rearrange("(c p) n -> p c n", p=128)

        def load_x(i):
            s = i % 3
            P.op('sp', lambda E: E.dma_start(out=xh[s][:], in_=src_r[:, :, i * TT:(i + 1) * TT]),
                 writes=[('xh', s, c) for c in range(8)], dsem="p%d_xh%d" % (which, s))
        def load_p(i):
            if not first:
                s2 = i % 2
                for kk in range(2):
                    P.op('pool', lambda E, kk=kk: E.dma_start(out=pt[s2][:, kk, :], in_=T['pT'][kk * 128:(kk + 1) * 128, i * TT:(i + 1) * TT]),
                         writes=[('pt', s2)], dsem="p2_pt%d" % s2)

        def norm(i, gain, gkey, dst, dkey):
            s = i % 3
            d = i % 2
            rms_stats(P, nc, [xh[s][:, c, :] for c in range(8)], [('xh', s, c) for c in range(8)],
                      [q[:] for q in sq], [('sq', 0), ('sq', 1)], ones_bf[:], ps_ss[:, 0:TT], 'ps_ss',
                      tmp[d % len(tmp)][:], ('tmp', d % len(tmp)), rstd[d][:], ('rstd', d), 1.0 / D, eps_ap=epsc[:, 0:1])
            for c in range(8):
                P.op('dve', lambda E, c=c: E.scalar_tensor_tensor(dst[d][:, c, :], xh[s][:, c, :], gain[:, c:c + 1], rstd[d][:],
                                                                   op0=ALU.mult, op1=ALU.mult),
                     reads=[('xh', s, c), gkey, ('rstd', d)], writes=[(dkey, d, c)])

        def ffn_in(i):
            d = i % 2
            for j in range(22):
                b = j % 2
                pa = ps_a[b]
                mm_group(P, pa[:, 0:TT], [(w1[:, k, j * 128:(j + 1) * 128], xn[d][:, k, :]) for k in range(8)],
                         reads=[('w1', 0, k, j // 4) for k in range(8)] + [('xn', d, k) for k in range(8)], writes=[('bka', b)])
                mm_group(P, pa[:, TT:2 * TT], [(w1[:, k, DFF + j * 128:DFF + (j + 1) * 128], xn[d][:, k, :]) for k in range(8)],
                         reads=[('w1', 1, k, j // 4) for k in range(8)] + [('xn', d, k) for k in range(8)], writes=[('bka', b)])
                P.op('act', lambda E, b=b, pa=pa: E.activation(out=sil[b][:], in_=pa[:, 0:TT], func=AF.Silu),
                     reads=[('bka', b)], writes=[('sil', b)])
                P.op('dve', lambda E, b=b, pa=pa, j=j: E.tensor_tensor(act[:, j, :], sil[b][:], pa[:, TT:2 * TT], op=ALU.mult),
                     reads=[('sil', b), ('bka', b)], writes=[('act', j)])

        def ffn_out(i):
            s = i % 3
            for pr in range(4):
                bk = pr % 2
                for hf in range(2):
                    oc = pr * 2 + hf
                    po = ps_o[bk][:, hf * TT:(hf + 1) * TT]
                    mm_group(P, po, [(w2[:, j, oc * 128:(oc + 1) * 128], act[:, j, :]) for j in range(22)],
                             reads=[('w2', j, oc // 4) for j in range(22)] + [('act', j) for j in range(22)], writes=[('bko', bk)])
                for hf in range(2):
                    oc = pr * 2 + hf
                    po = ps_o[bk][:, hf * TT:(hf + 1) * TT]
                    P.op('dve', lambda E, oc=oc, po=po: E.scalar_tensor_tensor(xh[s][:, oc, :], po, 0.5, xh[s][:, oc, :], op0=ALU.mult, op1=ALU.add),
                         reads=[('bko', bk), ('xh', s, oc)], writes=[('xh', s, oc)])

        def ple(i):
            s = i % 3
            d = i % 2
            for oc in range(8):
                b = oc % 2
                pg = ps_g[b]
                mm_group(P, pg[:, 0:TT], [(wpg[:, k, oc * 128:(oc + 1) * 128], un[d][:, k, :]) for k in range(8)],
                         reads=[('wpg', k, oc // 4) for k in range(8)] + [(unk, d, k) for k in range(8)], writes=[('bkg', b)])
                mm_group(P, pg[:, TT:2 * TT], [(wpe[:, k, oc * 128:(oc + 1) * 128], pt[d][:, k, :]) for k in range(2)],
                         reads=[('wpe', k, oc // 4) for k in range(2)] + [('pt', d)], writes=[('bkg', b)])
                P.op('act', lambda E, b=b, pg=pg: E.activation(out=sg[0][:], in_=pg[:, 0:TT], func=AF.Sigmoid),
                     reads=[('bkg', b)], writes=[('sg', 0)])
                P.op('dve', lambda E, b=b, pg=pg: E.tensor_tensor(sg[0][:], sg[0][:], pg[:, TT:2 * TT], op=ALU.mult),
                     reads=[('sg', 0), ('bkg', b)], writes=[('sg', 0)])
                P.op('dve', lambda E, b=b, oc=oc: E.tensor_tensor(xh[s][:, oc, :], sg[0][:], xh[s][:, oc, :], op=ALU.add),
                     reads=[('sg', 0), ('xh', s, oc)], writes=[('xh', s, oc)])
            P.op('sp', lambda E: E.dma_start(out=T['outT'].rearrange("(c p) n -> p c n", p=128)[:, :, i * TT:(i + 1) * TT], in_=xh[s][:]),
                 reads=[('xh', s, oc) for oc in range(8)], dsem="p2_ot%d" % s)

        KSTOP = os.environ.get('KSTOP', '')
        if KSTOP == 'w':
            P.barrier(); return
        load_x(0)
        load_x(1)
        load_p(0)
        norm(0, g1, 'g1', xn, 'xn')
        if first:
            for i in range(ntiles):
                if i + 2 < ntiles:
                    load_x(i + 2)
                ffn_in(i)
                if i + 1 < ntiles:
                    norm(i + 1, g1, 'g1', xn, 'xn')
                ffn_out(i)
                s = i % 3
                if i >= own0:
                    io = i - own0
                    P.op('sp', lambda E: E.dma_start(out=T['h1'].rearrange("(c p) n -> p c n", p=128)[:, :, io * TT:(io + 1) * TT], in_=xh[s][:]),
                         reads=[('xh', s, c) for c in range(8)], dsem="p1_h%d" % s)
                norm(i, g2, 'g2', un, unk)
                d = i % 2
                P.op('sp', lambda E: E.dma_start(out=T['u'].rearrange("(c p) n -> p c n", p=128)[:, :, i * TT:(i + 1) * TT], in_=un[d][:]),
                     reads=[('un', d, c) for c in range(8)], dsem="p1_u%d" % d)
        else:
            for i in range(ntiles):
                ffn_in(i)
                if i > 0:
                    ple(i - 1)
                if i + 2 < ntiles:
                    load_x(i + 2)
                if i + 1 < ntiles:
                    load_p(i + 1)
                    norm(i + 1, g1, 'g1', xn, 'xn')
                ffn_out(i)
                norm(i, g2, 'g2', un, unk)
            ple(ntiles - 1)
        P.barrier()


def merge_phase(P, nc, T):
    TT = 512
    ntiles = int(os.environ.get('KNT3', OWN // TT))
    with ExitStack() as ph:
        def sb(name, shape, dt):
            return ph.enter_context(nc.sbuf_tensor("p3_" + name, shape, dt))

        def pst(name, shape, dt=F32):
            return ph.enter_context(nc.psum_tensor("p3_" + name, shape, dt))
        wg = sb("wg", [128, 8, 2048], BF16)
        wua = sb("wua", [128, 4, D], BF16)
        wub = sb("wub", [128, 4, D], BF16)
        wo = sb("wo", [128, 8, D], BF16)
        stage = [sb("stg%d" % i, [128, 512], F32) for i in range(3)]
        ut = [sb("ut%d" % i, [128, 8, TT], BF16) for i in range(2)]
        at = [sb("at%d" % i, [128, 4, TT], BF16) for i in range(2)]
        mt = [sb("mt%d" % i, [128, 4, TT], BF16) for i in range(2)]
        ht = [sb("ht%d" % i, [128, 8, TT], F32) for i in range(2)]
        mg = sb("mg", [128, 8, TT], BF16)
        sg = [sb("sg%d" % i, [128, TT], F32) for i in range(2)]
        m1 = [sb("m1%d" % i, [128, TT], F32) for i in range(2)]
        psg = [pst("g%d" % i, [128, 512]) for i in range(2)]
        psy = [pst("y%d" % i, [128, 512]) for i in range(2)]
        pso = [pst("o%d" % i, [128, 512]) for i in range(2)]
        w_in_r = T['w_in'].rearrange("(c p) n -> p c n", p=128)
        pieces = []
        for k in range(8):
            for q in range(4):
                pieces.append((wg[:, k, q * 512:(q + 1) * 512], w_in_r[:, k, C_GA + q * 512:C_GA + (q + 1) * 512], ('wg', k, q)))
        for nm, wt, src in (('wua', wua, T['w_up_att']), ('wub', wub, T['w_up_mlstm'])):
            r = src.rearrange("(c p) n -> p c n", p=128)
            for k in range(4):
                for q in range(2):
                    pieces.append((wt[:, k, q * 512:(q + 1) * 512], r[:, k, q * 512:(q + 1) * 512], (nm, k, q)))
        r = T['w_out'].rearrange("(c p) n -> p c n", p=128)
        for k in range(8):
            for q in range(2):
                pieces.append((wo[:, k, q * 512:(q + 1) * 512], r[:, k, q * 512:(q + 1) * 512], ('wo', k, q)))
        load_cast(P, stage, pieces)
        u_r = T['u'].rearrange("(c p) n -> p c n", p=128)
        a_r = T['att'].rearrange("(c p) n -> p c n", p=128)
        m_r = T['hg'].rearrange("(c p) n -> p c n", p=128)
        h_r = T['h1'].rearrange("(c p) n -> p c n", p=128)
        o_r = T['h2'].rearrange("(c p) n -> p c n", p=128)

        def loads(i):
            d = i % 2
            P.op('sp', lambda E: E.dma_start(out=ut[d][:], in_=u_r[:, :, OWN + i * TT:OWN + (i + 1) * TT]), writes=[('ut', d)], dsem="p3_ut%d" % d)
            P.op('sp', lambda E: E.dma_start(out=at[d][:], in_=a_r[:, :, i * TT:(i + 1) * TT]), writes=[('at', d)], dsem="p3_at%d" % d)
            P.op('sp', lambda E: E.dma_start(out=mt[d][:], in_=m_r[:, :, i * TT:(i + 1) * TT]), writes=[('mt', d)], dsem="p3_mt%d" % d)
            P.op('sp', lambda E: E.dma_start(out=ht[d][:], in_=h_r[:, :, i * TT:(i + 1) * TT]), writes=[('ht', d, c) for c in range(8)], dsem="p3_ht%d" % d)
        loads(0)
        for i in range(ntiles):
            d = i % 2
            if i + 1 < ntiles:
                loads(i + 1)
            for oc in range(8):
                for br in range(2):
                    b = br
                    wy, ykey, yt, ytk = (wua, 'wua', at, 'at') if br == 0 else (wub, 'wub', mt, 'mt')
                    mm_group(P, psg[b][:], [(wg[:, k, br * 1024 + oc * 128:br * 1024 + (oc + 1) * 128], ut[d][:, k, :]) for k in range(8)],
                             reads=[('wg', k, (br * 1024 + oc * 128) // 512) for k in range(8)] + [('ut', d)], writes=[('bkg', b)])
                    mm_group(P, psy[b][:], [(wy[:, k, oc * 128:(oc + 1) * 128], yt[d][:, k, :]) for k in range(4)],
                             reads=[(ykey, k, oc // 4) for k in range(4)] + [(ytk, d)], writes=[('bky', b)])
                    P.op('act', lambda E: E.activation(out=sg[b][:], in_=psg[b][:], func=AF.Sigmoid), reads=[('bkg', b)], writes=[('sg', b)])
                    P.op('dve', lambda E: E.tensor_tensor(m1[b][:], sg[b][:], psy[b][:], op=ALU.mult), reads=[('sg', b), ('bky', b)], writes=[('m1', b)])
                P.op('pool', lambda E: E.tensor_tensor(mg[:, oc, :], m1[0][:], m1[1][:], op=ALU.add), reads=[('m1', 0), ('m1', 1)], writes=[('mg', oc)])
            for oc in range(8):
                b = oc % 2
                mm_group(P, pso[b][:], [(wo[:, k, oc * 128:(oc + 1) * 128], mg[:, k, :]) for k in range(8)],
                         reads=[('wo', k, oc // 4) for k in range(8)] + [('mg', k) for k in range(8)], writes=[('bko', b)])
                P.op('dve', lambda E: E.tensor_tensor(ht[d][:, oc, :], pso[b][:], ht[d][:, oc, :], op=ALU.add),
                     reads=[('bko', b), ('ht', d, oc)], writes=[('ht', d, oc)])
            P.op('sp', lambda E: E.dma_start(out=o_r[:, :, i * TT:(i + 1) * TT], in_=ht[d][:]), reads=[('ht', d, c) for c in range(8)], dsem="p3_o%d" % d)
        P.barrier()


def mlstm_phase(P, nc, T):
    TT = 512
    ntiles = NTOK // TT
    own0 = (NTOK - OWN) // TT
    t_start = int(os.environ.get('KT2A0', 0))
    t_end = int(os.environ.get('KT2A1', ntiles))
    SC = 128 ** -0.5
    with ExitStack() as ph:
        def sb(name, shape, dt):
            return ph.enter_context(nc.sbuf_tensor("p2a_" + name, shape, dt))

        def pst(name, shape, dt=F32):
            return ph.enter_context(nc.psum_tensor("p2a_" + name, shape, dt))
        wm = sb("wm", [128, 8, 2048], BF16)
        wgt = sb("wgt", [128, 8, 8], BF16)
        wgf = sb("wgf", [128, 8, 8], F32)
        stage = [sb("stg%d" % i, [128, 512], F32) for i in range(3)]
        ident_f = sb("identf", [128, 128], F32)
        tri_f = sb("trif", [128, 128], F32)
        ident_bf = sb("identb", [128, 128], BF16)
        mask_bf = sb("maskb", [128, 128], BF16)
        ones_bf = sb("onesb", [128, 128], BF16)
        ones4 = sb("ones4", [4, 128], F32)
        selc = sb("selc", [4, 4, 128], F32)
        flag = sb("flag", [128, 1], F32)
        cw = sb("cw", [128, 8, 4], F32)
        cb = sb("cb", [128, 8], F32)
        ibias = sb("ibias", [4, 1], F32)
        fbias = sb("fbias", [128, 4], F32)
        Cst = sb("Cst", [128, 4, 129], F32)
        mst = sb("mst", [4, 1], F32)
        ut = [sb("ut%d" % i, [128, 8, TT], BF16) for i in range(2)]
        pre = sb("pre", [128, 8, TT + 3], F32)
        cv = sb("cv", [128, 8, TT], F32)
        qT = sb("qT", [128, 4, TT], BF16)
        kT = sb("kT", [128, 4, TT], BF16)
        so = sb("so", [128, 4, TT], F32)
        hgt = [sb("hgt%d" % i, [128, 4, TT], BF16) for i in range(2)]
        vt = sb("vt", [128, 4, 129], BF16)
        kw = sb("kw", [128, 4, 128], BF16)
        pT = sb("pT", [128, 4, 128], BF16)
        Cb = sb("Cb", [128, 4, 128], BF16)
        nbc = sb("nbc", [128, 4, 128], BF16)
        thrS = sb("thrS", [128, 512], F32)
        dd = sb("dd", [128, 512], F32)
        rr = sb("rr", [128, 512], F32)
        hh = sb("hh", [128, 512], F32)
        fx = sb("fx", [128, 4], F32)
        lt = sb("lt", [128, 4], F32)
        nbr = sb("nbr", [4, 128], F32)
        gp = sb("gp", [4, 128], F32)
        wrow = sb("wrow", [4, 128], F32)
        trow = sb("trow", [4, 128], F32)
        gmax = sb("gmax", [4, 1], F32)
        Mv = sb("Mv", [4, 1], F32)
        negM = sb("negM", [4, 1], F32)
        av = sb("av", [4, 1], F32)
        da = sb("da", [4, 4], F32)
        wtok = sb("wtok", [128, 4], F32)
        abc = sb("abc", [128, 4], F32)
        B0 = pst("b0", [128, 512])
        B1 = pst("b1", [128, 512])
        B2 = pst("b2", [128, 512])
        B3 = pst("b3", [128, 512])
        B4 = pst("b4", [128, 1024], BF16)
        B5 = pst("b5", [128, 512])
        B6 = pst("b6", [128, 512])
        B7 = pst("b7", [128, 512])

        P.op('sp', lambda E: E.dma_start(out=ident_f[:], in_=T['c_ident'][:, :]), writes=['identf'], dsem="p2a_c0")
        P.op('sp', lambda E: E.dma_start(out=tri_f[:], in_=T['c_tri'][:, :]), writes=['trif'], dsem="p2a_c1")
        P.op('sp', lambda E: E.dma_start(out=flag[:], in_=T['c_flag'][:, :]), writes=['flag'], dsem="p2a_c2")
        P.op('sp', lambda E: E.dma_start(out=cw[:], in_=T['conv_w'][:, :, :]), writes=['cw'], dsem="p2a_c3")
        P.op('sp', lambda E: E.dma_start(out=cb[:], in_=T['conv_b'][:, :]), writes=['cb'], dsem="p2a_c4")
        P.op('sp', lambda E: E.dma_start(out=ibias[:], in_=T['i_bias'][:, :]), writes=['ibias'], dsem="p2a_c5")
        P.op('sp', lambda E: E.dma_start(out=fbias[:], in_=T['f_bias'][:, :]), writes=['fbias'], dsem="p2a_c6")
        w_in_r = T['w_in'].rearrange("(c p) n -> p c n", p=128)
        P.op('sp', lambda E: E.dma_start(out=wgf[:], in_=w_in_r[:, :, C_MI:C_MI + 8]), writes=['wgf'], dsem="p2a_c7")
        P.op('pool', lambda E: E.tensor_copy(wgt[:], wgf[:]), reads=['wgf'], writes=['wgt'])
        P.op('pool', lambda E: E.tensor_copy(ident_bf[:], ident_f[:]), reads=['identf'], writes=['identb'])
        P.op('pool', lambda E: E.tensor_copy(mask_bf[:], tri_f[:]), reads=['trif'], writes=['maskb'])
        P.op('pool', lambda E: E.memset(ones_bf[:], 1.0), writes=['onesb'])
        P.op('pool', lambda E: E.memset(ones4[:], 1.0), writes=['ones4'])
        P.op('pool', lambda E: E.memset(Cst[:], 0.0), writes=['Cst'])
        P.op('pool', lambda E: E.memset(mst[:], 0.0), writes=['mst'])
        P.op('pool', lambda E: E.memset(vt[:], 1.0), writes=['vt'])
        P.op('pool', lambda E: E.memset(pre[:], 0.0), writes=[('pre', c) for c in range(8)])
        for h in range(4):
            P.op('pool', lambda E, h=h: E.tensor_scalar(selc[:, h, :], ones4[:], ident_f[0:4, h:h + 1], None, op0=ALU.mult),
                 reads=['ones4', 'identf'], writes=['selc'])
        pieces = []
        for k in range(8):
            for q in range(4):
                pieces.append((wm[:, k, q * 512:(q + 1) * 512], w_in_r[:, k, C_MQ + q * 512:C_MQ + (q + 1) * 512], ('wm', k, q)))
        load_cast(P, stage, pieces)
        u_r = T['u'].rearrange("(c p) n -> p c n", p=128)
        hg_r = T['hg'].rearrange("(c p) n -> p c n", p=128)

        def load_u(i):
            d = i % 2
            P.op('sp', lambda E: E.dma_start(out=ut[d][:], in_=u_r[:, :, i * TT:(i + 1) * TT]), writes=[('ut', d)], dsem="p2a_ut%d" % d)

        load_u(t_start)
        for i in range(t_start, t_end):
            d = i % 2
            own = i >= own0
            if i + 1 < t_end:
                load_u(i + 1)
            fcs = ([(0, fc) for fc in range(4)] if (own or i == own0 - 1) else []) + [(1, fc) for fc in range(4)]
            for (kind, fc) in fcs:
                c8 = kind * 4 + fc
                mm_group(P, B0[:], [(wm[:, k, c8 * 128:(c8 + 1) * 128], ut[d][:, k, :]) for k in range(8)],
                         reads=[('wm', k, kind) for k in range(8)] + [('ut', d)], writes=['B0'])
                P.op('act', lambda E: E.copy(out=pre[:, c8, 3:TT + 3], in_=B0[:]), reads=['B0'], writes=[('pre', c8)])
            if own:
                for fc in range(4):
                    mm_group(P, B0[:], [(wm[:, k, 1536 + fc * 128:1536 + (fc + 1) * 128], ut[d][:, k, :]) for k in range(8)],
                             reads=[('wm', k, 3) for k in range(8)] + [('ut', d)], writes=['B0'])
                    P.op('act', lambda E: E.activation(out=so[:, fc, :], in_=B0[:], func=AF.Sigmoid), reads=['B0'], writes=[('so', fc)])
            for (kind, fc) in fcs:
                c8 = kind * 4 + fc
                eng = 'dve'
                P.op(eng, lambda E: E.tensor_scalar(cv[:, c8, :], pre[:, c8, 0:TT], cw[:, c8, 0:1], cb[:, c8:c8 + 1], op0=ALU.mult, op1=ALU.add),
                     reads=[('pre', c8), 'cw', 'cb'], writes=[('cv', c8)])
                for j in range(1, 4):
                    P.op(eng, lambda E: E.scalar_tensor_tensor(cv[:, c8, :], pre[:, c8, j:TT + j], cw[:, c8, j:j + 1], cv[:, c8, :], op0=ALU.mult, op1=ALU.add),
                         reads=[('pre', c8), ('cv', c8), 'cw'], writes=[('cv', c8)])
                P.op(eng, lambda E: E.tensor_copy(pre[:, c8, 0:3], pre[:, c8, TT:TT + 3]), reads=[('pre', c8)], writes=[('pre', c8)])
                if kind == 0:
                    P.op('act', lambda E: E.activation(out=cv[:, c8, :], in_=cv[:, c8, :], func=AF.Silu), reads=[('cv', c8)], writes=[('cv', c8)])
                    P.op('dve', lambda E: E.tensor_scalar(qT[:, fc, :], cv[:, c8, :], SC, None, op0=ALU.mult), reads=[('cv', c8)], writes=[('qT', fc)])
                else:
                    P.op('act', lambda E: E.activation(out=kT[:, fc, :], in_=cv[:, c8, :], func=AF.Silu), reads=[('cv', c8)], writes=[('kT', fc)])
            for ci in range(4):
                c0 = ci * 128
                cs = slice(c0, c0 + 128)
                mm_group(P, B1[:], [(ut[d][:, k, cs], wm[:, k, 1024:1536]) for k in range(8)],
                         reads=[('wm', k, 2) for k in range(8)] + [('ut', d)], writes=['B1'])
                mm_group(P, B2[:, 0:8], [(ut[d][:, k, cs], wgt[:, k, 0:8]) for k in range(8)], reads=['wgt', ('ut', d)], writes=['B2'])
                mm_group(P, B2[0:4, 8:136], [(wgt[:, k, 0:4], ut[d][:, k, cs]) for k in range(8)], reads=['wgt', ('ut', d)], writes=['B2'])
                P.op('act', lambda E: E.copy(out=vt[:, :, 0:128], in_=B1[:].rearrange("p (h e) -> p h e", h=4)), reads=['B1'], writes=['vt'])
                P.op('dve', lambda E: E.tensor_tensor(fx[:], B2[:, 4:8], fbias[:], op=ALU.add), reads=['B2', 'fbias'], writes=['fx'])
                P.op('act', lambda E: E.activation(out=fx[:], in_=fx[:], func=AF.Exp, scale=-1.0), reads=['fx'], writes=['fx'])
                P.op('act', lambda E: E.activation(out=lt[:], in_=fx[:], func=AF.Ln, bias=1.0, scale=1.0), reads=['fx'], writes=['lt'])
                mm_group(P, B3[0:4, 0:128], [(lt[:], tri_f[:])], reads=['lt', 'trif'], writes=['B3'])
                P.op('act', lambda E: E.copy(out=nbr[:], in_=B3[0:4, 0:128]), reads=['B3'], writes=['nbr'])
                P.op('dve', lambda E: E.scalar_tensor_tensor(gp[:], B2[0:4, 8:136], ibias[:, 0:1], nbr[:], op0=ALU.add, op1=ALU.add),
                     reads=['B2', 'ibias', 'nbr'], writes=['gp'])
                P.op('dve', lambda E: E.reduce_max(gmax[:], gp[:], axis=AX.X), reads=['gp'], writes=['gmax'])
                P.op('dve', lambda E: E.tensor_tensor(Mv[:], gmax[:], mst[:], op=ALU.max), reads=['gmax', 'mst'], writes=['Mv'])
                P.op('dve', lambda E: E.tensor_scalar(negM[:], Mv[:], -1.0, None, op0=ALU.mult), reads=['Mv'], writes=['negM'])
                P.op('act', lambda E: E.activation(out=wrow[:], in_=gp[:], func=AF.Exp, bias=negM[:, 0:1], scale=1.0), reads=['gp', 'negM'], writes=['wrow'])
                P.op('act', lambda E: E.activation(out=av[:], in_=mst[:], func=AF.Exp, bias=negM[:, 0:1], scale=1.0), reads=['mst', 'negM'], writes=['av'])
                if own:
                    P.op('act', lambda E: E.activation(out=trow[:], in_=nbr[:], func=AF.Exp, bias=negM[:, 0:1], scale=1.0), reads=['nbr', 'negM'], writes=['trow'])
                P.op('dve', lambda E: E.tensor_tensor(mst[:], Mv[:], nbr[:, 127:128], op=ALU.subtract), reads=['Mv', 'nbr'], writes=['mst'])
                mm_group(P, B3[:, 128:132], [(wrow[:], ident_f[0:4, 0:4])], reads=['wrow', 'identf'], writes=['B3'])
                P.op('dve', lambda E: E.tensor_scalar(da[:], ident_f[0:4, 0:4], av[:, 0:1], None, op0=ALU.mult), reads=['av', 'identf'], writes=['da'])
                mm_group(P, B3[:, 136:140], [(ones4[:], da[:])], reads=['ones4', 'da'], writes=['B3'])
                if own:
                    P.op('dve', lambda E: E.tensor_copy(wtok[:], B3[:, 128:132]), reads=['B3'], writes=['wtok'])
                else:
                    P.op('dve', lambda E: E.tensor_scalar(wtok[:], B3[:, 128:132], flag[:, 0:1], None, op0=ALU.mult), reads=['B3', 'flag'], writes=['wtok'])
                P.op('dve', lambda E: E.tensor_copy(abc[:], B3[:, 136:140]), reads=['B3'], writes=['abc'])
                for h in range(4):
                    P.op('dve', lambda E, h=h: E.tensor_scalar(Cst[:, h, :], Cst[:, h, :], abc[:, h:h + 1], None, op0=ALU.mult), reads=['Cst', 'abc'], writes=['Cst'])
                def trf(E):
                    ins = None
                    for h in range(4):
                        ins = E.transpose(B4[:, h * 128:(h + 1) * 128], kT[:, h, cs], ident_bf[:])
                    return ins
                P.op('pe', trf, reads=[('kT', h) for h in range(4)] + ['identb'], writes=['B4'])
                for h in range(4):
                    P.op('dve', lambda E, h=h: E.tensor_scalar(kw[:, h, :], B4[:, h * 128:(h + 1) * 128], wtok[:, h:h + 1], None, op0=ALU.mult),
                         reads=['B4', 'wtok'], writes=['kw'])
                if own:
                    P.op('act', lambda E: E.copy(out=Cb[:], in_=Cst[:, :, 0:128]), reads=['Cst'], writes=['Cb'])
                    for h in range(4):
                        P.op('dve', lambda E, h=h: E.tensor_scalar(nbc[:, h, :], ones_bf[:], Cst[:, h, 128:129], None, op0=ALU.mult), reads=['Cst', 'onesb'], writes=['nbc'])
                    def qkf(E):
                        ins = None
                        for h in range(4):
                            ins = E.matmul(B5[:, h * 128:(h + 1) * 128], kT[:, h, cs], qT[:, h, cs], start=True, stop=True)
                        return ins
                    P.op('pe', qkf, reads=[('kT', h) for h in range(4)] + [('qT', h) for h in range(4)], writes=['B5'])
                    for h in range(4):
                        P.op('dve', lambda E, h=h: E.scalar_tensor_tensor(pT[:, h, :], B5[:, h * 128:(h + 1) * 128], wtok[:, h:h + 1], mask_bf[:], op0=ALU.mult, op1=ALU.mult),
                             reads=['B5', 'wtok', 'maskb'], writes=['pT'])
                    def numf(E):
                        ins = None
                        for h in range(4):
                            E.matmul(B6[:, h * 128:(h + 1) * 128], Cb[:, h, :], qT[:, h, cs], start=True, stop=False)
                            ins = E.matmul(B6[:, h * 128:(h + 1) * 128], vt[:, h, 0:128], pT[:, h, :], start=False, stop=True)
                        return ins
                    P.op('pe', numf, reads=['Cb', 'vt', 'pT'] + [('qT', h) for h in range(4)], writes=['B6'])

                    def denf(E):
                        ins = None
                        for h in range(4):
                            E.matmul(B1[:, h * 128:(h + 1) * 128], nbc[:, h, :], qT[:, h, cs], start=True, stop=False)
                            ins = E.matmul(B1[:, h * 128:(h + 1) * 128], ones_bf[:], pT[:, h, :], start=False, stop=True)
                        return ins
                    P.op('pe', denf, reads=['nbc', 'onesb', 'pT'] + [('qT', h) for h in range(4)], writes=['B1'])

                    def thrf(E):
                        ins = None
                        for h in range(4):
                            ins = E.matmul(B0[:, h * 128:(h + 1) * 128], selc[:, h, :], trow[:], start=True, stop=True)
                        return ins
                    P.op('pe', thrf, reads=['selc', 'trow'], writes=['B0'])
                    P.op('act', lambda E: E.copy(out=thrS[:], in_=B0[:]), reads=['B0'], writes=['thrS'])
                    P.op('dve', lambda E: E.tensor_tensor(dd[:], B1[:], thrS[:], op=ALU.max), reads=['B1', 'thrS'], writes=['dd'])
                    P.op('dve', lambda E: E.scalar_tensor_tensor(dd[:], B1[:], -1.0, dd[:], op0=ALU.mult, op1=ALU.max), reads=['B1', 'dd'], writes=['dd'])
                    P.op('act', lambda E: E.activation(out=rr[:], in_=dd[:], func=AF.Ln), reads=['dd'], writes=['rr'])
                    P.op('act', lambda E: E.activation(out=rr[:], in_=rr[:], func=AF.Exp, scale=-1.0), reads=['rr'], writes=['rr'])
                    P.op('dve', lambda E: E.tensor_tensor(hh[:], B6[:], rr[:], op=ALU.mult), reads=['B6', 'rr'], writes=['hh'])
                    P.op('pool', lambda E: E.tensor_tensor(hgt[d][:, :, cs], hh[:].rearrange("p (h t) -> p h t", h=4), so[:, :, cs], op=ALU.mult),
                         reads=['hh'] + [('so', fc) for fc in range(4)], writes=[('hgt', d)])
                def dcf(E):
                    ins = None
                    for h in range(4):
                        bank = B5 if h < 2 else B7
                        ins = E.matmul(bank[:, (h % 2) * 129:(h % 2 + 1) * 129], kw[:, h, :], vt[:, h, :], start=True, stop=True)
                    return ins
                P.op('pe', dcf, reads=['kw', 'vt'], writes=['B5', 'B7'])
                P.op('dve', lambda E: E.tensor_tensor(Cst[:, 0:2, :], Cst[:, 0:2, :], B5[:, 0:258].rearrange("p (h e) -> p h e", h=2), op=ALU.add), reads=['Cst', 'B5'], writes=['Cst'])
                P.op('dve', lambda E: E.tensor_tensor(Cst[:, 2:4, :], Cst[:, 2:4, :], B7[:, 0:258].rearrange("p (h e) -> p h e", h=2), op=ALU.add), reads=['Cst', 'B7'], writes=['Cst'])
            if own:
                io = i - own0
                P.op('sp', lambda E: E.dma_start(out=hg_r[:, :, io * TT:(io + 1) * TT], in_=hgt[d][:]), reads=[('hgt', d)], dsem="p2a_hg%d" % d)
        P.barrier()


def attn_phase(P, nc, T):
    TT = 512
    L = 6144
    SCALE = 128 ** -0.5
    slots = int(os.environ.get('KSLOTS', 4))
    with ExitStack() as ph:
        def sb(name, shape, dt):
            return ph.enter_context(nc.sbuf_tensor("p2b_" + name, shape, dt))

        def pst(name, shape, dt=F32):
            return ph.enter_context(nc.psum_tensor("p2b_" + name, shape, dt))
        ures = sb("ures", [128, 8, L], BF16)
        wq = sb("wq", [128, 8, 128], BF16)
        wk = sb("wk", [128, 8, 128], BF16)
        wv = sb("wv", [128, 8, 128], BF16)
        stage = [sb("stg%d" % i, [128, 128], F32) for i in range(4)]
        qT = sb("qT", [128, OWN], BF16)
        kT = sb("kT", [128, L], BF16)
        vt = sb("vt", [128, 48, 128], BF16)
        acc = sb("acc", [128, 2, OWN], F32)
        ones_bf = sb("onesb", [128, 128], BF16)
        fones = sb("fones", [128, 128], BF16)
        mask2 = sb("mask2", [128, 512], BF16)
        trif = sb("trif", [128, 128], F32)
        tritf = sb("tritf", [128, 128], F32)
        flag = sb("flag", [128, 1], F32)
        gq = sb("gq", [128, 1], F32)
        gk = sb("gk", [128, 1], F32)
        rm = sb("rm", [32, 32], F32)
        sq = [sb("sq%d" % i, [128, TT], BF16) for i in range(2)]
        tmp = [sb("tmp%d" % i, [128, TT], F32) for i in range(2)]
        rstd = [sb("rstd%d" % i, [128, TT], F32) for i in range(2)]
        qn = [sb("qn%d" % i, [128, TT], F32) for i in range(2)]
        t2 = [sb("t2%d" % i, [32, TT], F32) for i in range(2)]
        epsc = sb("epsc", [128, 1], F32)
        qnb = [sb("qnb%d" % i, [32, TT], BF16) for i in range(2)]
        rmb = sb("rmb", [32, 32], BF16)
        cosb = [sb("cos%d" % i, [32, TT], F32) for i in range(2)]
        sinb = [sb("sin%d" % i, [32, TT], F32) for i in range(2)]
        pT = [sb("pT%d" % i, [128, 512], BF16) for i in range(2)]
        atto = [sb("atto%d" % i, [128, TT], BF16) for i in range(2)]
        rden = [sb("rden%d" % i, [128, TT], F32) for i in range(2)]
        BK = [pst("bk%d" % i, [128, 512]) for i in range(8)]
        BSC = [BK[0], BK[1]]
        BN = [BK[2], BK[3]]

        P.op('sp', lambda E: E.dma_start(out=trif[:], in_=T['c_tri'][:, :]), writes=['trif'], dsem="p2b_c0")
        P.op('sp', lambda E: E.dma_start(out=tritf[:], in_=T['c_trit'][:, :]), writes=['tritf'], dsem="p2b_c1")
        P.op('sp', lambda E: E.dma_start(out=flag[:], in_=T['c_flag'][:, :]), writes=['flag'], dsem="p2b_c2")
        P.op('sp', lambda E: E.dma_start(out=gq[:], in_=T['q_gain'][:, :]), writes=['gq'], dsem="p2b_c3")
        P.op('sp', lambda E: E.dma_start(out=gk[:], in_=T['k_gain'][:, :]), writes=['gk'], dsem="p2b_c4")
        P.op('sp', lambda E: E.dma_start(out=rm[:], in_=T['c_rm'][:, :]), writes=['rm'], dsem="p2b_c5")
        P.op('pool', lambda E: E.memset(ones_bf[:], 1.0), writes=['onesb'])
        P.op('pool', lambda E: E.memset(epsc[:], EPS), writes=['epsc'])
        P.op('pool', lambda E: E.tensor_copy(rmb[:], rm[:]), reads=['rm'], writes=['rmb'])
        P.op('pool', lambda E: E.tensor_scalar(fones[:], ones_bf[:], flag[:, 0:1], None, op0=ALU.mult), reads=['onesb', 'flag'], writes=['fones'])
        for qb in range(2):
            P.op('pool', lambda E, qb=qb: E.tensor_copy(mask2[:, (qb * 2) * 128:(qb * 2 + 1) * 128], tritf[:]), reads=['tritf'], writes=['mask2'])
            P.op('pool', lambda E, qb=qb: E.tensor_copy(mask2[:, (qb * 2 + 1) * 128:(qb * 2 + 2) * 128], trif[:]), reads=['trif'], writes=['mask2'])
        u_r = T['u'].rearrange("(c p) n -> p c n", p=128)
        for tl in range(12):
            P.op('sp', lambda E, tl=tl: E.dma_start(out=ures[:, :, tl * TT:(tl + 1) * TT], in_=u_r[:, :, 2048 + tl * TT:2048 + (tl + 1) * TT]),
                 writes=[('ures', tl)], dsem="p2b_u%d" % tl)
            if tl % 4 == 3:
                pass
        ures_all = [('ures', tl) for tl in range(12)]
        w_in_r = T['w_in'].rearrange("(c p) n -> p c n", p=128)
        att_r = T['att'].rearrange("(c p) n -> p c n", p=128)
        cnt = [0]

        def jobinfo(kind, tl, g):
            l0 = tl * TT
            w, wkey, gain, gkey = (wq, 'wq', gq, 'gq') if kind == 'q' else (wk, 'wk', gk, 'gk')
            dst = qT[:, l0 - 2048:l0 - 2048 + TT] if kind == 'q' else kT[:, l0:l0 + TT]
            cs = g % 2
            return dict(kind=kind, tl=tl, l0=l0, w=w, wkey=wkey, gain=gain, gkey=gkey, dst=dst, cs=cs,
                        BQ=BK[g % 3], BS=BK[3 + cs], BR=BK[5 + cs], kq=('bk', g % 3), ks=('bk', 3 + cs), kr=('bk', 5 + cs))

        def stA(j):
            cs, BQ, l0, w = j['cs'], j['BQ'], j['l0'], j['w']
            mm_group(P, BQ[:], [(w[:, k, :], ures[:, k, l0:l0 + TT]) for k in range(8)],
                     reads=[(j['wkey'], k) for k in range(8)] + [('ures', j['tl'])], writes=[j['kq']])
            P.op('act', lambda E: E.activation(out=sq[cs][:], in_=BQ[:], func=AF.Square), reads=[j['kq']], writes=[('sq', cs)])

        def stB(j):
            cs, BQ, BS, l0 = j['cs'], j['BQ'], j['BS'], j['l0']
            P.op('sp', lambda E: E.dma_start(out=cosb[cs][:], in_=T['c_cos'][:, l0:l0 + TT]), writes=[('cos', cs)], dsem="p2b_cos%d" % cs)
            P.op('sp', lambda E: E.dma_start(out=sinb[cs][:], in_=T['c_sin'][:, l0:l0 + TT]), writes=[('sin', cs)], dsem="p2b_sin%d" % cs)
            mm_group(P, BS[:], [(ones_bf[:], sq[cs][:])], reads=['onesb', ('sq', cs)], writes=[j['ks']])
            P.op('act', lambda E: E.activation(out=tmp[cs][:], in_=BS[:], func=AF.Ln, bias=epsc[:, 0:1], scale=1.0 / 128), reads=[j['ks'], 'epsc'], writes=[('tmp', cs)])
            P.op('act', lambda E: E.activation(out=rstd[cs][:], in_=tmp[cs][:], func=AF.Exp, scale=-0.5), reads=[('tmp', cs)], writes=[('rstd', cs)])
            dst, kind, tl = j['dst'], j['kind'], j['tl']
            P.op('dve', lambda E: E.scalar_tensor_tensor(dst[:, :], BQ[:], j['gain'][:, 0:1], rstd[cs][:], op0=ALU.mult, op1=ALU.mult),
                 reads=[j['kq'], j['gkey'], ('rstd', cs)], writes=[(kind + 'T', tl)])
            P.op('dve', lambda E: E.scalar_tensor_tensor(qn[cs][0:32, :], BQ[0:32, :], j['gain'][0:32, 0:1], rstd[cs][0:32, :], op0=ALU.mult, op1=ALU.mult),
                 reads=[j['kq'], j['gkey'], ('rstd', cs)], writes=[('qn', cs)])

        def stC(j):
            cs, BR, dst, kind, tl = j['cs'], j['BR'], j['dst'], j['kind'], j['tl']
            mm_group(P, BR[0:32, :], [(rmb[:], dst[0:32, :])], reads=['rmb', (kind + 'T', tl)], writes=[j['kr']])
            P.op('pool', lambda E: E.tensor_tensor(qn[cs][0:32, :], qn[cs][0:32, :], cosb[cs][:], op=ALU.mult), reads=[('qn', cs), ('cos', cs)], writes=[('qn', cs)])
            P.op('dve', lambda E: E.tensor_tensor(t2[cs][:], BR[0:32, :], sinb[cs][:], op=ALU.mult), reads=[j['kr'], ('sin', cs)], writes=[('t2', cs)])
            P.op('dve', lambda E: E.tensor_tensor(dst[0:32, :], qn[cs][0:32, :], t2[cs][:], op=ALU.add), reads=[('qn', cs), ('t2', cs)], writes=[(kind + 'T', tl)])

        for s in range(slots):
            for g in range(3):
                d = DILS[g]
                J = OWN // (128 * d)
                hk = g * 4 + s
                pieces = []
                for (wt, nm, c0) in ((wq, 'wq', C_AQ), (wk, 'wk', C_AK), (wv, 'wv', C_AV)):
                    for k in range(8):
                        pieces.append((wt[:, k, :], w_in_r[:, k, c0 + hk * 128:c0 + (hk + 1) * 128], (nm, k)))
                load_cast(P, stage, pieces, engs=('pool', 'act'))
                ktl0 = 0 if d == 16 else 3
                ktiles = list(range(ktl0, 12))
                jl = [('k', tl) for tl in ktiles] + [('q', tl) for tl in range(4, 12)]
                jobs = [jobinfo(kd, tl, cnt[0] + ix) for ix, (kd, tl) in enumerate(jl)]
                cnt[0] += len(jobs)
                nj = len(jobs)
                for t in range(nj + 2):
                    if t < nj:
                        stA(jobs[t])
                    if 0 <= t - 1 < nj:
                        stB(jobs[t - 1])
                    if 0 <= t - 2 < nj:
                        stC(jobs[t - 2])
                kkeys = [('kT', tl) for tl in ktiles] + [('kTb', tl) for tl in ktiles] + [('kTc', tl) for tl in ktiles]
                qkeys = [('qT', tl) for tl in range(4, 12)] + [('qTb', tl) for tl in range(4, 12)] + [('qTc', tl) for tl in range(4, 12)]
                nblk = d * (J + 1)
                blks = [(r, j) for r in range(d) for j in range(-1, J)]
                for b0 in range(0, nblk, 4):
                    grp = blks[b0:b0 + 4]

                    vb = (b0 // 4) % 2
                    BVb = BK[2 + vb]

                    def vf(E, grp=grp, BVb=BVb):
                        ins = None
                        for qi, (r, j) in enumerate(grp):
                            u0 = 2048 // d + 128 * j
                            for k in range(8):
                                lhs = ures[:, k, :].rearrange("p (u d) -> p d u", d=d)[:, r, u0:u0 + 128]
                                ins = E.matmul(BVb[:, qi * 128:(qi + 1) * 128], lhs, wv[:, k, :], start=(k == 0), stop=(k == 7))
                        return ins
                    P.op('pe', vf, reads=[('wv', k) for k in range(8)] + ures_all, writes=[('bk', 2 + vb)])
                    n = len(grp)
                    P.op('act' if vb == 0 else 'dve', (lambda E, b0=b0, n=n, BVb=BVb: E.copy(out=vt[:, b0:b0 + n, :], in_=BVb[:, 0:n * 128].rearrange("p (b e) -> p b e", e=128))) if vb == 0 else
                         (lambda E, b0=b0, n=n, BVb=BVb: E.tensor_copy(vt[:, b0:b0 + n, :], BVb[:, 0:n * 128].rearrange("p (b e) -> p b e", e=128))),
                         reads=[('bk', 2 + vb)], writes=[('vt', b0)])
                kview = kT[:, :].rearrange("p (u d) -> p d u", d=d)
                qview = qT[:, :].rearrange("p (u d) -> p d u", d=d)
                accv = acc[:, :, :].rearrange("p n (u d) -> p n d u", d=d)
                it = 0
                pend = None
                for r in range(d):
                    for jp in range(J // 2):
                        j0 = 2 * jp
                        b = it % 2
                        it += 1

                        def sf(E, r=r, j0=j0, b=b):
                            ins = None
                            for qb in range(2):
                                j = j0 + qb
                                qa = qview[:, r, 128 * j:128 * j + 128]
                                for pc in range(2):
                                    jj = j - 1 + pc
                                    u0 = 2048 // d + 128 * jj
                                    ka = kview[:, r, u0:u0 + 128]
                                    ins = E.matmul(BSC[b][:, (qb * 2 + pc) * 128:(qb * 2 + pc + 1) * 128], ka, qa, start=True, stop=True)
                            return ins
                        P.op('pe', sf, reads=kkeys + qkeys, writes=[('bk', b)])
                        P.op('act', lambda E, b=b: E.activation(out=pT[b][:], in_=BSC[b][:], func=AF.Exp, scale=SCALE), reads=[('bk', b)], writes=[('pT', b)])
                        P.op('pool' if b == 0 else 'dve', lambda E, b=b: E.tensor_tensor(pT[b][:], pT[b][:], mask2[:], op=ALU.mult), reads=[('pT', b), 'mask2'], writes=[('pT', b)])

                        def fin(r=r, j0=j0, b=b):
                            def nf(E):
                                ins = None
                                for qb in range(2):
                                    j = j0 + qb
                                    bp = r * (J + 1) + j
                                    bc = bp + 1
                                    pp = pT[b][:, (qb * 2) * 128:(qb * 2 + 1) * 128]
                                    pcur = pT[b][:, (qb * 2 + 1) * 128:(qb * 2 + 2) * 128]
                                    E.matmul(BN[b][:, qb * 128:(qb + 1) * 128], vt[:, bp, :], pp, start=True, stop=False)
                                    E.matmul(BN[b][:, qb * 128:(qb + 1) * 128], vt[:, bc, :], pcur, start=False, stop=True)
                                    E.matmul(BN[b][:, (2 + qb) * 128:(3 + qb) * 128], (fones if j == 0 else ones_bf)[:], pp, start=True, stop=False)
                                    ins = E.matmul(BN[b][:, (2 + qb) * 128:(3 + qb) * 128], ones_bf[:], pcur, start=False, stop=True)
                                return ins
                            P.op('pe', nf, reads=[('vt', (bb // 4) * 4) for bb in (r * (J + 1) + j0, r * (J + 1) + j0 + 1, r * (J + 1) + j0 + 2)] + [('pT', b), 'onesb', 'fones'], writes=[('bk', 2 + b)])
                            av = accv[:, :, r, 128 * j0:128 * j0 + 256]
                            bnv = BN[b][:].rearrange("p (n x) -> p n x", n=2)
                            if g == 0:
                                P.op('act', lambda E: E.copy(out=av, in_=bnv), reads=[('bk', 2 + b)], writes=['acc'])
                            else:
                                P.op('dve', lambda E: E.tensor_tensor(av, av, bnv, op=ALU.add), reads=[('bk', 2 + b), 'acc'], writes=['acc'])
                        if pend is not None:
                            pend()
                        pend = fin
                if pend is not None:
                    pend()
            for tl in range(8):
                o = tl % 2
                P.op('act', lambda E: E.activation(out=rden[o][:], in_=acc[:, 1, tl * TT:(tl + 1) * TT], func=AF.Ln), reads=['acc'], writes=[('rden', o)])
                P.op('act', lambda E: E.activation(out=rden[o][:], in_=rden[o][:], func=AF.Exp, scale=-1.0), reads=[('rden', o)], writes=[('rden', o)])
                P.op('dve', lambda E: E.tensor_tensor(atto[o][:], acc[:, 0, tl * TT:(tl + 1) * TT], rden[o][:], op=ALU.mult), reads=['acc', ('rden', o)], writes=[('atto', o)])
                P.op('sp', lambda E: E.dma_start(out=att_r[:, s, tl * TT:(tl + 1) * TT], in_=atto[o][:]), reads=[('atto', o)], dsem="p2b_ao%d" % o)
        P.barrier()


def build(debug=0):
    nc = bass.Bass("TRN2", target_bir_lowering=False)
    T = {}

    def din(name, shape, dt=F32):
        T[name] = nc.dram_tensor(name, shape, dt, kind="ExternalInput").ap()

    def scratch(name, shape, dt):
        kind = {"kind": "ExternalOutput"} if debug else {}
        T[name] = nc.dram_tensor(name, shape, dt, **kind).ap()
    din('xT', [D, NTOK])
    din('pT', [256, OWN])
    din('ffn1_norm', [128, 8]); din('mix_norm', [128, 8]); din('ffn2_norm', [128, 8]); din('ple_norm', [128, 8])
    din('ffn1_w_in', [D, 2 * DFF]); din('ffn1_w_out', [DFF, D])
    din('ffn2_w_in', [D, 2 * DFF]); din('ffn2_w_out', [DFF, D])
    din('w_in', [D, DIN])
    din('w_up_att', [512, D]); din('w_up_mlstm', [512, D]); din('w_out', [D, D])
    din('w_ple_gate', [D, D]); din('w_ple_proj', [256, D])
    din('conv_w', [128, 8, 4]); din('conv_b', [128, 8]); din('i_bias', [4, 1]); din('f_bias', [128, 4])
    din('q_gain', [128, 1]); din('k_gain', [128, 1])
    din('c_ident', [128, 128]); din('c_tri', [128, 128]); din('c_trit', [128, 128]); din('c_flag', [128, 1])
    din('c_rm', [32, 32]); din('c_cos', [32, 6144]); din('c_sin', [32, 6144])
    T['outT'] = nc.dram_tensor('outT', [D, OWN], F32, kind="ExternalOutput").ap()
    scratch('h1', [D, OWN], F32)
    scratch('u', [D, NTOK], BF16)
    scratch('att', [512, OWN], BF16)
    scratch('hg', [512, OWN], BF16)
    scratch('h2', [D, OWN], F32)
    stages = os.environ.get('KSTAGES', '1abcd')
    with ExitStack() as es:
        P = Prog(nc, es)
        if '1' in stages:
            ffn_phase(P, nc, T, 1)
        if 'a' in stages:
            mlstm_phase(P, nc, T)
        if 'b' in stages:
            attn_phase(P, nc, T)
        if 'c' in stages:
            merge_phase(P, nc, T)
        if 'd' in stages:
            ffn_phase(P, nc, T, 2)
    return nc


def _chunk_major(v):
    return np.ascontiguousarray(v.reshape(-1, 128).T).astype(np.float32)


def make_in_maps(inputs):
    x = np.asarray(inputs['x'], dtype=np.float32)
    p = np.asarray(inputs['p'], dtype=np.float32)[0]
    shared = {
        'ffn1_norm': _chunk_major(inputs['ffn1_norm'][0]), 'mix_norm': _chunk_major(inputs['mix_norm'][0]),
        'ffn2_norm': _chunk_major(inputs['ffn2_norm'][0]), 'ple_norm': _chunk_major(inputs['ple_norm'][0]),
        'ffn1_w_in': np.ascontiguousarray(inputs['ffn1_w_in'][0]), 'ffn1_w_out': np.ascontiguousarray(inputs['ffn1_w_out'][0]),
        'ffn2_w_in': np.ascontiguousarray(inputs['ffn2_w_in'][0]), 'ffn2_w_out': np.ascontiguousarray(inputs['ffn2_w_out'][0]),
        'w_in': np.ascontiguousarray(inputs['w_in'][0]),
        'w_up_att': np.ascontiguousarray(inputs['w_up_att'][0]), 'w_up_mlstm': np.ascontiguousarray(inputs['w_up_mlstm'][0]),
        'w_out': np.ascontiguousarray(inputs['w_out'][0]),
        'w_ple_gate': np.ascontiguousarray(inputs['w_ple_gate'][0]), 'w_ple_proj': np.ascontiguousarray(inputs['w_ple_proj'][0]),
    }
    cw = np.asarray(inputs['conv_w'][0], np.float32)
    shared['conv_w'] = np.ascontiguousarray(cw.reshape(4, 8, 128).transpose(2, 1, 0))
    shared['conv_b'] = _chunk_major(inputs['conv_b'][0])
    shared['i_bias'] = np.asarray(inputs['i_bias'][0], np.float32).reshape(4, 1).copy()
    shared['f_bias'] = np.ascontiguousarray(np.broadcast_to(np.asarray(inputs['f_bias'][0], np.float32)[None, :], (128, 4)))
    shared['q_gain'] = np.asarray(inputs['q_gain'][0], np.float32).reshape(128, 1).copy()
    shared['k_gain'] = np.asarray(inputs['k_gain'][0], np.float32).reshape(128, 1).copy()
    shared['c_ident'] = np.eye(128, dtype=np.float32)
    tri = np.triu(np.ones((128, 128), np.float32))
    shared['c_tri'] = tri
    shared['c_trit'] = np.ascontiguousarray(tri.T)
    rmm = np.zeros((32, 32), np.float32)
    for m_ in range(32):
        rmm[(m_ + 16) % 32, m_] = 1.0
    shared['c_rm'] = rmm
    half = 16
    inv_freq = (1.0 / (np.float32(500000.0) ** (np.arange(half, dtype=np.float32) / np.float32(half)))).astype(np.float32)
    maps = []
    for c in range(8):
        b, h = c // 2, c % 2
        xT = np.zeros((D, NTOK), np.float32)
        xT[:, OWN:] = x[b, h * OWN:(h + 1) * OWN].T
        if h == 1:
            xT[:, :OWN] = x[b, :OWN].T
        m = dict(shared)
        m['xT'] = xT
        m['pT'] = np.ascontiguousarray(p[b, h * OWN:(h + 1) * OWN].T)
        m['c_flag'] = np.full((128, 1), float(h), np.float32)
        pos = (np.arange(2048, 8192) - 4096 + 4096 * h).astype(np.float32)
        ang = (pos[None, :] * inv_freq[:, None]).astype(np.float32)
        cs_, sn_ = np.cos(ang).astype(np.float32), np.sin(ang).astype(np.float32)
        m['c_cos'] = np.ascontiguousarray(np.concatenate([cs_, cs_], axis=0))
        m['c_sin'] = np.ascontiguousarray(np.concatenate([-sn_, sn_], axis=0))
        maps.append(m)
    return maps


def kernel(**inputs):
    nc = build(0)
    maps = make_in_maps(inputs)
    res = run_bass_kernel_spmd(nc, maps, core_ids=list(range(8)))
    out = np.empty((4, 8192, D), np.float32)
    for c in range(8):
        b, h = c // 2, c % 2
        out[b, h * OWN:(h + 1) * OWN] = res.results[c]['outT'].T
    return out
```

```python
import os
import numpy as np
from contextlib import ExitStack
import concourse.bass as bass
import concourse.mybir as mybir
from concourse.bass_utils import run_bass_kernel_spmd

F32 = mybir.dt.float32
BF16 = mybir.dt.bfloat16
AF = mybir.ActivationFunctionType
ALU = mybir.AluOpType
AX = mybir.AxisListType

D = 1024
DFF = 2816
NTOK = 8192
OWN = 4096
DIN = 8712
EPS = 1e-6
C_AQ, C_AK, C_AV = 0, 1536, 3072
C_MQ, C_MK, C_MV, C_MO, C_MI, C_MF, C_GA, C_GB = 4608, 5120, 5632, 6144, 6656, 6660, 6664, 7688
DILS = (1, 4, 16)


class Sem:
    def __init__(self, h):
        self.h = h
        self.n = 0


class Prog:
    ENG = ('pe', 'act', 'dve', 'pool', 'sp')

    def __init__(self, nc, es):
        self.nc, self.es = nc, es
        self.e = {'pe': nc.tensor, 'act': nc.scalar, 'dve': nc.vector, 'pool': nc.gpsimd, 'sp': nc.sync}
        self.prog = {}
        self.seen = {k: {} for k in self.ENG}
        self.lastw = {}
        self.rd = {}
        self.nsem = 0
        self.allsems = []
        self.dsems = {}
        self.new_epoch()

    def mksem(self):
        self.nsem += 1
        s = Sem(self.es.enter_context(self.nc.semaphore("s%d" % self.nsem)))
        self.allsems.append(s)
        return s

    def new_epoch(self):
        for k in self.ENG:
            self.prog[k] = self.mksem()

    def dsem(self, name):
        if name not in self.dsems:
            self.dsems[name] = self.mksem()
        return self.dsems[name]

    def op(self, eng, fn, reads=(), writes=(), dsem=None):
        need = {}

        def add(t):
            if t is None:
                return
            sem, v = t
            if need.get(sem, 0) < v:
                need[sem] = v
        for k in reads:
            add(self.lastw.get(k))
        for k in writes:
            add(self.lastw.get(k))
            for sem, v in self.rd.get(k, {}).items():
                add((sem, v))
        E = self.e[eng]
        for sem, v in need.items():
            if eng == 'pe' and sem is self.prog['pe']:
                continue
            if self.seen[eng].get(sem, 0) >= v:
                continue
            E.wait_ge(sem.h, v)
            self.seen[eng][sem] = v
        ins = fn(E)
        if dsem is not None:
            sem = self.dsem(dsem) if isinstance(dsem, str) else dsem
            sem.n += 16
            ins.then_inc(sem.h, 16)
        else:
            sem = self.prog[eng]
            sem.n += 1
            ins.then_inc(sem.h, 1)
        tok = (sem, sem.n)
        for k in reads:
            d = self.rd.setdefault(k, {})
            if d.get(sem, 0) < sem.n:
                d[sem] = sem.n
        for k in writes:
            self.lastw[k] = tok
            self.rd[k] = {}
        return tok

    def barrier(self):
        for eng in self.ENG:
            E = self.e[eng]
            for sem in self.allsems:
                if sem.n > self.seen[eng].get(sem, 0):
                    E.wait_ge(sem.h, sem.n)
                    self.seen[eng][sem] = sem.n
        self.lastw = {}
        self.rd = {}
        self.new_epoch()


def mm_group(P, out, pairs, reads, writes):
    n = len(pairs)

    def fn(E):
        ins = None
        for i, (l, r) in enumerate(pairs):
            ins = E.matmul(out, l, r, start=(i == 0), stop=(i == n - 1))
        return ins
    return P.op('pe', fn, reads=reads, writes=writes)


def load_w_groups(P, dst, src, ngroups, nj, key, eng='pool'):
    bounds = [round(i * nj / ngroups) for i in range(ngroups + 1)]
    for g in range(ngroups):
        j0, j1 = bounds[g], bounds[g + 1]
        if j1 == j0:
            continue
        ks = [(key, j) for j in range(j0, j1)]
        P.op(eng, lambda E, j0=j0, j1=j1: E.dma_start(out=dst[:, :, j0 * 128:j1 * 128], in_=src[:, :, j0 * 128:j1 * 128]),
             writes=ks, dsem="%s_g%d" % (key, g))


def load_cast(P, stage, pieces, engs=('pool', 'act')):
    for (dst, src, key) in pieces:
        i = P.lc_i = getattr(P, 'lc_i', -1) + 1
        sl = i % len(stage)
        n = dst.shape[-1]
        st = stage[sl][:, 0:n]
        P.op('sp', lambda E: E.dma_start(out=st, in_=src), writes=[('stg', sl)], dsem="stg%d" % sl)
        eng = engs[i % len(engs)]
        if eng == 'act':
            P.op('act', lambda E: E.copy(out=dst, in_=st), reads=[('stg', sl)], writes=[key])
        else:
            P.op(eng, lambda E: E.tensor_copy(dst, st), reads=[('stg', sl)], writes=[key])


def rms_stats(P, nc, src_aps, skeys, sq, sqk, ones_bf, ps_ap, psk, tmp, tmpk, rstd, rstdk, inv_n, eps_ap=None):
    nchunk = len(src_aps)
    for c in range(nchunk):
        s = sq[c % len(sq)]
        sk = sqk[c % len(sq)]
        P.op('act', lambda E, s=s, c=c: E.activation(out=s, in_=src_aps[c], func=AF.Square),
             reads=[skeys[c]], writes=[sk])
        P.op('pe', lambda E, s=s, c=c: E.matmul(ps_ap, ones_bf, s, start=(c == 0), stop=(c == nchunk - 1)),
             reads=[sk, 'ones'], writes=[psk])
    P.op('act', lambda E: E.activation(out=tmp, in_=ps_ap, func=AF.Ln, bias=eps_ap, scale=inv_n), reads=[psk, 'epsc'], writes=[tmpk])
    P.op('act', lambda E: E.activation(out=rstd, in_=tmp, func=AF.Exp, scale=-0.5), reads=[tmpk], writes=[rstdk])


def ffn_phase(P, nc, T, which):
    TT = 256
    first = (which == 1)
    ntiles = (NTOK if first else OWN) // TT
    ntiles = int(os.environ.get('KNT', ntiles))
    own0 = (NTOK - OWN) // TT if first else 0
    w_in = T['ffn1_w_in'] if first else T['ffn2_w_in']
    w_out = T['ffn1_w_out'] if first else T['ffn2_w_out']
    src = T['xT'] if first else T['h2']
    with ExitStack() as ph:
        def sb(name, shape, dt):
            return ph.enter_context(nc.sbuf_tensor("p%d_%s" % (which, name), shape, dt))

        def pst(name, shape, dt=F32):
            return ph.enter_context(nc.psum_tensor("p%d_ps_%s" % (which, name), shape, dt))
        w1 = sb("w1", [128, 8, 2 * DFF], BF16)
        w2 = sb("w2", [128, 22, D], BF16)
        g1 = sb("g1", [128, 8], F32)
        g2 = sb("g2", [128, 8], F32)
        ones_bf = sb("ones", [128, 128], BF16)
        epsc = sb("epsc", [128, 1], F32)
        xh = [sb("xh%d" % i, [128, 8, TT], F32) for i in range(3)]
        xn = [sb("xn%d" % i, [128, 8, TT], BF16) for i in range(2)]
        un = [sb("un%d" % i, [128, 8, TT], BF16) for i in range(2)] if first else xn
        unk = 'un' if first else 'xn'
        sq = [sb("sq%d" % i, [128, TT], BF16) for i in range(2)]
        act = sb("act", [128, 22, TT], BF16)
        sil = [sb("sil%d" % i, [128, TT], BF16) for i in range(2)]
        tmp = [sb("tmp%d" % i, [128, TT], F32) for i in range(2 if first else 1)]
        rstd = [sb("rstd%d" % i, [128, TT], F32) for i in range(2)]
        stage = [sb("stg%d" % i, [128, 512], F32) for i in range(3 if first else 2)]
        ps_ss = pst("ss", [128, 512])
        ps_a = [pst("a%d" % i, [128, 512]) for i in range(2)]
        ps_o = [pst("o%d" % i, [128, 512]) for i in range(2)]
        if not first:
            wpg = sb("wpg", [128, 8, D], BF16)
            wpe = sb("wpe", [128, 2, D], BF16)
            pt = [sb("pt%d" % i, [128, 2, TT], BF16) for i in range(2)]
            sg = [sb("sg%d" % i, [128, TT], F32) for i in range(1)]
            ps_g = [pst("g%d" % i, [128, 512]) for i in range(2)]

        P.op('pool', lambda E: E.memset(ones_bf[:], 1.0), writes=['ones'])
        P.op('pool', lambda E: E.memset(epsc[:], EPS), writes=['epsc'])
        P.op('sp', lambda E: E.dma_start(out=g1[:], in_=T['ffn1_norm' if first else 'ffn2_norm'][:, :]), writes=['g1'], dsem="p%d_g1" % which)
        P.op('sp', lambda E: E.dma_start(out=g2[:], in_=T['mix_norm' if first else 'ple_norm'][:, :]), writes=['g2'], dsem="p%d_g2" % which)
        w_in_r = w_in.rearrange("(c p) n -> p c n", p=128)
        w_out_r = w_out.rearrange("(j p) n -> p j n", p=128)
        w1_groups = []
        for jg in range(0, 22, 4):
            j1 = min(jg + 4, 22)
            grp = []
            for part in range(2):
                for k in range(8):
                    c0 = part * DFF + jg * 128
                    c1 = part * DFF + j1 * 128
                    grp.append((w1[:, k, c0:c1], w_in_r[:, k, c0:c1], ('w1', part, k, jg // 4)))
            w1_groups.append(grp)
        late = []
        for j in range(22):
            for hh in range(2):
                late.append((w2[:, j, hh * 512:(hh + 1) * 512], w_out_r[:, j, hh * 512:(hh + 1) * 512], ('w2', j, hh)))
        if not first:
            wpg_r = T['w_ple_gate'].rearrange("(c p) n -> p c n", p=128)
            wpe_r = T['w_ple_proj'].rearrange("(c p) n -> p c n", p=128)
            for k in range(8):
                for hh in range(2):
                    late.append((wpg[:, k, hh * 512:(hh + 1) * 512], wpg_r[:, k, hh * 512:(hh + 1) * 512], ('wpg', k, hh)))
            for k in range(2):
                for hh in range(2):
                    late.append((wpe[:, k, hh * 512:(hh + 1) * 512], wpe_r[:, k, hh * 512:(hh + 1) * 512], ('wpe', k, hh)))
        load_cast(P, stage, w1_groups[0], engs=('pool', 'dve'))
        load_cast(P, stage, w1_groups[1], engs=('pool', 'dve'))

        src_r = src.rearrange("(c p) n -> p c n", p=128)

        def load_x(i):
            s = i % 3
            P.op('sp', lambda E: E.dma_start(out=xh[s][:], in_=src_r[:, :, i * TT:(i + 1) * TT]),
                 writes=[('xh', s, c) for c in range(8)], dsem="p%d_xh%d" % (which, s))
        def load_p(i):
            if not first:
                s2 = i % 2
                for kk in range(2):
                    P.op('pool', lambda E, kk=kk: E.dma_start(out=pt[s2][:, kk, :], in_=T['pT'][kk * 128:(kk + 1) * 128, i * TT:(i + 1) * TT]),
                         writes=[('pt', s2)], dsem="p2_pt%d" % s2)

        def norm(i, gain, gkey, dst, dkey):
            s = i % 3
            d = i % 2
            rms_stats(P, nc, [xh[s][:, c, :] for c in range(8)], [('xh', s, c) for c in range(8)],
                      [q[:] for q in sq], [('sq', 0), ('sq', 1)], ones_bf[:], ps_ss[:, 0:TT], 'ps_ss',
                      tmp[d % len(tmp)][:], ('tmp', d % len(tmp)), rstd[d][:], ('rstd', d), 1.0 / D, eps_ap=epsc[:, 0:1])
            for c in range(8):
                P.op('dve', lambda E, c=c: E.scalar_tensor_tensor(dst[d][:, c, :], xh[s][:, c, :], gain[:, c:c + 1], rstd[d][:],
                                                                   op0=ALU.mult, op1=ALU.mult),
                     reads=[('xh', s, c), gkey, ('rstd', d)], writes=[(dkey, d, c)])

        def ffn_in(i):
            d = i % 2
            for j in range(22):
                b = j % 2
                if i == 0 and j % 4 == 0:
                    g_ = j // 4
                    if g_ + 2 < len(w1_groups):
                        load_cast(P, stage, w1_groups[g_ + 2], engs=('pool',))
                    nl = 12 if g_ < 5 else len(late)
                    load_cast(P, stage, late[:nl], engs=('pool',))
                    del late[:nl]
                pa = ps_a[b]
                mm_group(P, pa[:, 0:TT], [(w1[:, k, j * 128:(j + 1) * 128], xn[d][:, k, :]) for k in range(8)],
                         reads=[('w1', 0, k, j // 4) for k in range(8)] + [('xn', d, k) for k in range(8)], writes=[('bka', b)])
                mm_group(P, pa[:, TT:2 * TT], [(w1[:, k, DFF + j * 128:DFF + (j + 1) * 128], xn[d][:, k, :]) for k in range(8)],
                         reads=[('w1', 1, k, j // 4) for k in range(8)] + [('xn', d, k) for k in range(8)], writes=[('bka', b)])
                P.op('act', lambda E, b=b, pa=pa: E.activation(out=sil[b][:], in_=pa[:, 0:TT], func=AF.Silu),
                     reads=[('bka', b)], writes=[('sil', b)])
                P.op('dve', lambda E, b=b, pa=pa, j=j: E.tensor_tensor(act[:, j, :], sil[b][:], pa[:, TT:2 * TT], op=ALU.mult),
                     reads=[('sil', b), ('bka', b)], writes=[('act', j)])

        def ffn_out(i):
            s = i % 3
            for pr in range(4):
                bk = pr % 2
                for hf in range(2):
                    oc = pr * 2 + hf
                    po = ps_o[bk][:, hf * TT:(hf + 1) * TT]
                    mm_group(P, po, [(w2[:, j, oc * 128:(oc + 1) * 128], act[:, j, :]) for j in range(22)],
                             reads=[('w2', j, oc // 4) for j in range(22)] + [('act', j) for j in range(22)], writes=[('bko', bk)])
                for hf in range(2):
                    oc = pr * 2 + hf
                    po = ps_o[bk][:, hf * TT:(hf + 1) * TT]
                    P.op('dve', lambda E, oc=oc, po=po: E.scalar_tensor_tensor(xh[s][:, oc, :], po, 0.5, xh[s][:, oc, :], op0=ALU.mult, op1=ALU.add),
                         reads=[('bko', bk), ('xh', s, oc)], writes=[('xh', s, oc)])

        def ple(i):
            s = i % 3
            d = i % 2
            for oc in range(8):
                b = oc % 2
                pg = ps_g[b]
                mm_group(P, pg[:, 0:TT], [(wpg[:, k, oc * 128:(oc + 1) * 128], un[d][:, k, :]) for k in range(8)],
                         reads=[('wpg', k, oc // 4) for k in range(8)] + [(unk, d, k) for k in range(8)], writes=[('bkg', b)])
                mm_group(P, pg[:, TT:2 * TT], [(wpe[:, k, oc * 128:(oc + 1) * 128], pt[d][:, k, :]) for k in range(2)],
                         reads=[('wpe', k, oc // 4) for k in range(2)] + [('pt', d)], writes=[('bkg', b)])
                P.op('act', lambda E, b=b, pg=pg: E.activation(out=sg[0][:], in_=pg[:, 0:TT], func=AF.Sigmoid),
                     reads=[('bkg', b)], writes=[('sg', 0)])
                P.op('dve', lambda E, b=b, pg=pg: E.tensor_tensor(sg[0][:], sg[0][:], pg[:, TT:2 * TT], op=ALU.mult),
                     reads=[('sg', 0), ('bkg', b)], writes=[('sg', 0)])
                P.op('dve', lambda E, b=b, oc=oc: E.tensor_tensor(xh[s][:, oc, :], sg[0][:], xh[s][:, oc, :], op=ALU.add),
                     reads=[('sg', 0), ('xh', s, oc)], writes=[('xh', s, oc)])
            P.op('sp', lambda E: E.dma_start(out=T['outT'].rearrange("(c p) n -> p c n", p=128)[:, :, i * TT:(i + 1) * TT], in_=xh[s][:]),
                 reads=[('xh', s, oc) for oc in range(8)], dsem="p2_ot%d" % s)

        KSTOP = os.environ.get('KSTOP', '')
        if KSTOP == 'w':
            P.barrier(); return
        load_x(0)
        load_x(1)
        load_p(0)
        norm(0, g1, 'g1', xn, 'xn')
        if first:
            for i in range(ntiles):
                if i + 2 < ntiles:
                    load_x(i + 2)
                ffn_in(i)
                if i + 1 < ntiles:
                    norm(i + 1, g1, 'g1', xn, 'xn')
                ffn_out(i)
                s = i % 3
                if i >= own0:
                    io = i - own0
                    P.op('sp', lambda E: E.dma_start(out=T['h1'].rearrange("(c p) n -> p c n", p=128)[:, :, io * TT:(io + 1) * TT], in_=xh[s][:]),
                         reads=[('xh', s, c) for c in range(8)], dsem="p1_h%d" % s)
                norm(i, g2, 'g2', un, unk)
                d = i % 2
                P.op('sp', lambda E: E.dma_start(out=T['u'].rearrange("(c p) n -> p c n", p=128)[:, :, i * TT:(i + 1) * TT], in_=un[d][:]),
                     reads=[('un', d, c) for c in range(8)], dsem="p1_u%d" % d)
        else:
            for i in range(ntiles):
                ffn_in(i)
                if i > 0:
                    ple(i - 1)
                if i + 2 < ntiles:
                    load_x(i + 2)
                if i + 1 < ntiles:
                    load_p(i + 1)
                    norm(i + 1, g1, 'g1', xn, 'xn')
                ffn_out(i)
                norm(i, g2, 'g2', un, unk)
            ple(ntiles - 1)
        P.barrier()


def merge_phase(P, nc, T):
    TT = 512
    ntiles = int(os.environ.get('KNT3', OWN // TT))
    with ExitStack() as ph:
        def sb(name, shape, dt):
            return ph.enter_context(nc.sbuf_tensor("p3_" + name, shape, dt))

        def pst(name, shape, dt=F32):
            return ph.enter_context(nc.psum_tensor("p3_" + name, shape, dt))
        wg = sb("wg", [128, 8, 2048], BF16)
        wua = sb("wua", [128, 4, D], BF16)
        wub = sb("wub", [128, 4, D], BF16)
        wo = sb("wo", [128, 8, D], BF16)
        stage = [sb("stg%d" % i, [128, 512], F32) for i in range(3)]
        ut = [sb("ut%d" % i, [128, 8, TT], BF16) for i in range(2)]
        at = [sb("at%d" % i, [128, 4, TT], BF16) for i in range(2)]
        mt = [sb("mt%d" % i, [128, 4, TT], BF16) for i in range(2)]
        ht = [sb("ht%d" % i, [128, 8, TT], F32) for i in range(2)]
        mg = sb("mg", [128, 8, TT], BF16)
        sg = [sb("sg%d" % i, [128, TT], F32) for i in range(2)]
        m1 = [sb("m1%d" % i, [128, TT], F32) for i in range(2)]
        psg = [pst("g%d" % i, [128, 512]) for i in range(2)]
        psy = [pst("y%d" % i, [128, 512]) for i in range(2)]
        pso = [pst("o%d" % i, [128, 512]) for i in range(2)]
        w_in_r = T['w_in'].rearrange("(c p) n -> p c n", p=128)
        pieces = []
        for k in range(8):
            for q in range(4):
                pieces.append((wg[:, k, q * 512:(q + 1) * 512], w_in_r[:, k, C_GA + q * 512:C_GA + (q + 1) * 512], ('wg', k, q)))
        for nm, wt, src in (('wua', wua, T['w_up_att']), ('wub', wub, T['w_up_mlstm'])):
            r = src.rearrange("(c p) n -> p c n", p=128)
            for k in range(4):
                for q in range(2):
                    pieces.append((wt[:, k, q * 512:(q + 1) * 512], r[:, k, q * 512:(q + 1) * 512], (nm, k, q)))
        r = T['w_out'].rearrange("(c p) n -> p c n", p=128)
        for k in range(8):
            for q in range(2):
                pieces.append((wo[:, k, q * 512:(q + 1) * 512], r[:, k, q * 512:(q + 1) * 512], ('wo', k, q)))
        load_cast(P, stage, pieces)
        u_r = T['u'].rearrange("(c p) n -> p c n", p=128)
        a_r = T['att'].rearrange("(c p) n -> p c n", p=128)
        m_r = T['hg'].rearrange("(c p) n -> p c n", p=128)
        h_r = T['h1'].rearrange("(c p) n -> p c n", p=128)
        o_r = T['h2'].rearrange("(c p) n -> p c n", p=128)

        def loads(i):
            d = i % 2
            P.op('sp', lambda E: E.dma_start(out=ut[d][:], in_=u_r[:, :, OWN + i * TT:OWN + (i + 1) * TT]), writes=[('ut', d)], dsem="p3_ut%d" % d)
            P.op('sp', lambda E: E.dma_start(out=at[d][:], in_=a_r[:, :, i * TT:(i + 1) * TT]), writes=[('at', d)], dsem="p3_at%d" % d)
            P.op('sp', lambda E: E.dma_start(out=mt[d][:], in_=m_r[:, :, i * TT:(i + 1) * TT]), writes=[('mt', d)], dsem="p3_mt%d" % d)
            P.op('sp', lambda E: E.dma_start(out=ht[d][:], in_=h_r[:, :, i * TT:(i + 1) * TT]), writes=[('ht', d, c) for c in range(8)], dsem="p3_ht%d" % d)
        loads(0)
        for i in range(ntiles):
            d = i % 2
            if i + 1 < ntiles:
                loads(i + 1)
            for oc in range(8):
                for br in range(2):
                    b = br
                    wy, ykey, yt, ytk = (wua, 'wua', at, 'at') if br == 0 else (wub, 'wub', mt, 'mt')
                    mm_group(P, psg[b][:], [(wg[:, k, br * 1024 + oc * 128:br * 1024 + (oc + 1) * 128], ut[d][:, k, :]) for k in range(8)],
                             reads=[('wg', k, (br * 1024 + oc * 128) // 512) for k in range(8)] + [('ut', d)], writes=[('bkg', b)])
                    mm_group(P, psy[b][:], [(wy[:, k, oc * 128:(oc + 1) * 128], yt[d][:, k, :]) for k in range(4)],
                             reads=[(ykey, k, oc // 4) for k in range(4)] + [(ytk, d)], writes=[('bky', b)])
                    P.op('act', lambda E: E.activation(out=sg[b][:], in_=psg[b][:], func=AF.Sigmoid), reads=[('bkg', b)], writes=[('sg', b)])
                    P.op('dve', lambda E: E.tensor_tensor(m1[b][:], sg[b][:], psy[b][:], op=ALU.mult), reads=[('sg', b), ('bky', b)], writes=[('m1', b)])
                P.op('pool', lambda E: E.tensor_tensor(mg[:, oc, :], m1[0][:], m1[1][:], op=ALU.add), reads=[('m1', 0), ('m1', 1)], writes=[('mg', oc)])
            for oc in range(8):
                b = oc % 2
                mm_group(P, pso[b][:], [(wo[:, k, oc * 128:(oc + 1) * 128], mg[:, k, :]) for k in range(8)],
                         reads=[('wo', k, oc // 4) for k in range(8)] + [('mg', k) for k in range(8)], writes=[('bko', b)])
                P.op('dve', lambda E: E.tensor_tensor(ht[d][:, oc, :], pso[b][:], ht[d][:, oc, :], op=ALU.add),
                     reads=[('bko', b), ('ht', d, oc)], writes=[('ht', d, oc)])
            P.op('sp', lambda E: E.dma_start(out=o_r[:, :, i * TT:(i + 1) * TT], in_=ht[d][:]), reads=[('ht', d, c) for c in range(8)], dsem="p3_o%d" % d)
        P.barrier()


def mlstm_phase(P, nc, T):
    TT = 512
    ntiles = NTOK // TT
    own0 = (NTOK - OWN) // TT
    t_start = int(os.environ.get('KT2A0', 0))
    t_end = int(os.environ.get('KT2A1', ntiles))
    SC = 128 ** -0.5
    with ExitStack() as ph:
        def sb(name, shape, dt):
            return ph.enter_context(nc.sbuf_tensor("p2a_" + name, shape, dt))

        def pst(name, shape, dt=F32):
            return ph.enter_context(nc.psum_tensor("p2a_" + name, shape, dt))
        wm = sb("wm", [128, 8, 2048], BF16)
        wgt = sb("wgt", [128, 8, 8], BF16)
        wgf = sb("wgf", [128, 8, 8], F32)
        stage = [sb("stg%d" % i, [128, 512], F32) for i in range(3)]
        ident_f = sb("identf", [128, 128], F32)
        tri_f = sb("trif", [128, 128], F32)
        ident_bf = sb("identb", [128, 128], BF16)
        mask_bf = sb("maskb", [128, 128], BF16)
        ones_bf = sb("onesb", [128, 128], BF16)
        ones4 = sb("ones4", [4, 128], F32)
        selc = sb("selc", [4, 4, 128], F32)
        flag = sb("flag", [128, 1], F32)
        cw = sb("cw", [128, 8, 4], F32)
        cb = sb("cb", [128, 8], F32)
        ibias = sb("ibias", [4, 1], F32)
        fbias = sb("fbias", [128, 4], F32)
        Cst = sb("Cst", [128, 4, 129], F32)
        mst = sb("mst", [4, 1], F32)
        ut = [sb("ut%d" % i, [128, 8, TT], BF16) for i in range(2)]
        pre = sb("pre", [128, 8, TT + 3], F32)
        cv = sb("cv", [128, 8, TT], F32)
        qT = sb("qT", [128, 4, TT], BF16)
        kT = sb("kT", [128, 4, TT], BF16)
        so = sb("so", [128, 4, TT], F32)
        hgt = [sb("hgt%d" % i, [128, 4, TT], BF16) for i in range(2)]
        vt = sb("vt", [128, 4, 129], BF16)
        kw = sb("kw", [128, 4, 128], BF16)
        pT = sb("pT", [128, 4, 128], BF16)
        Cb = sb("Cb", [128, 4, 128], BF16)
        nbc = sb("nbc", [128, 4, 128], BF16)
        thrS = sb("thrS", [128, 512], F32)
        dd = sb("dd", [128, 512], F32)
        rr = sb("rr", [128, 512], F32)
        hh = sb("hh", [128, 512], F32)
        fx = sb("fx", [128, 4], F32)
        lt = sb("lt", [128, 4], F32)
        nbr = sb("nbr", [4, 128], F32)
        gp = sb("gp", [4, 128], F32)
        wrow = sb("wrow", [4, 128], F32)
        trow = sb("trow", [4, 128], F32)
        gmax = sb("gmax", [4, 1], F32)
        Mv = sb("Mv", [4, 1], F32)
        negM = sb("negM", [4, 1], F32)
        av = sb("av", [4, 1], F32)
        da = sb("da", [4, 4], F32)
        wtok = sb("wtok", [128, 4], F32)
        abc = sb("abc", [128, 4], F32)
        B0 = pst("b0", [128, 512])
        B1 = pst("b1", [128, 512])
        B2 = pst("b2", [128, 512])
        B3 = pst("b3", [128, 512])
        B4 = pst("b4", [128, 1024], BF16)
        B5 = pst("b5", [128, 512])
        B6 = pst("b6", [128, 512])
        B7 = pst("b7", [128, 512])

        P.op('sp', lambda E: E.dma_start(out=ident_f[:], in_=T['c_ident'][:, :]), writes=['identf'], dsem="p2a_c0")
        P.op('sp', lambda E: E.dma_start(out=tri_f[:], in_=T['c_tri'][:, :]), writes=['trif'], dsem="p2a_c1")
        P.op('sp', lambda E: E.dma_start(out=flag[:], in_=T['c_flag'][:, :]), writes=['flag'], dsem="p2a_c2")
        P.op('sp', lambda E: E.dma_start(out=cw[:], in_=T['conv_w'][:, :, :]), writes=['cw'], dsem="p2a_c3")
        P.op('sp', lambda E: E.dma_start(out=cb[:], in_=T['conv_b'][:, :]), writes=['cb'], dsem="p2a_c4")
        P.op('sp', lambda E: E.dma_start(out=ibias[:], in_=T['i_bias'][:, :]), writes=['ibias'], dsem="p2a_c5")
        P.op('sp', lambda E: E.dma_start(out=fbias[:], in_=T['f_bias'][:, :]), writes=['fbias'], dsem="p2a_c6")
        w_in_r = T['w_in'].rearrange("(c p) n -> p c n", p=128)
        P.op('sp', lambda E: E.dma_start(out=wgf[:], in_=w_in_r[:, :, C_MI:C_MI + 8]), writes=['wgf'], dsem="p2a_c7")
        P.op('pool', lambda E: E.tensor_copy(wgt[:], wgf[:]), reads=['wgf'], writes=['wgt'])
        P.op('pool', lambda E: E.tensor_copy(ident_bf[:], ident_f[:]), reads=['identf'], writes=['identb'])
        P.op('pool', lambda E: E.tensor_copy(mask_bf[:], tri_f[:]), reads=['trif'], writes=['maskb'])
        P.op('pool', lambda E: E.memset(ones_bf[:], 1.0), writes=['onesb'])
        P.op('pool', lambda E: E.memset(ones4[:], 1.0), writes=['ones4'])
        P.op('pool', lambda E: E.memset(Cst[:], 0.0), writes=['Cst'])
        P.op('pool', lambda E: E.memset(mst[:], 0.0), writes=['mst'])
        P.op('pool', lambda E: E.memset(vt[:], 1.0), writes=['vt'])
        P.op('pool', lambda E: E.memset(pre[:], 0.0), writes=[('pre', c) for c in range(8)])
        for h in range(4):
            P.op('pool', lambda E, h=h: E.tensor_scalar(selc[:, h, :], ones4[:], ident_f[0:4, h:h + 1], None, op0=ALU.mult),
                 reads=['ones4', 'identf'], writes=['selc'])
        pieces = []
        for k in range(8):
            for q in range(4):
                pieces.append((wm[:, k, q * 512:(q + 1) * 512], w_in_r[:, k, C_MQ + q * 512:C_MQ + (q + 1) * 512], ('wm', k, q)))
        load_cast(P, stage, pieces)
        u_r = T['u'].rearrange("(c p) n -> p c n", p=128)
        hg_r = T['hg'].rearrange("(c p) n -> p c n", p=128)

        def load_u(i):
            d = i % 2
            P.op('sp', lambda E: E.dma_start(out=ut[d][:], in_=u_r[:, :, i * TT:(i + 1) * TT]), writes=[('ut', d)], dsem="p2a_ut%d" % d)

        load_u(t_start)
        for i in range(t_start, t_end):
            d = i % 2
            own = i >= own0
            if i + 1 < t_end:
                load_u(i + 1)
            fcs = ([(0, fc) for fc in range(4)] if (own or i == own0 - 1) else []) + [(1, fc) for fc in range(4)]
            for (kind, fc) in fcs:
                c8 = kind * 4 + fc
                mm_group(P, B0[:], [(wm[:, k, c8 * 128:(c8 + 1) * 128], ut[d][:, k, :]) for k in range(8)],
                         reads=[('wm', k, kind) for k in range(8)] + [('ut', d)], writes=['B0'])
                P.op('act', lambda E: E.copy(out=pre[:, c8, 3:TT + 3], in_=B0[:]), reads=['B0'], writes=[('pre', c8)])
            if own:
                for fc in range(4):
                    mm_group(P, B0[:], [(wm[:, k, 1536 + fc * 128:1536 + (fc + 1) * 128], ut[d][:, k, :]) for k in range(8)],
                             reads=[('wm', k, 3) for k in range(8)] + [('ut', d)], writes=['B0'])
                    P.op('act', lambda E: E.activation(out=so[:, fc, :], in_=B0[:], func=AF.Sigmoid), reads=['B0'], writes=[('so', fc)])
            for (kind, fc) in fcs:
                c8 = kind * 4 + fc
                eng = 'dve'
                P.op(eng, lambda E: E.tensor_scalar(cv[:, c8, :], pre[:, c8, 0:TT], cw[:, c8, 0:1], cb[:, c8:c8 + 1], op0=ALU.mult, op1=ALU.add),
                     reads=[('pre', c8), 'cw', 'cb'], writes=[('cv', c8)])
                for j in range(1, 4):
                    P.op(eng, lambda E: E.scalar_tensor_tensor(cv[:, c8, :], pre[:, c8, j:TT + j], cw[:, c8, j:j + 1], cv[:, c8, :], op0=ALU.mult, op1=ALU.add),
                         reads=[('pre', c8), ('cv', c8), 'cw'], writes=[('cv', c8)])
                P.op(eng, lambda E: E.tensor_copy(pre[:, c8, 0:3], pre[:, c8, TT:TT + 3]), reads=[('pre', c8)], writes=[('pre', c8)])
                if kind == 0:
                    P.op('act', lambda E: E.activation(out=cv[:, c8, :], in_=cv[:, c8, :], func=AF.Silu), reads=[('cv', c8)], writes=[('cv', c8)])
                    P.op('dve', lambda E: E.tensor_scalar(qT[:, fc, :], cv[:, c8, :], SC, None, op0=ALU.mult), reads=[('cv', c8)], writes=[('qT', fc)])
                else:
                    P.op('act', lambda E: E.activation(out=kT[:, fc, :], in_=cv[:, c8, :], func=AF.Silu), reads=[('cv', c8)], writes=[('kT', fc)])
            for ci in range(4):
                c0 = ci * 128
                cs = slice(c0, c0 + 128)
                mm_group(P, B1[:], [(ut[d][:, k, cs], wm[:, k, 1024:1536]) for k in range(8)],
                         reads=[('wm', k, 2) for k in range(8)] + [('ut', d)], writes=['B1'])
                mm_group(P, B2[:, 0:8], [(ut[d][:, k, cs], wgt[:, k, 0:8]) for k in range(8)], reads=['wgt', ('ut', d)], writes=['B2'])
                mm_group(P, B2[0:4, 8:136], [(wgt[:, k, 0:4], ut[d][:, k, cs]) for k in range(8)], reads=['wgt', ('ut', d)], writes=['B2'])
                P.op('act', lambda E: E.copy(out=vt[:, :, 0:128], in_=B1[:].rearrange("p (h e) -> p h e", h=4)), reads=['B1'], writes=['vt'])
                P.op('dve', lambda E: E.tensor_tensor(fx[:], B2[:, 4:8], fbias[:], op=ALU.add), reads=['B2', 'fbias'], writes=['fx'])
                P.op('act', lambda E: E.activation(out=fx[:], in_=fx[:], func=AF.Exp, scale=-1.0), reads=['fx'], writes=['fx'])
                P.op('act', lambda E: E.activation(out=lt[:], in_=fx[:], func=AF.Ln, bias=1.0, scale=1.0), reads=['fx'], writes=['lt'])
                mm_group(P, B3[0:4, 0:128], [(lt[:], tri_f[:])], reads=['lt', 'trif'], writes=['B3'])
                P.op('act', lambda E: E.copy(out=nbr[:], in_=B3[0:4, 0:128]), reads=['B3'], writes=['nbr'])
                P.op('dve', lambda E: E.scalar_tensor_tensor(gp[:], B2[0:4, 8:136], ibias[:, 0:1], nbr[:], op0=ALU.add, op1=ALU.add),
                     reads=['B2', 'ibias', 'nbr'], writes=['gp'])
                P.op('dve', lambda E: E.reduce_max(gmax[:], gp[:], axis=AX.X), reads=['gp'], writes=['gmax'])
                P.op('dve', lambda E: E.tensor_tensor(Mv[:], gmax[:], mst[:], op=ALU.max), reads=['gmax', 'mst'], writes=['Mv'])
                P.op('dve', lambda E: E.tensor_scalar(negM[:], Mv[:], -1.0, None, op0=ALU.mult), reads=['Mv'], writes=['negM'])
                P.op('act', lambda E: E.activation(out=wrow[:], in_=gp[:], func=AF.Exp, bias=negM[:, 0:1], scale=1.0), reads=['gp', 'negM'], writes=['wrow'])
                P.op('act', lambda E: E.activation(out=av[:], in_=mst[:], func=AF.Exp, bias=negM[:, 0:1], scale=1.0), reads=['mst', 'negM'], writes=['av'])
                if own:
                    P.op('act', lambda E: E.activation(out=trow[:], in_=nbr[:], func=AF.Exp, bias=negM[:, 0:1], scale=1.0), reads=['nbr', 'negM'], writes=['trow'])
                P.op('dve', lambda E: E.tensor_tensor(mst[:], Mv[:], nbr[:, 127:128], op=ALU.subtract), reads=['Mv', 'nbr'], writes=['mst'])
                mm_group(P, B3[:, 128:132], [(wrow[:], ident_f[0:4, 0:4])], reads=['wrow', 'identf'], writes=['B3'])
                P.op('dve', lambda E: E.tensor_scalar(da[:], ident_f[0:4, 0:4], av[:, 0:1], None, op0=ALU.mult), reads=['av', 'identf'], writes=['da'])
                mm_group(P, B3[:, 136:140], [(ones4[:], da[:])], reads=['ones4', 'da'], writes=['B3'])
                if own:
                    P.op('dve', lambda E: E.tensor_copy(wtok[:], B3[:, 128:132]), reads=['B3'], writes=['wtok'])
                else:
                    P.op('dve', lambda E: E.tensor_scalar(wtok[:], B3[:, 128:132], flag[:, 0:1], None, op0=ALU.mult), reads=['B3', 'flag'], writes=['wtok'])
                P.op('dve', lambda E: E.tensor_copy(abc[:], B3[:, 136:140]), reads=['B3'], writes=['abc'])
                for h in range(4):
                    P.op('dve', lambda E, h=h: E.tensor_scalar(Cst[:, h, :], Cst[:, h, :], abc[:, h:h + 1], None, op0=ALU.mult), reads=['Cst', 'abc'], writes=['Cst'])
                def trf(E):
                    ins = None
                    for h in range(4):
                        ins = E.transpose(B4[:, h * 128:(h + 1) * 128], kT[:, h, cs], ident_bf[:])
                    return ins
                P.op('pe', trf, reads=[('kT', h) for h in range(4)] + ['identb'], writes=['B4'])
                for h in range(4):
                    P.op('dve', lambda E, h=h: E.tensor_scalar(kw[:, h, :], B4[:, h * 128:(h + 1) * 128], wtok[:, h:h + 1], None, op0=ALU.mult),
                         reads=['B4', 'wtok'], writes=['kw'])
                if own:
                    P.op('act', lambda E: E.copy(out=Cb[:], in_=Cst[:, :, 0:128]), reads=['Cst'], writes=['Cb'])
                    for h in range(4):
                        P.op('dve', lambda E, h=h: E.tensor_scalar(nbc[:, h, :], ones_bf[:], Cst[:, h, 128:129], None, op0=ALU.mult), reads=['Cst', 'onesb'], writes=['nbc'])
                    def qkf(E):
                        ins = None
                        for h in range(4):
                            ins = E.matmul(B5[:, h * 128:(h + 1) * 128], kT[:, h, cs], qT[:, h, cs], start=True, stop=True)
                        return ins
                    P.op('pe', qkf, reads=[('kT', h) for h in range(4)] + [('qT', h) for h in range(4)], writes=['B5'])
                    for h in range(4):
                        P.op('dve', lambda E, h=h: E.scalar_tensor_tensor(pT[:, h, :], B5[:, h * 128:(h + 1) * 128], wtok[:, h:h + 1], mask_bf[:], op0=ALU.mult, op1=ALU.mult),
                             reads=['B5', 'wtok', 'maskb'], writes=['pT'])
                    def numf(E):
                        ins = None
                        for h in range(4):
                            E.matmul(B6[:, h * 128:(h + 1) * 128], Cb[:, h, :], qT[:, h, cs], start=True, stop=False)
                            ins = E.matmul(B6[:, h * 128:(h + 1) * 128], vt[:, h, 0:128], pT[:, h, :], start=False, stop=True)
                        return ins
                    P.op('pe', numf, reads=['Cb', 'vt', 'pT'] + [('qT', h) for h in range(4)], writes=['B6'])

                    def denf(E):
                        ins = None
                        for h in range(4):
                            E.matmul(B1[:, h * 128:(h + 1) * 128], nbc[:, h, :], qT[:, h, cs], start=True, stop=False)
                            ins = E.matmul(B1[:, h * 128:(h + 1) * 128], ones_bf[:], pT[:, h, :], start=False, stop=True)
                        return ins
                    P.op('pe', denf, reads=['nbc', 'onesb', 'pT'] + [('qT', h) for h in range(4)], writes=['B1'])

                    def thrf(E):
                        ins = None
                        for h in range(4):
                            ins = E.matmul(B0[:, h * 128:(h + 1) * 128], selc[:, h, :], trow[:], start=True, stop=True)
                        return ins
                    P.op('pe', thrf, reads=['selc', 'trow'], writes=['B0'])
                    P.op('act', lambda E: E.copy(out=thrS[:], in_=B0[:]), reads=['B0'], writes=['thrS'])
                    P.op('dve', lambda E: E.tensor_tensor(dd[:], B1[:], thrS[:], op=ALU.max), reads=['B1', 'thrS'], writes=['dd'])
                    P.op('dve', lambda E: E.scalar_tensor_tensor(dd[:], B1[:], -1.0, dd[:], op0=ALU.mult, op1=ALU.max), reads=['B1', 'dd'], writes=['dd'])
                    P.op('act', lambda E: E.activation(out=rr[:], in_=dd[:], func=AF.Ln), reads=['dd'], writes=['rr'])
                    P.op('act', lambda E: E.activation(out=rr[:], in_=rr[:], func=AF.Exp, scale=-1.0), reads=['rr'], writes=['rr'])
                    P.op('dve', lambda E: E.tensor_tensor(hh[:], B6[:], rr[:], op=ALU.mult), reads=['B6', 'rr'], writes=['hh'])
                    P.op('pool', lambda E: E.tensor_tensor(hgt[d][:, :, cs], hh[:].rearrange("p (h t) -> p h t", h=4), so[:, :, cs], op=ALU.mult),
                         reads=['hh'] + [('so', fc) for fc in range(4)], writes=[('hgt', d)])
                def dcf(E):
                    ins = None
                    for h in range(4):
                        bank = B5 if h < 2 else B7
                        ins = E.matmul(bank[:, (h % 2) * 129:(h % 2 + 1) * 129], kw[:, h, :], vt[:, h, :], start=True, stop=True)
                    return ins
                P.op('pe', dcf, reads=['kw', 'vt'], writes=['B5', 'B7'])
                P.op('dve', lambda E: E.tensor_tensor(Cst[:, 0:2, :], Cst[:, 0:2, :], B5[:, 0:258].rearrange("p (h e) -> p h e", h=2), op=ALU.add), reads=['Cst', 'B5'], writes=['Cst'])
                P.op('dve', lambda E: E.tensor_tensor(Cst[:, 2:4, :], Cst[:, 2:4, :], B7[:, 0:258].rearrange("p (h e) -> p h e", h=2), op=ALU.add), reads=['Cst', 'B7'], writes=['Cst'])
            if own:
                io = i - own0
                P.op('sp', lambda E: E.dma_start(out=hg_r[:, :, io * TT:(io + 1) * TT], in_=hgt[d][:]), reads=[('hgt', d)], dsem="p2a_hg%d" % d)
        P.barrier()


def attn_phase(P, nc, T):
    TT = 512
    L = 6144
    SCALE = 128 ** -0.5
    slots = int(os.environ.get('KSLOTS', 4))
    with ExitStack() as ph:
        def sb(name, shape, dt):
            return ph.enter_context(nc.sbuf_tensor("p2b_" + name, shape, dt))

        def pst(name, shape, dt=F32):
            return ph.enter_context(nc.psum_tensor("p2b_" + name, shape, dt))
        ures = sb("ures", [128, 8, L], BF16)
        wq = sb("wq", [128, 8, 128], BF16)
        wk = sb("wk", [128, 8, 128], BF16)
        wv = sb("wv", [128, 8, 128], BF16)
        stage = [sb("stg%d" % i, [128, 128], F32) for i in range(4)]
        qT = sb("qT", [128, OWN], BF16)
        kT = sb("kT", [128, L], BF16)
        vt = sb("vt", [128, 48, 128], BF16)
        acc = sb("acc", [128, 2, OWN], F32)
        ones_bf = sb("onesb", [128, 128], BF16)
        fones = sb("fones", [128, 128], BF16)
        mask2 = sb("mask2", [128, 512], BF16)
        trif = sb("trif", [128, 128], F32)
        tritf = sb("tritf", [128, 128], F32)
        flag = sb("flag", [128, 1], F32)
        gq = sb("gq", [128, 1], F32)
        gk = sb("gk", [128, 1], F32)
        rm = sb("rm", [32, 32], F32)
        sq = [sb("sq%d" % i, [128, TT], BF16) for i in range(2)]
        tmp = [sb("tmp%d" % i, [128, TT], F32) for i in range(2)]
        rstd = [sb("rstd%d" % i, [128, TT], F32) for i in range(2)]
        qn = [sb("qn%d" % i, [128, TT], F32) for i in range(2)]
        t2 = [sb("t2%d" % i, [32, TT], F32) for i in range(2)]
        epsc = sb("epsc", [128, 1], F32)
        qnb = [sb("qnb%d" % i, [32, TT], BF16) for i in range(2)]
        rmb = sb("rmb", [32, 32], BF16)
        cosb = [sb("cos%d" % i, [32, TT], F32) for i in range(2)]
        sinb = [sb("sin%d" % i, [32, TT], F32) for i in range(2)]
        pT = [sb("pT%d" % i, [128, 512], BF16) for i in range(2)]
        atto = [sb("atto%d" % i, [128, TT], BF16) for i in range(2)]
        rden = [sb("rden%d" % i, [128, TT], F32) for i in range(2)]
        BK = [pst("bk%d" % i, [128, 512]) for i in range(8)]
        BSC = [BK[0], BK[1]]
        BN = [BK[2], BK[3]]

        P.op('sp', lambda E: E.dma_start(out=trif[:], in_=T['c_tri'][:, :]), writes=['trif'], dsem="p2b_c0")
        P.op('sp', lambda E: E.dma_start(out=tritf[:], in_=T['c_trit'][:, :]), writes=['tritf'], dsem="p2b_c1")
        P.op('sp', lambda E: E.dma_start(out=flag[:], in_=T['c_flag'][:, :]), writes=['flag'], dsem="p2b_c2")
        P.op('sp', lambda E: E.dma_start(out=gq[:], in_=T['q_gain'][:, :]), writes=['gq'], dsem="p2b_c3")
        P.op('sp', lambda E: E.dma_start(out=gk[:], in_=T['k_gain'][:, :]), writes=['gk'], dsem="p2b_c4")
        P.op('sp', lambda E: E.dma_start(out=rm[:], in_=T['c_rm'][:, :]), writes=['rm'], dsem="p2b_c5")
        P.op('pool', lambda E: E.memset(ones_bf[:], 1.0), writes=['onesb'])
        P.op('pool', lambda E: E.memset(epsc[:], EPS), writes=['epsc'])
        P.op('pool', lambda E: E.tensor_copy(rmb[:], rm[:]), reads=['rm'], writes=['rmb'])
        P.op('pool', lambda E: E.tensor_scalar(fones[:], ones_bf[:], flag[:, 0:1], None, op0=ALU.mult), reads=['onesb', 'flag'], writes=['fones'])
        for qb in range(2):
            P.op('pool', lambda E, qb=qb: E.tensor_copy(mask2[:, (qb * 2) * 128:(qb * 2 + 1) * 128], tritf[:]), reads=['tritf'], writes=['mask2'])
            P.op('pool', lambda E, qb=qb: E.tensor_copy(mask2[:, (qb * 2 + 1) * 128:(qb * 2 + 2) * 128], trif[:]), reads=['trif'], writes=['mask2'])
        u_r = T['u'].rearrange("(c p) n -> p c n", p=128)
        for tl in range(12):
            P.op('sp', lambda E, tl=tl: E.dma_start(out=ures[:, :, tl * TT:(tl + 1) * TT], in_=u_r[:, :, 2048 + tl * TT:2048 + (tl + 1) * TT]),
                 writes=[('ures', tl)], dsem="p2b_u%d" % tl)
            if tl % 4 == 3:
                pass
        ures_all = [('ures', tl) for tl in range(12)]
        w_in_r = T['w_in'].rearrange("(c p) n -> p c n", p=128)
        att_r = T['att'].rearrange("(c p) n -> p c n", p=128)
        cnt = [0]

        def jobinfo(kind, tl, g):
            l0 = tl * TT
            w, wkey, gain, gkey = (wq, 'wq', gq, 'gq') if kind == 'q' else (wk, 'wk', gk, 'gk')
            dst = qT[:, l0 - 2048:l0 - 2048 + TT] if kind == 'q' else kT[:, l0:l0 + TT]
            cs = g % 2
            return dict(kind=kind, tl=tl, l0=l0, w=w, wkey=wkey, gain=gain, gkey=gkey, dst=dst, cs=cs,
                        BQ=BK[g % 3], BS=BK[3 + cs], BR=BK[5 + cs], kq=('bk', g % 3), ks=('bk', 3 + cs), kr=('bk', 5 + cs))

        def stA(j):
            cs, BQ, l0, w = j['cs'], j['BQ'], j['l0'], j['w']
            mm_group(P, BQ[:], [(w[:, k, :], ures[:, k, l0:l0 + TT]) for k in range(8)],
                     reads=[(j['wkey'], k) for k in range(8)] + [('ures', j['tl'])], writes=[j['kq']])
            P.op('act', lambda E: E.activation(out=sq[cs][:], in_=BQ[:], func=AF.Square), reads=[j['kq']], writes=[('sq', cs)])

        def stB(j):
            cs, BQ, BS, l0 = j['cs'], j['BQ'], j['BS'], j['l0']
            P.op('sp', lambda E: E.dma_start(out=cosb[cs][:], in_=T['c_cos'][:, l0:l0 + TT]), writes=[('cos', cs)], dsem="p2b_cos%d" % cs)
            P.op('sp', lambda E: E.dma_start(out=sinb[cs][:], in_=T['c_sin'][:, l0:l0 + TT]), writes=[('sin', cs)], dsem="p2b_sin%d" % cs)
            mm_group(P, BS[:], [(ones_bf[:], sq[cs][:])], reads=['onesb', ('sq', cs)], writes=[j['ks']])
            P.op('act', lambda E: E.activation(out=tmp[cs][:], in_=BS[:], func=AF.Ln, bias=epsc[:, 0:1], scale=1.0 / 128), reads=[j['ks'], 'epsc'], writes=[('tmp', cs)])
            P.op('act', lambda E: E.activation(out=rstd[cs][:], in_=tmp[cs][:], func=AF.Exp, scale=-0.5), reads=[('tmp', cs)], writes=[('rstd', cs)])
            dst, kind, tl = j['dst'], j['kind'], j['tl']
            P.op('dve', lambda E: E.scalar_tensor_tensor(dst[:, :], BQ[:], j['gain'][:, 0:1], rstd[cs][:], op0=ALU.mult, op1=ALU.mult),
                 reads=[j['kq'], j['gkey'], ('rstd', cs)], writes=[(kind + 'T', tl)])
            P.op('dve', lambda E: E.scalar_tensor_tensor(qn[cs][0:32, :], BQ[0:32, :], j['gain'][0:32, 0:1], rstd[cs][0:32, :], op0=ALU.mult, op1=ALU.mult),
                 reads=[j['kq'], j['gkey'], ('rstd', cs)], writes=[('qn', cs)])

        def stC(j):
            cs, BR, dst, kind, tl = j['cs'], j['BR'], j['dst'], j['kind'], j['tl']
            mm_group(P, BR[0:32, :], [(rmb[:], dst[0:32, :])], reads=['rmb', (kind + 'T', tl)], writes=[j['kr']])
            P.op('pool', lambda E: E.tensor_tensor(qn[cs][0:32, :], qn[cs][0:32, :], cosb[cs][:], op=ALU.mult), reads=[('qn', cs), ('cos', cs)], writes=[('qn', cs)])
            P.op('dve', lambda E: E.tensor_tensor(t2[cs][:], BR[0:32, :], sinb[cs][:], op=ALU.mult), reads=[j['kr'], ('sin', cs)], writes=[('t2', cs)])
            P.op('dve', lambda E: E.tensor_tensor(dst[0:32, :], qn[cs][0:32, :], t2[cs][:], op=ALU.add), reads=[('qn', cs), ('t2', cs)], writes=[(kind + 'T', tl)])

        for s in range(slots):
            for g in range(3):
                d = DILS[g]
                J = OWN // (128 * d)
                hk = g * 4 + s
                pieces = []
                for (wt, nm, c0) in ((wq, 'wq', C_AQ), (wk, 'wk', C_AK), (wv, 'wv', C_AV)):
                    for k in range(8):
                        pieces.append((wt[:, k, :], w_in_r[:, k, c0 + hk * 128:c0 + (hk + 1) * 128], (nm, k)))
                load_cast(P, stage, pieces, engs=('pool', 'act'))
                ktl0 = 0 if d == 16 else 3
                ktiles = list(range(ktl0, 12))
                jl = [('k', tl) for tl in ktiles] + [('q', tl) for tl in range(4, 12)]
                jobs = [jobinfo(kd, tl, cnt[0] + ix) for ix, (kd, tl) in enumerate(jl)]
                cnt[0] += len(jobs)
                nj = len(jobs)
                for t in range(nj + 2):
                    if t < nj:
                        stA(jobs[t])
                    if 0 <= t - 1 < nj:
                        stB(jobs[t - 1])
                    if 0 <= t - 2 < nj:
                        stC(jobs[t - 2])
                kkeys = [('kT', tl) for tl in ktiles] + [('kTb', tl) for tl in ktiles] + [('kTc', tl) for tl in ktiles]
                qkeys = [('qT', tl) for tl in range(4, 12)] + [('qTb', tl) for tl in range(4, 12)] + [('qTc', tl) for tl in range(4, 12)]
                nblk = d * (J + 1)
                blks = [(r, j) for r in range(d) for j in range(-1, J)]
                for b0 in range(0, nblk, 4):
                    grp = blks[b0:b0 + 4]

                    vb = (b0 // 4) % 2
                    BVb = BK[2 + vb]

                    def vf(E, grp=grp, BVb=BVb):
                        ins = None
                        for qi, (r, j) in enumerate(grp):
                            u0 = 2048 // d + 128 * j
                            for k in range(8):
                                lhs = ures[:, k, :].rearrange("p (u d) -> p d u", d=d)[:, r, u0:u0 + 128]
                                ins = E.matmul(BVb[:, qi * 128:(qi + 1) * 128], lhs, wv[:, k, :], start=(k == 0), stop=(k == 7))
                        return ins
                    P.op('pe', vf, reads=[('wv', k) for k in range(8)] + ures_all, writes=[('bk', 2 + vb)])
                    n = len(grp)
                    P.op('act' if vb == 0 else 'dve', (lambda E, b0=b0, n=n, BVb=BVb: E.copy(out=vt[:, b0:b0 + n, :], in_=BVb[:, 0:n * 128].rearrange("p (b e) -> p b e", e=128))) if vb == 0 else
                         (lambda E, b0=b0, n=n, BVb=BVb: E.tensor_copy(vt[:, b0:b0 + n, :], BVb[:, 0:n * 128].rearrange("p (b e) -> p b e", e=128))),
                         reads=[('bk', 2 + vb)], writes=[('vt', b0)])
                kview = kT[:, :].rearrange("p (u d) -> p d u", d=d)
                qview = qT[:, :].rearrange("p (u d) -> p d u", d=d)
                accv = acc[:, :, :].rearrange("p n (u d) -> p n d u", d=d)
                it = 0
                pend = None
                for r in range(d):
                    for jp in range(J // 2):
                        j0 = 2 * jp
                        b = it % 2
                        it += 1

                        def sf(E, r=r, j0=j0, b=b):
                            ins = None
                            for qb in range(2):
                                j = j0 + qb
                                qa = qview[:, r, 128 * j:128 * j + 128]
                                for pc in range(2):
                                    jj = j - 1 + pc
                                    u0 = 2048 // d + 128 * jj
                                    ka = kview[:, r, u0:u0 + 128]
                                    ins = E.matmul(BSC[b][:, (qb * 2 + pc) * 128:(qb * 2 + pc + 1) * 128], ka, qa, start=True, stop=True)
                            return ins
                        P.op('pe', sf, reads=kkeys + qkeys, writes=[('bk', b)])
                        P.op('act', lambda E, b=b: E.activation(out=pT[b][:], in_=BSC[b][:], func=AF.Exp, scale=SCALE), reads=[('bk', b)], writes=[('pT', b)])
                        P.op('pool' if b == 0 else 'dve', lambda E, b=b: E.tensor_tensor(pT[b][:], pT[b][:], mask2[:], op=ALU.mult), reads=[('pT', b), 'mask2'], writes=[('pT', b)])

                        def fin(r=r, j0=j0, b=b):
                            def nf(E):
                                ins = None
                                for qb in range(2):
                                    j = j0 + qb
                                    bp = r * (J + 1) + j
                                    bc = bp + 1
                                    pp = pT[b][:, (qb * 2) * 128:(qb * 2 + 1) * 128]
                                    pcur = pT[b][:, (qb * 2 + 1) * 128:(qb * 2 + 2) * 128]
                                    E.matmul(BN[b][:, qb * 128:(qb + 1) * 128], vt[:, bp, :], pp, start=True, stop=False)
                                    E.matmul(BN[b][:, qb * 128:(qb + 1) * 128], vt[:, bc, :], pcur, start=False, stop=True)
                                    E.matmul(BN[b][:, (2 + qb) * 128:(3 + qb) * 128], (fones if j == 0 else ones_bf)[:], pp, start=True, stop=False)
                                    ins = E.matmul(BN[b][:, (2 + qb) * 128:(3 + qb) * 128], ones_bf[:], pcur, start=False, stop=True)
                                return ins
                            P.op('pe', nf, reads=[('vt', (bb // 4) * 4) for bb in (r * (J + 1) + j0, r * (J + 1) + j0 + 1, r * (J + 1) + j0 + 2)] + [('pT', b), 'onesb', 'fones'], writes=[('bk', 2 + b)])
                            av = accv[:, :, r, 128 * j0:128 * j0 + 256]
                            bnv = BN[b][:].rearrange("p (n x) -> p n x", n=2)
                            if g == 0:
                                P.op('act', lambda E: E.copy(out=av, in_=bnv), reads=[('bk', 2 + b)], writes=['acc'])
                            else:
                                P.op('dve', lambda E: E.tensor_tensor(av, av, bnv, op=ALU.add), reads=[('bk', 2 + b), 'acc'], writes=['acc'])
                        if pend is not None:
                            pend()
                        pend = fin
                if pend is not None:
                    pend()
            for tl in range(8):
                o = tl % 2
                P.op('act', lambda E: E.activation(out=rden[o][:], in_=acc[:, 1, tl * TT:(tl + 1) * TT], func=AF.Ln), reads=['acc'], writes=[('rden', o)])
                P.op('act', lambda E: E.activation(out=rden[o][:], in_=rden[o][:], func=AF.Exp, scale=-1.0), reads=[('rden', o)], writes=[('rden', o)])
                P.op('dve', lambda E: E.tensor_tensor(atto[o][:], acc[:, 0, tl * TT:(tl + 1) * TT], rden[o][:], op=ALU.mult), reads=['acc', ('rden', o)], writes=[('atto', o)])
                P.op('sp', lambda E: E.dma_start(out=att_r[:, s, tl * TT:(tl + 1) * TT], in_=atto[o][:]), reads=[('atto', o)], dsem="p2b_ao%d" % o)
        P.barrier()


def build(debug=0):
    nc = bass.Bass("TRN2", target_bir_lowering=False)
    T = {}

    def din(name, shape, dt=F32):
        T[name] = nc.dram_tensor(name, shape, dt, kind="ExternalInput").ap()

    def scratch(name, shape, dt):
        kind = {"kind": "ExternalOutput"} if debug else {}
        T[name] = nc.dram_tensor(name, shape, dt, **kind).ap()
    din('xT', [D, NTOK])
    din('pT', [256, OWN])
    din('ffn1_norm', [128, 8]); din('mix_norm', [128, 8]); din('ffn2_norm', [128, 8]); din('ple_norm', [128, 8])
    din('ffn1_w_in', [D, 2 * DFF]); din('ffn1_w_out', [DFF, D])
    din('ffn2_w_in', [D, 2 * DFF]); din('ffn2_w_out', [DFF, D])
    din('w_in', [D, DIN])
    din('w_up_att', [512, D]); din('w_up_mlstm', [512, D]); din('w_out', [D, D])
    din('w_ple_gate', [D, D]); din('w_ple_proj', [256, D])
    din('conv_w', [128, 8, 4]); din('conv_b', [128, 8]); din('i_bias', [4, 1]); din('f_bias', [128, 4])
    din('q_gain', [128, 1]); din('k_gain', [128, 1])
    din('c_ident', [128, 128]); din('c_tri', [128, 128]); din('c_trit', [128, 128]); din('c_flag', [128, 1])
    din('c_rm', [32, 32]); din('c_cos', [32, 6144]); din('c_sin', [32, 6144])
    T['outT'] = nc.dram_tensor('outT', [D, OWN], F32, kind="ExternalOutput").ap()
    scratch('h1', [D, OWN], F32)
    scratch('u', [D, NTOK], BF16)
    scratch('att', [512, OWN], BF16)
    scratch('hg', [512, OWN], BF16)
    scratch('h2', [D, OWN], F32)
    stages = os.environ.get('KSTAGES', '1abcd')
    with ExitStack() as es:
        P = Prog(nc, es)
        if '1' in stages:
            ffn_phase(P, nc, T, 1)
        if 'a' in stages:
            mlstm_phase(P, nc, T)
        if 'b' in stages:
            attn_phase(P, nc, T)
        if 'c' in stages:
            merge_phase(P, nc, T)
        if 'd' in stages:
            ffn_phase(P, nc, T, 2)
    return nc


def _chunk_major(v):
    return np.ascontiguousarray(v.reshape(-1, 128).T).astype(np.float32)


def make_in_maps(inputs):
    x = np.asarray(inputs['x'], dtype=np.float32)
    p = np.asarray(inputs['p'], dtype=np.float32)[0]
    shared = {
        'ffn1_norm': _chunk_major(inputs['ffn1_norm'][0]), 'mix_norm': _chunk_major(inputs['mix_norm'][0]),
        'ffn2_norm': _chunk_major(inputs['ffn2_norm'][0]), 'ple_norm': _chunk_major(inputs['ple_norm'][0]),
        'ffn1_w_in': np.ascontiguousarray(inputs['ffn1_w_in'][0]), 'ffn1_w_out': np.ascontiguousarray(inputs['ffn1_w_out'][0]),
        'ffn2_w_in': np.ascontiguousarray(inputs['ffn2_w_in'][0]), 'ffn2_w_out': np.ascontiguousarray(inputs['ffn2_w_out'][0]),
        'w_in': np.ascontiguousarray(inputs['w_in'][0]),
        'w_up_att': np.ascontiguousarray(inputs['w_up_att'][0]), 'w_up_mlstm': np.ascontiguousarray(inputs['w_up_mlstm'][0]),
        'w_out': np.ascontiguousarray(inputs['w_out'][0]),
        'w_ple_gate': np.ascontiguousarray(inputs['w_ple_gate'][0]), 'w_ple_proj': np.ascontiguousarray(inputs['w_ple_proj'][0]),
    }
    cw = np.asarray(inputs['conv_w'][0], np.float32)
    shared['conv_w'] = np.ascontiguousarray(cw.reshape(4, 8, 128).transpose(2, 1, 0))
    shared['conv_b'] = _chunk_major(inputs['conv_b'][0])
    shared['i_bias'] = np.asarray(inputs['i_bias'][0], np.float32).reshape(4, 1).copy()
    shared['f_bias'] = np.ascontiguousarray(np.broadcast_to(np.asarray(inputs['f_bias'][0], np.float32)[None, :], (128, 4)))
    shared['q_gain'] = np.asarray(inputs['q_gain'][0], np.float32).reshape(128, 1).copy()
    shared['k_gain'] = np.asarray(inputs['k_gain'][0], np.float32).reshape(128, 1).copy()
    shared['c_ident'] = np.eye(128, dtype=np.float32)
    tri = np.triu(np.ones((128, 128), np.float32))
    shared['c_tri'] = tri
    shared['c_trit'] = np.ascontiguousarray(tri.T)
    rmm = np.zeros((32, 32), np.float32)
    for m_ in range(32):
        rmm[(m_ + 16) % 32, m_] = 1.0
    shared['c_rm'] = rmm
    half = 16
    inv_freq = (1.0 / (np.float32(500000.0) ** (np.arange(half, dtype=np.float32) / np.float32(half)))).astype(np.float32)
    maps = []
    for c in range(8):
        b, h = c // 2, c % 2
        xT = np.zeros((D, NTOK), np.float32)
        xT[:, OWN:] = x[b, h * OWN:(h + 1) * OWN].T
        if h == 1:
            xT[:, :OWN] = x[b, :OWN].T
        m = dict(shared)
        m['xT'] = xT
        m['pT'] = np.ascontiguousarray(p[b, h * OWN:(h + 1) * OWN].T)
        m['c_flag'] = np.full((128, 1), float(h), np.float32)
        pos = (np.arange(2048, 8192) - 4096 + 4096 * h).astype(np.float32)
        ang = (pos[None, :] * inv_freq[:, None]).astype(np.float32)
        cs_, sn_ = np.cos(ang).astype(np.float32), np.sin(ang).astype(np.float32)
        m['c_cos'] = np.ascontiguousarray(np.concatenate([cs_, cs_], axis=0))
        m['c_sin'] = np.ascontiguousarray(np.concatenate([-sn_, sn_], axis=0))
        maps.append(m)
    return maps


def kernel(**inputs):
    nc = build(0)
    maps = make_in_maps(inputs)
    res = run_bass_kernel_spmd(nc, maps, core_ids=list(range(8)))
    out = np.empty((4, 8192, D), np.float32)
    for c in range(8):
        b, h = c // 2, c % 2
        out[b, h * OWN:(h + 1) * OWN] = res.results[c]['outT'].T
    return out
```
